# Optimizing a Trainium2 kernel written in Bass

```python
import math
import jax, jax.numpy as jnp
from jax import lax
import numpy as np

D_MODEL = 1024
BATCH = 8
SEQ = 4096
DEPTH = 2

RG_WIDTH = 512
RG_HEADS = 8
RG_HEAD_DIM = RG_WIDTH // RG_HEADS
RG_CONV = 4
RG_C = 8.0
SB_HEADS = 8
SB_HEAD_DIM = 64
SB_WIDTH = SB_HEADS * SB_HEAD_DIM
SB_QBLOCK = 128
AB_IN = 2 * RG_WIDTH + 3 * SB_WIDTH
AB_MIX = RG_WIDTH + SB_WIDTH

S5_WIDTH = 512
S5_GROUP = 16
S5_GROUPS = S5_WIDTH // S5_GROUP
S5_STATE = 64
MOBA_HEADS = 4
MOBA_HEAD_DIM = 128
MOBA_WIDTH = MOBA_HEADS * MOBA_HEAD_DIM
MOBA_BLOCK = 256
MOBA_TOPK = 3
MOBA_QCHUNK = 32
CD_IN = S5_WIDTH + 3 * MOBA_WIDTH
CD_MIX = S5_WIDTH + MOBA_WIDTH

FFN_HIDDEN = ((8 * D_MODEL + 3 * 256 - 1) // (3 * 256)) * 256
N_EVEN = (DEPTH + 1) // 2
N_ODD = DEPTH // 2
EPS = 1e-6

kernel_name = "hybrid_rglru_stickbreak_s5_moba"


def rms_norm(x, g):
    xf = x.astype(jnp.float32)
    y = xf * lax.rsqrt(jnp.mean(xf * xf, axis=-1, keepdims=True) + EPS)
    return (y * g.astype(jnp.float32)).astype(x.dtype)


def swiglu(h, w_gate, w_up, w_down):
    return (jax.nn.silu(h @ w_gate) * (h @ w_up)) @ w_down


def alibi_slopes(n_heads):
    return 2.0 ** (-8.0 * jnp.arange(1, n_heads + 1, dtype=jnp.float32) / n_heads)


def rglru_branch(xr, gate, conv_w, conv_b, wa, ba, wx, bx, lam):
    f32 = jnp.float32
    B_, S_, _ = xr.shape
    xc = lax.conv_general_dilated(xr, conv_w, window_strides=(1,), padding=[(RG_CONV - 1, 0)],
                                  dimension_numbers=('NWC', 'WIO', 'NWC'),
                                  feature_group_count=RG_WIDTH) + conv_b
    xh = xc.reshape(B_, S_, RG_HEADS, RG_HEAD_DIM)
    r = jax.nn.sigmoid(jnp.einsum('bshi,hij->bshj', xh, wa) + ba).reshape(B_, S_, RG_WIDTH)
    i = jax.nn.sigmoid(jnp.einsum('bshi,hij->bshj', xh, wx) + bx).reshape(B_, S_, RG_WIDTH)
    log_a = (-RG_C * r.astype(f32)) * jax.nn.softplus(-lam.astype(f32))
    a = jnp.exp(log_a)
    b = jnp.sqrt(-jnp.expm1(2.0 * log_a)) * (i * xc).astype(f32)

    def combine(l, rr):
        a_l, b_l = l
        a_r, b_r = rr
        return a_r * a_l, a_r * b_l + b_r

    _, h = lax.associative_scan(combine, (a, b), axis=1)
    return h.astype(xr.dtype) * jax.nn.gelu(gate)


def stick_breaking_attention(q, k, v):
    f32 = jnp.float32
    S_ = q.shape[2]
    scale = SB_HEAD_DIM ** -0.5
    outs = []
    for blk in range(S_ // SB_QBLOCK):
        q0 = blk * SB_QBLOCK
        kend = q0 + SB_QBLOCK
        z = jnp.einsum('bhqd,bhkd->bhqk', q[:, :, q0:kend], k[:, :, :kend]).astype(f32) * scale
        q_pos = q0 + jnp.arange(SB_QBLOCK)
        k_pos = jnp.arange(kend)
        past = k_pos[None, :] < q_pos[:, None]
        log_keep = jnp.where(past, jax.nn.log_sigmoid(-z), 0.0)
        later = lax.cumsum(log_keep, axis=3, reverse=True) - log_keep
        w = jnp.where(past, jnp.exp(jax.nn.log_sigmoid(z) + later), 0.0)
        outs.append(jnp.einsum('bhqk,bhkd->bhqd', w.astype(v.dtype), v[:, :, :kend]))
    return jnp.concatenate(outs, axis=2)


def s5_branch(u, lam_re, lam_im, log_dt, b_re, b_im, c_re, c_im, d_skip, glu_w, glu_b):
    f32 = jnp.float32
    B_, S_, _ = u.shape
    uf = u.astype(f32).reshape(B_, S_, S5_GROUPS, S5_GROUP)
    dt = jnp.exp(log_dt.astype(f32))[:, None]
    lr, li = lam_re.astype(f32), lam_im.astype(f32)
    mag = jnp.exp(lr * dt)
    ab_re, ab_im = mag * jnp.cos(li * dt), mag * jnp.sin(li * dt)
    den = lr * lr + li * li
    nr, ni = ab_re - 1.0, ab_im
    f_re = (nr * lr + ni * li) / den
    f_im = (ni * lr - nr * li) / den
    br, bi = b_re.astype(f32), b_im.astype(f32)
    bb_re = f_re[..., None] * br - f_im[..., None] * bi
    bb_im = f_re[..., None] * bi + f_im[..., None] * br
    bu_re = jnp.einsum('bsgp,gnp->bsgn', uf, bb_re)
    bu_im = jnp.einsum('bsgp,gnp->bsgn', uf, bb_im)
    a_re = jnp.broadcast_to(ab_re, (1, S_) + ab_re.shape)
    a_im = jnp.broadcast_to(ab_im, (1, S_) + ab_im.shape)

    def combine(l, r):
        ar_l, ai_l, br_l, bi_l = l
        ar_r, ai_r, br_r, bi_r = r
        return (ar_r * ar_l - ai_r * ai_l,
                ar_r * ai_l + ai_r * ar_l,
                ar_r * br_l - ai_r * bi_l + br_r,
                ar_r * bi_l + ai_r * br_l + bi_r)

    _, _, x_re, x_im = lax.associative_scan(combine, (a_re, a_im, bu_re, bu_im), axis=1)
    y = (jnp.einsum('bsgn,gpn->bsgp', x_re, c_re.astype(f32))
         - jnp.einsum('bsgn,gpn->bsgp', x_im, c_im.astype(f32)))
    y = y.reshape(B_, S_, S5_WIDTH) + d_skip.astype(f32) * u.astype(f32)
    y = jax.nn.gelu(y)
    y = y * jax.nn.sigmoid(y @ glu_w.astype(f32) + glu_b.astype(f32))
    return y.astype(u.dtype)


def moba_attention(q, k, v):
    f32 = jnp.float32
    B_, H_, S_, hd = q.shape
    n_blk = -(-S_ // MOBA_BLOCK)
    s_pad = n_blk * MOBA_BLOCK
    pad = [(0, 0), (0, 0), (0, s_pad - S_), (0, 0)]
    q, k, v = jnp.pad(q, pad), jnp.pad(k, pad), jnp.pad(v, pad)
    k_blocks = k.reshape(B_, H_, n_blk, MOBA_BLOCK, hd)
    v_blocks = v.reshape(B_, H_, n_blk, MOBA_BLOCK, hd)
    k_mean = jnp.mean(k_blocks.astype(f32), axis=3)
    gate = jnp.einsum('bhsd,bhnd->bhsn', q.astype(f32), k_mean)
    q_blk = jnp.arange(s_pad) // MOBA_BLOCK
    fully_past = jnp.arange(n_blk)[None, :] < q_blk[:, None]
    gate = jnp.where(fully_past, gate, -jnp.inf)
    n_sel = min(MOBA_TOPK, n_blk)
    _, sel = lax.top_k(gate, n_sel)

    n_chunk = s_pad // MOBA_QCHUNK
    q_c = q.reshape(B_, H_, n_chunk, MOBA_QCHUNK, hd).transpose(2, 0, 1, 3, 4)
    sel_c = sel.reshape(B_, H_, n_chunk, MOBA_QCHUNK, n_sel).transpose(2, 0, 1, 3, 4)
    gather = jax.vmap(jax.vmap(lambda blocks, idx: blocks[idx]))
    slopes = alibi_slopes(H_)[None, :, None, None]
    scale = hd ** -0.5
    in_blk = jnp.arange(MOBA_BLOCK)
    n_s = n_sel * MOBA_BLOCK

    def chunk(args):
        c, qc, sc = args
        t = c * MOBA_QCHUNK + jnp.arange(MOBA_QCHUNK)
        own = (c * MOBA_QCHUNK) // MOBA_BLOCK
        k_sel = gather(k_blocks, sc).reshape(B_, H_, MOBA_QCHUNK, n_s, hd)
        v_sel = gather(v_blocks, sc).reshape(B_, H_, MOBA_QCHUNK, n_s, hd)
        k_own = lax.dynamic_slice_in_dim(k, own * MOBA_BLOCK, MOBA_BLOCK, axis=2)
        v_own = lax.dynamic_slice_in_dim(v, own * MOBA_BLOCK, MOBA_BLOCK, axis=2)
        s_pos_sel = (sc[..., None] * MOBA_BLOCK + in_blk).reshape(B_, H_, MOBA_QCHUNK, n_s)
        ok_sel = jnp.repeat(jnp.arange(n_sel) < own, MOBA_BLOCK)
        s_pos_own = own * MOBA_BLOCK + in_blk
        ok_own = s_pos_own[None, :] <= t[:, None]
        sc_sel = (jnp.einsum('bhqd,bhqkd->bhqk', qc, k_sel).astype(f32) * scale
                  - slopes * (t[:, None] - s_pos_sel).astype(f32))
        sc_sel = jnp.where(ok_sel, sc_sel, -jnp.inf)
        sc_own = (jnp.einsum('bhqd,bhkd->bhqk', qc, k_own).astype(f32) * scale
                  - slopes * (t[:, None] - s_pos_own[None, :]).astype(f32))
        sc_own = jnp.where(ok_own, sc_own, -jnp.inf)
        p = jax.nn.softmax(jnp.concatenate([sc_sel, sc_own], axis=-1), axis=-1).astype(v.dtype)
        return (jnp.einsum('bhqk,bhqkd->bhqd', p[..., :n_s], v_sel)
                + jnp.einsum('bhqk,bhkd->bhqd', p[..., n_s:], v_own))

    out = lax.map(chunk, (jnp.arange(n_chunk), q_c, sel_c))
    return out.transpose(1, 2, 0, 3, 4).reshape(B_, H_, s_pad, hd)[:, :, :S_]


def ab_mixer(h, w_in, conv_w, conv_b, wa, ba, wx, bx, lam, w_out):
    B_, S_, _ = h.shape
    proj = h @ w_in
    xr, gate, q, k, v = jnp.split(
        proj, [RG_WIDTH, 2 * RG_WIDTH, 2 * RG_WIDTH + SB_WIDTH, 2 * RG_WIDTH + 2 * SB_WIDTH], axis=-1)
    y_rg = rglru_branch(xr, gate, conv_w, conv_b, wa, ba, wx, bx, lam)
    heads = lambda t: t.reshape(B_, S_, SB_HEADS, SB_HEAD_DIM).transpose(0, 2, 1, 3)
    y_sb = stick_breaking_attention(heads(q), heads(k), heads(v))
    y_sb = y_sb.transpose(0, 2, 1, 3).reshape(B_, S_, SB_WIDTH).astype(y_rg.dtype)
    return jnp.concatenate([y_rg, y_sb], axis=-1) @ w_out


def cd_mixer(h, w_in, lam_re, lam_im, log_dt, b_re, b_im, c_re, c_im, d_skip, glu_w, glu_b, w_out):
    B_, S_, _ = h.shape
    proj = h @ w_in
    u, q, k, v = jnp.split(proj, [S5_WIDTH, S5_WIDTH + MOBA_WIDTH, S5_WIDTH + 2 * MOBA_WIDTH], axis=-1)
    y_s5 = s5_branch(u, lam_re, lam_im, log_dt, b_re, b_im, c_re, c_im, d_skip, glu_w, glu_b)
    heads = lambda t: t.reshape(B_, S_, MOBA_HEADS, MOBA_HEAD_DIM).transpose(0, 2, 1, 3)
    y_mb = moba_attention(heads(q), heads(k), heads(v))
    y_mb = y_mb.transpose(0, 2, 1, 3).reshape(B_, S_, MOBA_WIDTH).astype(y_s5.dtype)
    return jnp.concatenate([y_s5, y_mb], axis=-1) @ w_out


def setup_inputs(seed: int = 0) -> dict:
    key = jax.random.key(seed)
    ks = iter(jax.random.split(key, 32))
    f32 = jnp.float32
    NE, NO = N_EVEN, N_ODD
    G, N, P = S5_GROUPS, S5_STATE, S5_GROUP

    def nrm(shape, scale):
        return jax.random.normal(next(ks), shape, f32) * scale

    def gain(shape):
        return 1.0 + 0.01 * jax.random.normal(next(ks), shape, f32)

    x = jax.random.normal(next(ks), (BATCH, SEQ, D_MODEL), f32)
    ab_norm = gain((NE, D_MODEL))
    ab_w_in = nrm((NE, D_MODEL, AB_IN), D_MODEL ** -0.5)
    ab_conv_w = nrm((NE, RG_CONV, 1, RG_WIDTH), RG_CONV ** -0.5)
    ab_conv_b = nrm((NE, RG_WIDTH), 0.01)
    ab_gate_a_w = nrm((NE, RG_HEADS, RG_HEAD_DIM, RG_HEAD_DIM), RG_HEAD_DIM ** -0.5)
    ab_gate_a_b = nrm((NE, RG_HEADS, RG_HEAD_DIM), 0.01)
    ab_gate_x_w = nrm((NE, RG_HEADS, RG_HEAD_DIM, RG_HEAD_DIM), RG_HEAD_DIM ** -0.5)
    ab_gate_x_b = nrm((NE, RG_HEADS, RG_HEAD_DIM), 0.01)
    a_c = jax.random.uniform(next(ks), (NE, RG_WIDTH), f32, 0.9, 0.999)
    a0 = a_c ** (1.0 / RG_C)
    ab_lambda = jnp.log(a0) - jnp.log1p(-a0)
    ab_w_out = nrm((NE, AB_MIX, D_MODEL), AB_MIX ** -0.5)

    cd_norm = gain((NO, D_MODEL))
    cd_w_in = nrm((NO, D_MODEL, CD_IN), D_MODEL ** -0.5)
    cd_lam_re = -0.5 + nrm((NO, G, N), 0.01)
    cd_lam_im = math.pi * jnp.arange(N, dtype=f32) + nrm((NO, G, N), 0.01)
    cd_log_dt = jax.random.uniform(next(ks), (NO, G), f32, math.log(1e-3), math.log(1e-1))
    cd_b_re = nrm((NO, G, N, P), (2.0 * P) ** -0.5)
    cd_b_im = nrm((NO, G, N, P), (2.0 * P) ** -0.5)
    cd_c_re = nrm((NO, G, P, N), N ** -0.5)
    cd_c_im = nrm((NO, G, P, N), N ** -0.5)
    cd_d = nrm((NO, S5_WIDTH), 1.0)
    cd_glu_w = nrm((NO, S5_WIDTH, S5_WIDTH), S5_WIDTH ** -0.5)
    cd_glu_b = nrm((NO, S5_WIDTH), 0.01)
    cd_w_out = nrm((NO, CD_MIX, D_MODEL), CD_MIX ** -0.5)

    ffn_norm = gain((DEPTH, D_MODEL))
    ffn_w_gate = nrm((DEPTH, D_MODEL, FFN_HIDDEN), D_MODEL ** -0.5)
    ffn_w_up = nrm((DEPTH, D_MODEL, FFN_HIDDEN), D_MODEL ** -0.5)
    ffn_w_down = nrm((DEPTH, FFN_HIDDEN, D_MODEL), FFN_HIDDEN ** -0.5)
    final_norm = gain((D_MODEL,))
    return {"x": x, "ab_norm": ab_norm, "ab_w_in": ab_w_in, "ab_conv_w": ab_conv_w,
            "ab_conv_b": ab_conv_b, "ab_gate_a_w": ab_gate_a_w, "ab_gate_a_b": ab_gate_a_b,
            "ab_gate_x_w": ab_gate_x_w, "ab_gate_x_b": ab_gate_x_b, "ab_lambda": ab_lambda,
            "ab_w_out": ab_w_out, "cd_norm": cd_norm, "cd_w_in": cd_w_in,
            "cd_lam_re": cd_lam_re, "cd_lam_im": cd_lam_im, "cd_log_dt": cd_log_dt,
            "cd_b_re": cd_b_re, "cd_b_im": cd_b_im, "cd_c_re": cd_c_re, "cd_c_im": cd_c_im,
            "cd_d": cd_d, "cd_glu_w": cd_glu_w, "cd_glu_b": cd_glu_b, "cd_w_out": cd_w_out,
            "ffn_norm": ffn_norm, "ffn_w_gate": ffn_w_gate, "ffn_w_up": ffn_w_up,
            "ffn_w_down": ffn_w_down, "final_norm": final_norm}


def reference(x, ab_norm, ab_w_in, ab_conv_w, ab_conv_b, ab_gate_a_w, ab_gate_a_b,
              ab_gate_x_w, ab_gate_x_b, ab_lambda, ab_w_out, cd_norm, cd_w_in,
              cd_lam_re, cd_lam_im, cd_log_dt, cd_b_re, cd_b_im, cd_c_re, cd_c_im,
              cd_d, cd_glu_w, cd_glu_b, cd_w_out, ffn_norm, ffn_w_gate, ffn_w_up,
              ffn_w_down, final_norm):
    for layer in range(DEPTH):
        j = layer // 2
        if layer % 2 == 0:
            h = rms_norm(x, ab_norm[j])
            x = x + ab_mixer(h, ab_w_in[j], ab_conv_w[j], ab_conv_b[j], ab_gate_a_w[j],
                             ab_gate_a_b[j], ab_gate_x_w[j], ab_gate_x_b[j], ab_lambda[j],
                             ab_w_out[j])
        else:
            h = rms_norm(x, cd_norm[j])
            x = x + cd_mixer(h, cd_w_in[j], cd_lam_re[j], cd_lam_im[j], cd_log_dt[j],
                             cd_b_re[j], cd_b_im[j], cd_c_re[j], cd_c_im[j], cd_d[j],
                             cd_glu_w[j], cd_glu_b[j], cd_w_out[j])
        h = rms_norm(x, ffn_norm[layer])
        x = x + swiglu(h, ffn_w_gate[layer], ffn_w_up[layer], ffn_w_down[layer])
    return rms_norm(x, final_norm)
```

```python
import contextlib
import math
import numpy as np
import concourse.bass as bass
import concourse.mybir as mybir
from concourse.bass_utils import run_bass_kernel_spmd

F32 = mybir.dt.float32
BF16 = mybir.dt.bfloat16
AF = mybir.ActivationFunctionType
ALU = mybir.AluOpType
AX = mybir.AxisListType

T = 4096
D = 1024
NT = T // 512
FH = 2816
NJ = FH // 128
EPS = 1e-6


class Prog:
    ENG = ("tensor", "vector", "scalar", "gpsimd", "sync")

    def __init__(self, nc):
        self.nc = nc
        self.es = contextlib.ExitStack()
        self.ops = {e: [] for e in self.ENG}
        self.sems = {}
        self.cnt = {}
        self.seen = {e: {} for e in self.ENG}
        self.lastw = {}
        self.readers = {}
        self.block = None
        for e in self.ENG:
            self._sem("e_" + e)

    def _sem(self, name):
        if name not in self.sems:
            self.sems[name] = self.es.enter_context(self.nc.semaphore(name))
            self.cnt[name] = 0
        return name

    def sb(self, name, shape, dt):
        return self.es.enter_context(self.nc.sbuf_tensor(name, list(shape), dt))

    def ps(self, name, shape, dt=F32):
        return self.es.enter_context(self.nc.psum_tensor(name, list(shape), dt))

    def _deps(self, eng, reads, writes):
        need = {}

        def add(tok):
            if tok is not None:
                need[tok[0]] = max(need.get(tok[0], 0), tok[1])

        for k in reads:
            add(self.lastw.get(k))
        for k in writes:
            add(self.lastw.get(k))
            for s, v in self.readers.get(k, {}).items():
                add((s, v))
        own = "e_" + eng
        for s, v in need.items():
            if eng == "tensor" and s == own:
                continue
            if self.seen[eng].get(s, 0) < v:
                self.ops[eng].append(("wait", s, v))
                self.seen[eng][s] = v

    def _commit(self, tok, reads, writes):
        for k in writes:
            self.lastw[k] = tok
            self.readers[k] = {}
        for k in reads:
            r = self.readers.setdefault(k, {})
            r[tok[0]] = max(r.get(tok[0], 0), tok[1])

    def op(self, eng, meth, reads=(), writes=(), **kw):
        self._deps(eng, reads, writes)
        s = "e_" + eng
        self.cnt[s] += 1
        tok = (s, self.cnt[s])
        self.ops[eng].append(("op", meth, kw, s, 1))
        self._commit(tok, reads, writes)

    def dma(self, q, out, in_, reads=(), writes=(), semkey=None, **kw):
        self._deps(q, reads, writes)
        s = self._sem("d_" + (semkey or writes[0]))
        self.cnt[s] += 16
        tok = (s, self.cnt[s])
        kw = dict(kw)
        kw["out"] = out
        kw["in_"] = in_
        self.ops[q].append(("op", "dma_start", kw, s, 16))
        self._commit(tok, reads, writes)

    def pe_drain(self):
        c = self.cnt["e_tensor"]
        if c > 0:
            self.ops["tensor"].append(("wait", "e_tensor", c))

    def barrier(self):
        for eng in self.ENG:
            for s, c in self.cnt.items():
                if c > 0 and self.seen[eng].get(s, 0) < c and not (eng == "tensor" and s == "e_tensor"):
                    self.ops[eng].append(("wait", s, c))
                    self.seen[eng][s] = c

    def flush(self):
        if self.block is None:
            self.block = self.nc.Block()
            self.blk = self.block.__enter__()
        for eng in self.ENG:
            items = self.ops[eng]
            self.ops[eng] = []
            if not items:
                continue

            def body(e, items=items):
                for it in items:
                    if it[0] == "wait":
                        e.wait_ge(self.sems[it[1]], it[2])
                    else:
                        getattr(e, it[1])(**it[2]).then_inc(self.sems[it[3]], it[4])
            getattr(self.blk, eng)(body)

    def end_phase(self):
        self.barrier()
        self.flush()

    def finish(self):
        for s, c in self.cnt.items():
            if s.startswith("d_") and c > 0 and self.seen["sync"].get(s, 0) < c:
                self.ops["sync"].append(("wait", s, c))
                self.seen["sync"][s] = c
        self.flush()
        self.block.__exit__(None, None, None)
        self.es.close()


def build(dbg=(), upto=99):
    nc = bass.Bass("TRN2", target_bir_lowering=False)
    P = Prog(nc)

    def dram(name, shape, dt, kind=None):
        if kind is None:
            kind = "ExternalOutput" if name in dbg else "Internal"
        return nc.dram_tensor(name, list(shape), dt, kind=kind).ap()

    def ext(name, shape):
        return dram(name, shape, F32, kind="ExternalInput")

    x_in = ext("x", [T, D])
    ab_norm = ext("ab_norm", [D])
    ab_w_in = ext("ab_w_in", [D, 2560])
    ab_conv_w = ext("ab_conv_w", [4, 512])
    ab_conv_b = ext("ab_conv_b", [512])
    ab_gate_a_w = ext("ab_gate_a_w", [8, 64, 64])
    ab_gate_a_b = ext("ab_gate_a_b", [512])
    ab_gate_x_w = ext("ab_gate_x_w", [8, 64, 64])
    ab_gate_x_b = ext("ab_gate_x_b", [512])
    ab_lambda = ext("ab_lambda", [512])
    ab_w_out = ext("ab_w_out", [D, D])
    cd_norm = ext("cd_norm", [D])
    cd_w_in = ext("cd_w_in", [D, 2048])
    cd_lam_re = ext("cd_lam_re", [32, 64])
    cd_lam_im = ext("cd_lam_im", [32, 64])
    cd_log_dt = ext("cd_log_dt", [32])
    cd_b_re = ext("cd_b_re", [32, 64, 16])
    cd_b_im = ext("cd_b_im", [32, 64, 16])
    cd_c_re = ext("cd_c_re", [32, 16, 64])
    cd_c_im = ext("cd_c_im", [32, 16, 64])
    cd_d = ext("cd_d", [512])
    cd_glu_w = ext("cd_glu_w", [512, 512])
    cd_glu_b = ext("cd_glu_b", [512])
    cd_w_out = ext("cd_w_out", [D, D])
    ffn_norm = ext("ffn_norm", [2, D])
    ffn_w_gate = ext("ffn_w_gate", [2, D, FH])
    ffn_w_up = ext("ffn_w_up", [2, D, FH])
    ffn_w_down = ext("ffn_w_down", [2, FH, D])
    final_norm = ext("final_norm", [D])
    y_out = dram("y", [T, D], F32, kind="ExternalOutput")

    w_in0_bf = dram("w_in0_bf", [D, 2560], BF16)
    w_out0_bf = dram("w_out0_bf", [D, D], BF16)
    w_in1_bf = dram("w_in1_bf", [D, 2048], BF16)
    w_out1_bf = dram("w_out1_bf", [D, D], BF16)
    wg_bf = [dram(f"wg_bf{l}", [NJ, 128, 8, 128], BF16) for l in range(2)]
    wu_bf = [dram(f"wu_bf{l}", [NJ, 128, 8, 128], BF16) for l in range(2)]
    wd_bf = [dram(f"wd_bf{l}", [FH, D], BF16) for l in range(2)]
    xT = [dram(f"xT{i}", [D, T], F32) for i in range(5)]
    rgin = dram("rgin", [1024, T], F32)
    qk0 = dram("qk0", [1024, T], BF16)
    v0 = dram("v0", [T, 512], BF16)
    ymix0 = dram("ymix0", [1024, T], BF16)
    s5u = dram("s5u", [512, T], F32)
    qk1 = dram("qk1", [1024, T], BF16)
    v1 = dram("v1", [T, 512], BF16)
    ymix1 = dram("ymix1", [1024, T], BF16)
    s5wd = dram("s5wd", [16, 2, 2, 16, 64], BF16)

    ones_bf = P.sb("ones_bf", [128, 128], BF16)
    ones_f = P.sb("ones_f", [128, 128], F32)
    ident_f = P.sb("ident_f", [128, 128], F32)
    P.op("gpsimd", "memset", writes=["ones_bf"], ap=ones_bf[:], constant=1.0)
    P.op("gpsimd", "memset", writes=["ones_f"], ap=ones_f[:], constant=1.0)
    P.op("gpsimd", "affine_select", reads=["ones_f"], writes=["ident_f"],
         out=ident_f[:], in_=ones_f[:], pattern=[[-1, 128]], compare_op=ALU.is_equal, fill=0.0,
         base=0, channel_multiplier=1)
    eps_c = P.sb("eps_c", [128, 1], F32)
    P.op("gpsimd", "memset", writes=["eps_c"], ap=eps_c[:], constant=EPS)
    gains = P.sb("gains", [128, 5, 8], F32)
    for i, g in enumerate([ab_norm, ffn_norm[0], cd_norm, ffn_norm[1], final_norm]):
        P.dma("sync", gains[:, i, :], g.rearrange("(c p) -> p c", p=128), writes=["gains"],
              allow_slow_non_contiguous=True)

    psb = [P.ps(f"psb{i}", [128, 512]) for i in range(8)]

    def cast_copy(dst, src, key, nsplit, axis_rows):
        n = axis_rows // nsplit
        for i in range(nsplit):
            P.dma("gpsimd", dst[i * n:(i + 1) * n], src[i * n:(i + 1) * n], writes=[key])

    cast_copy(w_in0_bf, ab_w_in, "w_in0_bf", 8, D)
    cast_copy(w_out0_bf, ab_w_out, "w_out0_bf", 4, D)

    def cast_ffn(l):
        for j in range(NJ):
            P.dma("gpsimd", wg_bf[l][j], ffn_w_gate[l].rearrange("(c p) n -> p c n", p=128)[:, :, j * 128:(j + 1) * 128],
                  writes=[f"wg_bf{l}"])
            P.dma("gpsimd", wu_bf[l][j], ffn_w_up[l].rearrange("(c p) n -> p c n", p=128)[:, :, j * 128:(j + 1) * 128],
                  writes=[f"wu_bf{l}"])
        cast_copy(wd_bf[l], ffn_w_down[l], f"wd_bf{l}", 11, FH)

    cast_ffn(0)
    cast_copy(w_in1_bf, cd_w_in, "w_in1_bf", 8, D)
    cast_copy(w_out1_bf, cd_w_out, "w_out1_bf", 4, D)
    cast_ffn(1)

    evac_rr = [0]

    def evac(out_ap, in_ap, reads, writes):
        evac_rr[0] ^= 1
        if evac_rr[0]:
            P.op("scalar", "copy", reads=reads, writes=writes, out=out_ap, in_=in_ap)
        else:
            P.op("vector", "tensor_copy", reads=reads, writes=writes, out=out_ap, in_=in_ap)

    def mm(pi, lhsT, rhs, start, stop, reads, out=None):
        P.op("tensor", "matmul", reads=reads, writes=[f"psb{pi}"],
             out=(psb[pi][:] if out is None else out), lhsT=lhsT, rhs=rhs, start=start, stop=stop)

    sq_t = P.sb("sq_t", [128, 8, 512], BF16)
    rstd_t = P.sb("rstd_t", [128, 512], F32)

    def rmsnorm_T(xt, xkey, gi, out_t, okey):
        P.op("scalar", "activation", reads=[xkey], writes=["sq_t"], out=sq_t[:], in_=xt[:], func=AF.Square)
        for c in range(8):
            mm(0, ones_bf[:], sq_t[:, c, :], c == 0, c == 7, ["sq_t", "ones_bf"])
        P.op("scalar", "activation", reads=["psb0", "eps_c"], writes=["rstd_t"],
             out=rstd_t[:], in_=psb[0][:], func=AF.Ln, scale=1.0 / D, bias=eps_c[:])
        P.op("scalar", "activation", reads=["rstd_t"], writes=["rstd_t"],
             out=rstd_t[:], in_=rstd_t[:], func=AF.Exp, scale=-0.5)
        for c in range(8):
            P.op("vector", "scalar_tensor_tensor", reads=[xkey, "rstd_t", "gains"], writes=[okey],
                 out=out_t[:, c, :], in0=xt[:, c, :], scalar=gains[:, gi, c:c + 1], in1=rstd_t[:],
                 op0=ALU.mult, op1=ALU.mult)

    def inproj(layer):
        ncol = 2560 if layer == 0 else 2048
        nf = 8 if layer == 0 else 4
        wsrc = w_in0_bf if layer == 0 else w_in1_bf
        wkey = "w_in0_bf" if layer == 0 else "w_in1_bf"
        f_dst, f_key = (rgin, "rgin") if layer == 0 else (s5u, "s5u")
        qk_dst, qk_key = (qk0, "qk0") if layer == 0 else (qk1, "qk1")
        v_dst, v_key = (v0, "v0") if layer == 0 else (v1, "v1")
        gi = 0 if layer == 0 else 2
        with contextlib.ExitStack() as ph:
            w_in = ph.enter_context(nc.sbuf_tensor(f"w_in_a{layer}", [128, 8, ncol], BF16))
            hw = ncol // 2
            for hh in range(2):
                P.dma("sync", w_in[:, :, hh * hw:(hh + 1) * hw],
                      wsrc.rearrange("(c p) n -> p c n", p=128)[:, :, hh * hw:(hh + 1) * hw],
                      reads=[wkey], writes=["w_in_a"])
            if layer == 0:
                xtok = [ph.enter_context(nc.sbuf_tensor(f"xtok{i}", [128, 4, D], F32)) for i in range(2)]
            xt_a = [ph.enter_context(nc.sbuf_tensor(f"xt_a{layer}{i}", [128, 8, 512], F32)) for i in range(2)]
            h_a = ph.enter_context(nc.sbuf_tensor(f"h_a{layer}", [128, 8, 512], BF16))
            st_rg = [ph.enter_context(nc.sbuf_tensor(f"st_rg{layer}{i}", [128, nf, 512], F32)) for i in range(2)]
            st_qk = [ph.enter_context(nc.sbuf_tensor(f"st_qk{layer}{i}", [128, 8, 512], BF16)) for i in range(2)]
            st_v = [ph.enter_context(nc.sbuf_tensor(f"st_v{layer}{i}", [128, 4, 512], BF16)) for i in range(2)]
            for it in range(NT):
                s = it % 2
                t0 = it * 512
                if layer == 0:
                    P.dma("sync", xtok[s][:], x_in[t0:t0 + 512, :].rearrange("(s p) d -> p s d", p=128),
                          writes=[f"xtok{s}"])
                    for c in range(8):
                        pi = 1 + (c % 2)
                        for sub in range(4):
                            P.op("tensor", "transpose", reads=[f"xtok{s}", "ident_f"], writes=[f"psb{pi}"],
                                 out=psb[pi][:, sub * 128:(sub + 1) * 128],
                                 in_=xtok[s][:, sub, c * 128:(c + 1) * 128], identity=ident_f[:])
                        evac(xt_a[s][:, c, :], psb[pi][:], [f"psb{pi}"], [f"xt_a{s}"])
                    P.dma("sync", xT[0].rearrange("(c p) t -> p c t", p=128)[:, :, t0:t0 + 512], xt_a[s][:],
                          reads=[f"xt_a{s}"], writes=["xT0"])
                else:
                    P.dma("sync", xt_a[s][:], xT[2].rearrange("(c p) t -> p c t", p=128)[:, :, t0:t0 + 512],
                          reads=["xT2"], writes=[f"xt_a{s}"])
                rmsnorm_T(xt_a[s], f"xt_a{s}", gi, h_a, "h_a")
                for m in range(nf + 8):
                    pi = 3 + (m % 5)
                    for k in range(8):
                        mm(pi, w_in[:, k, m * 128:(m + 1) * 128], h_a[:, k, :], k == 0, k == 7, ["w_in_a", "h_a"])
                    if m < nf:
                        evac(st_rg[s][:, m, :], psb[pi][:], [f"psb{pi}"], [f"st_rg{s}"])
                    elif layer == 1 and m < nf + 4:
                        P.op("vector", "tensor_scalar", reads=[f"psb{pi}"], writes=[f"st_qk{s}"],
                             out=st_qk[s][:, m - nf, :], in0=psb[pi][:], scalar1=128.0 ** -0.5, scalar2=None,
                             op0=ALU.mult)
                    else:
                        evac(st_qk[s][:, m - nf, :], psb[pi][:], [f"psb{pi}"], [f"st_qk{s}"])
                for sub in range(4):
                    pi = 3 + (sub % 5)
                    for k in range(8):
                        mm(pi, h_a[:, k, sub * 128:(sub + 1) * 128], w_in[:, k, ncol - 512:ncol], k == 0, k == 7,
                           ["w_in_a", "h_a"])
                    evac(st_v[s][:, sub, :], psb[pi][:], [f"psb{pi}"], [f"st_v{s}"])
                P.dma("sync", f_dst.rearrange("(c p) t -> p c t", p=128)[:, :, t0:t0 + 512], st_rg[s][:],
                      reads=[f"st_rg{s}"], writes=[f_key])
                P.dma("sync", qk_dst.rearrange("(c p) t -> p c t", p=128)[:, :, t0:t0 + 512], st_qk[s][:],
                      reads=[f"st_qk{s}"], writes=[qk_key])
                P.dma("sync", v_dst[t0:t0 + 512, :].rearrange("(s p) d -> p s d", p=128), st_v[s][:],
                      reads=[f"st_v{s}"], writes=[v_key])
            P.end_phase()

    def out_ffn(layer):
        wo_src, wo_key = (w_out0_bf, "w_out0_bf") if layer == 0 else (w_out1_bf, "w_out1_bf")
        ym_src, ym_key = (ymix0, "ymix0") if layer == 0 else (ymix1, "ymix1")
        x_src, x_key = xT[2 * layer], f"xT{2 * layer}"
        x_dst, x_dkey = xT[2 * layer + 2], f"xT{2 * layer + 2}"
        gi = 1 if layer == 0 else 3
        with contextlib.ExitStack() as ph:
            def sbt(name, shape, dt):
                return ph.enter_context(nc.sbuf_tensor(f"{name}_{layer}", list(shape), dt))
            wo = sbt("of_wo", [128, 8, D], BF16)
            wd = sbt("of_wd", [128, NJ, D], BF16)
            P.dma("sync", wo[:], wo_src.rearrange("(c p) n -> p c n", p=128), reads=[wo_key], writes=["of_wo"])
            for i in range(2):
                P.dma("sync", wd[:, i * 11:(i + 1) * 11, :],
                      wd_bf[layer].rearrange("(j p) n -> p j n", p=128)[:, i * 11:(i + 1) * 11, :],
                      reads=[f"wd_bf{layer}"], writes=["of_wd"])
            ym = [sbt(f"of_ym{i}", [128, 8, 512], BF16) for i in range(2)]
            xt = [sbt(f"of_x{i}", [128, 8, 512], F32) for i in range(2)]
            hT = sbt("of_h", [128, 8, 512], BF16)
            act = sbt("of_act", [128, NJ, 512], BF16)
            sg = [sbt(f"of_sg{i}", [128, 512], F32) for i in range(2)]
            wgu = [sbt(f"of_wgu{i}", [128, 2, 8, 128], BF16) for i in range(3)]
            if layer == 1:
                yo = sbt("of_yo", [128, 8, 512], F32)
                ytok = [sbt("of_ytok0", [128, 4, D], F32)] * 2
            nw = 0
            for it in range(NT):
                s = it % 2
                t0 = it * 512
                P.dma("sync", ym[s][:], ym_src.rearrange("(c p) t -> p c t", p=128)[:, :, t0:t0 + 512],
                      reads=[ym_key], writes=[f"of_ym{s}"])
                P.dma("sync", xt[s][:], x_src.rearrange("(c p) t -> p c t", p=128)[:, :, t0:t0 + 512],
                      reads=[x_key], writes=[f"of_x{s}"])
                for m in range(8):
                    pi = 1 + (m % 3)
                    for k in range(8):
                        mm(pi, wo[:, k, m * 128:(m + 1) * 128], ym[s][:, k, :], k == 0, k == 7,
                           ["of_wo", f"of_ym{s}"])
                    P.op("vector", "tensor_tensor", reads=[f"psb{pi}", f"of_x{s}"], writes=[f"of_x{s}"],
                         out=xt[s][:, m, :], in0=psb[pi][:], in1=xt[s][:, m, :], op=ALU.add)
                rmsnorm_T(xt[s], f"of_x{s}", gi, hT, "of_h")
                for j in range(NJ):
                    ws = nw % 3
                    nw += 1
                    P.dma("sync", wgu[ws][:, 0], wg_bf[layer][j], reads=[f"wg_bf{layer}"], writes=[f"of_wgu{ws}"])
                    P.dma("sync", wgu[ws][:, 1], wu_bf[layer][j], reads=[f"wu_bf{layer}"], writes=[f"of_wgu{ws}"])
                    pg, pu = 4 + 2 * (j % 2), 5 + 2 * (j % 2)
                    for k in range(8):
                        mm(pg, wgu[ws][:, 0, k, :], hT[:, k, :], k == 0, k == 7, [f"of_wgu{ws}", "of_h"])
                    for k in range(8):
                        mm(pu, wgu[ws][:, 1, k, :], hT[:, k, :], k == 0, k == 7, [f"of_wgu{ws}", "of_h"])
                    P.op("scalar", "activation", reads=[f"psb{pg}"], writes=[f"of_sg{j % 2}"], out=sg[j % 2][:],
                         in_=psb[pg][:], func=AF.Silu)
                    P.op("vector", "tensor_tensor", reads=[f"of_sg{j % 2}", f"psb{pu}"], writes=["of_act"],
                         out=act[:, j, :], in0=sg[j % 2][:], in1=psb[pu][:], op=ALU.mult)
                for m in range(8):
                    pi = 1 + (m % 3)
                    for j in range(NJ):
                        mm(pi, wd[:, j, m * 128:(m + 1) * 128], act[:, j, :], j == 0, j == NJ - 1,
                           ["of_wd", "of_act"])
                    P.op("vector", "tensor_tensor", reads=[f"psb{pi}", f"of_x{s}"], writes=[f"of_x{s}"],
                         out=xt[s][:, m, :], in0=psb[pi][:], in1=xt[s][:, m, :], op=ALU.add)
                if layer == 0 or "xT4" in dbg:
                    P.dma("sync", x_dst.rearrange("(c p) t -> p c t", p=128)[:, :, t0:t0 + 512], xt[s][:],
                          reads=[f"of_x{s}"], writes=[x_dkey])
                if layer == 1:
                    rmsnorm_T(xt[s], f"of_x{s}", 4, yo, "of_yo")
                    for sub in range(4):
                        for c in range(8):
                            pi = 1 + (sub % 3)
                            P.op("tensor", "transpose", reads=["of_yo", "ident_f"], writes=[f"psb{pi}"],
                                 out=psb[pi][:, (c % 4) * 128:(c % 4 + 1) * 128],
                                 in_=yo[:, c, sub * 128:(sub + 1) * 128], identity=ident_f[:])
                            if c % 4 == 3:
                                evac(ytok[s][:, sub, (c // 4) * 512:(c // 4 + 1) * 512], psb[pi][:], [f"psb{pi}"],
                                     ["of_ytok0"])
                                pi = 1 + ((sub + 1) % 3)
                    P.dma("sync", y_out[t0:t0 + 512, :].rearrange("(s p) d -> p s d", p=128), ytok[s][:],
                          reads=["of_ytok0"], writes=["y"])
            P.end_phase()

    inproj(0)

    if upto <= 1:
        P.finish()
        return nc

    with contextlib.ExitStack() as ph:
        def sbt(name, shape, dt):
            return ph.enter_context(nc.sbuf_tensor(name, list(shape), dt))
        convw = sbt("rg_convw", [128, 4, 4], F32)
        convb = sbt("rg_convb", [128, 4], F32)
        ba = sbt("rg_ba", [128, 4], F32)
        bx = sbt("rg_bx", [128, 4], F32)
        lam = sbt("rg_lam", [128, 4], F32)
        cvec = sbt("rg_cvec", [128, 4], F32)
        cvec2 = sbt("rg_cvec2", [128, 4], F32)
        wst = sbt("rg_wst", [128, 2, 4, 128], F32)
        wbd = sbt("rg_wbd", [128, 2, 4, 128], BF16)
        for j in range(4):
            P.dma("sync", convw[:, j, :], ab_conv_w[j].rearrange("(c p) -> p c", p=128), writes=["rg_convw"],
                  allow_slow_non_contiguous=True)
        for tl, src, key in ((convb, ab_conv_b, "rg_convb"), (ba, ab_gate_a_b, "rg_ba"),
                             (bx, ab_gate_x_b, "rg_bx"), (lam, ab_lambda, "rg_lam")):
            P.dma("sync", tl[:], src.rearrange("(c p) -> p c", p=128), writes=[key],
                  allow_slow_non_contiguous=True)
        P.op("gpsimd", "memset", writes=["rg_wst"], ap=wst[:], constant=0.0)
        for gi_, wsrc in enumerate((ab_gate_a_w, ab_gate_x_w)):
            for hd in range(8):
                cc, hl = hd // 2, hd % 2
                P.dma("sync", wst[hl * 64:(hl + 1) * 64, gi_, cc, hl * 64:(hl + 1) * 64], wsrc[hd],
                      writes=["rg_wst"])
        P.op("vector", "tensor_copy", reads=["rg_wst"], writes=["rg_wbd"], out=wbd[:], in_=wst[:])
        P.op("scalar", "activation", reads=["rg_lam"], writes=["rg_cvec"], out=cvec[:], in_=lam[:],
             func=AF.Exp, scale=-1.0)
        P.op("scalar", "activation", reads=["rg_cvec", "ones_f"], writes=["rg_cvec"], out=cvec[:], in_=cvec[:],
             func=AF.Ln, bias=ones_f[:, 0:1])
        P.op("vector", "tensor_scalar", reads=["rg_cvec"], writes=["rg_cvec2"], out=cvec2[:], in0=cvec[:],
             scalar1=-16.0, scalar2=None, op0=ALU.mult)
        P.op("vector", "tensor_scalar", reads=["rg_cvec"], writes=["rg_cvec"], out=cvec[:], in0=cvec[:],
             scalar1=-8.0, scalar2=None, op0=ALU.mult)
        B = [sbt(f"rgB{i}", [128, T], F32) for i in range(7)]
        xc_bf = sbt("rg_xcbf", [128, T], BF16)
        y_bf = sbt("rg_ybf", [128, T], BF16)
        rg_rows = rgin.rearrange("(c p) t -> c p t", p=128)
        ym_rows = ymix0.rearrange("(c p) t -> c p t", p=128)
        for cc in range(4):
            xr, gt, xc, rr, ii, a2, hh = B
            P.dma("sync", xr[:], rg_rows[cc], reads=["rgin"], writes=["rgB0"])
            P.dma("sync", gt[:], rg_rows[4 + cc], reads=["rgin"], writes=["rgB1"])
            P.op("vector", "tensor_scalar", reads=["rgB0", "rg_convw", "rg_convb"], writes=["rgB2"],
                 out=xc[:], in0=xr[:], scalar1=convw[:, 3, cc:cc + 1], scalar2=convb[:, cc:cc + 1],
                 op0=ALU.mult, op1=ALU.add)
            for j in (2, 1, 0):
                dl = 3 - j
                P.op("vector", "scalar_tensor_tensor", reads=["rgB0", "rgB2", "rg_convw"], writes=["rgB2"],
                     out=xc[:, dl:], in0=xr[:, 0:T - dl], scalar=convw[:, j, cc:cc + 1], in1=xc[:, dl:],
                     op0=ALU.mult, op1=ALU.add)
            P.op("gpsimd", "tensor_copy", reads=["rgB2"], writes=["rg_xcbf"], out=xc_bf[:], in_=xc[:])
            for tt in range(8):
                for gi_, (dst, dkey, bias) in enumerate(((rr, "rgB3", ba), (ii, "rgB4", bx))):
                    pi = (tt * 2 + gi_) % 4
                    mm(pi, wbd[:, gi_, cc, :], xc_bf[:, tt * 512:(tt + 1) * 512], True, True,
                       ["rg_wbd", "rg_xcbf"])
                    P.op("scalar", "activation", reads=[f"psb{pi}", "rg_ba", "rg_bx"], writes=[dkey],
                         out=dst[:, tt * 512:(tt + 1) * 512], in_=psb[pi][:], func=AF.Sigmoid,
                         bias=bias[:, cc:cc + 1])
            P.op("scalar", "activation", reads=["rgB3", "rg_cvec2"], writes=["rgB5"], out=a2[:], in_=rr[:],
                 func=AF.Exp, scale=cvec2[:, cc:cc + 1])
            P.op("scalar", "activation", reads=["rgB3", "rg_cvec"], writes=["rgB3"], out=rr[:], in_=rr[:],
                 func=AF.Exp, scale=cvec[:, cc:cc + 1])
            P.op("vector", "tensor_scalar", reads=["rgB5"], writes=["rgB5"], out=a2[:], in0=a2[:],
                 scalar1=-1.0, scalar2=1.0, op0=ALU.mult, op1=ALU.add)
            P.op("scalar", "activation", reads=["rgB5"], writes=["rgB5"], out=a2[:], in_=a2[:], func=AF.Sqrt)
            P.op("gpsimd", "tensor_tensor", reads=["rgB4", "rgB2"], writes=["rgB4"], out=ii[:], in0=ii[:],
                 in1=xc[:], op=ALU.mult)
            P.op("vector", "tensor_tensor", reads=["rgB4", "rgB5"], writes=["rgB4"], out=ii[:], in0=ii[:],
                 in1=a2[:], op=ALU.mult)
            P.op("vector", "tensor_tensor_scan", reads=["rgB3", "rgB4"], writes=["rgB6"], out=hh[:],
                 data0=rr[:], data1=ii[:], initial=0.0, op0=ALU.mult, op1=ALU.add)
            P.op("gpsimd", "tensor_tensor", reads=["rgB1"], writes=["rgB0"], out=xr[:], in0=gt[:], in1=gt[:],
                 op=ALU.mult)
            P.op("gpsimd", "tensor_scalar", reads=["rgB0"], writes=["rgB0"], out=xr[:], in0=xr[:],
                 scalar1=0.044715, scalar2=1.0, op0=ALU.mult, op1=ALU.add)
            P.op("gpsimd", "tensor_tensor", reads=["rgB0", "rgB1"], writes=["rgB0"], out=xr[:], in0=xr[:],
                 in1=gt[:], op=ALU.mult)
            P.op("scalar", "activation", reads=["rgB0"], writes=["rgB0"], out=xr[:], in_=xr[:],
                 func=AF.Sigmoid, scale=1.5957691216057308)
            P.op("vector", "tensor_tensor", reads=["rgB6", "rgB1"], writes=["rgB6"], out=hh[:], in0=hh[:],
                 in1=gt[:], op=ALU.mult)
            P.op("vector", "tensor_tensor", reads=["rgB6", "rgB0"], writes=["rg_ybf"], out=y_bf[:], in0=hh[:],
                 in1=xr[:], op=ALU.mult)
            P.dma("sync", ym_rows[cc], y_bf[:], reads=["rg_ybf"], writes=["ymix0"])
        P.end_phase()

    if upto <= 2:
        P.finish()
        return nc

    with contextlib.ExitStack() as ph:
        def sbt(name, shape, dt):
            return ph.enter_context(nc.sbuf_tensor(name, list(shape), dt))
        tri = sbt("sb_tri", [128, 128], BF16)
        mstr = sbt("sb_mstr", [128, 128], F32)
        P.op("gpsimd", "affine_select", reads=["ones_bf"], writes=["sb_tri"], out=tri[:], in_=ones_bf[:],
             pattern=[[-1, 128]], compare_op=ALU.is_ge, fill=0.0, base=0, channel_multiplier=1)
        P.op("gpsimd", "affine_select", reads=["ones_f"], writes=["sb_mstr"], out=mstr[:], in_=ones_f[:],
             pattern=[[1, 128]], compare_op=ALU.is_gt, fill=0.0, base=0, channel_multiplier=-1)
        v_all = sbt("sb_v", [128, 32, 512], BF16)
        v_src = v0.rearrange("(n p) d -> p n d", p=128)
        for i in range(4):
            P.dma("sync", v_all[:, i * 8:(i + 1) * 8, :], v_src[:, i * 8:(i + 1) * 8, :], reads=["v0"],
                  writes=["sb_v"])
        qT = [sbt(f"sb_q{i}", [128, T], BF16) for i in range(2)]
        kT = [sbt(f"sb_k{i}", [128, T], BF16) for i in range(2)]
        yst = [sbt(f"sb_y{i}", [128, T], BF16) for i in range(2)]
        for i in range(2):
            P.op("gpsimd", "memset", writes=[f"sb_q{i}"], ap=qT[i][:], constant=0.0)
            P.op("gpsimd", "memset", writes=[f"sb_k{i}"], ap=kT[i][:], constant=0.0)
        e_t = [sbt(f"sb_e{i}", [128, 512], F32) for i in range(2)]
        sp_t = [sbt(f"sb_sp{i}", [128, 512], BF16) for i in range(2)]
        en_t = [sbt(f"sb_en{i}", [128, 512], F32) for i in range(2)]
        w_t = [sbt(f"sb_w{i}", [128, 512], BF16) for i in range(2)]
        lacc_f = sbt("sb_laccf", [128, 512], F32)
        lacc_b = sbt("sb_laccb", [128, 512], BF16)
        qk_rows = qk0.rearrange("(h p) t -> h p t", p=64)
        ym128 = ymix0.rearrange("(h p) t -> h p t", p=128)
        n = 0
        for hd in range(8):
            hs = hd % 2
            P.dma("sync", qT[hs][0:64, :], qk_rows[hd], reads=["qk0"], writes=[f"sb_q{hs}"])
            P.dma("sync", kT[hs][0:64, :], qk_rows[8 + hd], reads=["qk0"], writes=[f"sb_k{hs}"])
            ys = (hd // 2) % 2
            hr = slice((hd % 2) * 64, (hd % 2) * 64 + 64)
            for qi in range(8):
                q0 = qi * 512
                po = 4 + (qi % 2)
                P.op("gpsimd", "memset", writes=["sb_laccf"], ap=lacc_f[:], constant=0.0)
                P.op("gpsimd", "memset", writes=["sb_laccb"], ap=lacc_b[:], constant=0.0)
                kbs = list(range(q0 // 128 + 3, -1, -1))
                for bi, kb in enumerate(kbs):
                    r = n % 2
                    n += 1
                    c0 = max(0, kb * 128 - q0)
                    diag = kb * 128 >= q0
                    cs = slice(c0, 512)
                    dg = slice(c0, c0 + 128)
                    pa, pc = r, 2 + r
                    mm(pa, kT[hs][:, kb * 128:(kb + 1) * 128], qT[hs][:, q0 + c0:q0 + 512], True, True,
                       [f"sb_k{hs}", f"sb_q{hs}"], out=psb[pa][:, cs])
                    P.op("scalar", "activation", reads=[f"psb{pa}"], writes=[f"sb_e{r}"], out=e_t[r][:, cs],
                         in_=psb[pa][:, cs], func=AF.Exp, scale=0.125)
                    P.op("scalar", "activation", reads=[f"sb_e{r}", "ones_f"], writes=[f"sb_sp{r}"],
                         out=sp_t[r][:, cs], in_=e_t[r][:, cs], func=AF.Ln, bias=ones_f[:, 0:1])
                    if diag:
                        P.op("vector", "tensor_tensor", reads=[f"sb_sp{r}", "sb_mstr"], writes=[f"sb_sp{r}"],
                             out=sp_t[r][:, dg], in0=sp_t[r][:, dg], in1=mstr[:], op=ALU.mult)
                    mm(pc, tri[:], sp_t[r][:, cs], True, bi == 0, ["sb_tri", f"sb_sp{r}"], out=psb[pc][:, cs])
                    if bi > 0:
                        mm(pc, ones_bf[:], lacc_b[:, cs], False, True, ["ones_bf", "sb_laccb"], out=psb[pc][:, cs])
                    P.op("scalar", "activation", reads=[f"psb{pc}"], writes=[f"sb_en{r}"], out=en_t[r][:, cs],
                         in_=psb[pc][:, cs], func=AF.Exp, scale=-1.0)
                    P.op("vector", "tensor_tensor", reads=[f"sb_e{r}", f"sb_en{r}"], writes=[f"sb_w{r}"],
                         out=w_t[r][:, cs], in0=e_t[r][:, cs], in1=en_t[r][:, cs], op=ALU.mult)
                    if diag:
                        P.op("vector", "tensor_tensor", reads=[f"sb_w{r}", "sb_mstr"], writes=[f"sb_w{r}"],
                             out=w_t[r][:, dg], in0=w_t[r][:, dg], in1=mstr[:], op=ALU.mult)
                    mm(po, v_all[:, kb, (hd // 2) * 128:(hd // 2) * 128 + 128], w_t[r][:, cs], bi == 0, kb == 0,
                       ["sb_v", f"sb_w{r}"], out=psb[po][:, cs])
                    if kb > 0:
                        P.op("gpsimd", "tensor_tensor", reads=["sb_laccf", f"sb_sp{r}"], writes=["sb_laccf"],
                             out=lacc_f[:, cs], in0=lacc_f[:, cs], in1=sp_t[r][:, cs], op=ALU.add)
                        P.op("gpsimd", "tensor_copy", reads=["sb_laccf"], writes=["sb_laccb"],
                             out=lacc_b[:, cs], in_=lacc_f[:, cs])
                evac(yst[ys][hr, q0:q0 + 512], psb[po][hr, :], [f"psb{po}"], [f"sb_y{ys}"])
            if hd % 2 == 1:
                P.dma("sync", ym128[4 + hd // 2], yst[ys][:], reads=[f"sb_y{ys}"], writes=["ymix0"])
        P.end_phase()

    if upto <= 3:
        P.finish()
        return nc

    out_ffn(0)
    if upto <= 4:
        P.finish()
        return nc

    inproj(1)
    if upto <= 5:
        P.finish()
        return nc

    NEG = -30000.0
    with contextlib.ExitStack() as ph:
        def sbt(name, shape, dt):
            return ph.enter_context(nc.sbuf_tensor(name, list(shape), dt))
        slopes = [2.0 ** (-2.0 * (h + 1)) for h in range(4)]
        io33 = sbt("mb_io33", [128, 33], F32)
        pidx = sbt("mb_pidx", [128, 2], F32)
        biasT = sbt("mb_biasT", [128, 4, 33], F32)
        nsl = sbt("mb_nsl", [128, 4, 2], F32)
        P.op("gpsimd", "iota", writes=["mb_io33"], out=io33[:], pattern=[[-128, 33]], base=128,
             channel_multiplier=1, allow_small_or_imprecise_dtypes=True)
        P.op("gpsimd", "iota", writes=["mb_pidx"], out=pidx[:], pattern=[[128, 2]], base=0,
             channel_multiplier=1, allow_small_or_imprecise_dtypes=True)
        for h in range(4):
            P.op("vector", "tensor_scalar", reads=["mb_io33"], writes=["mb_biasT"], out=biasT[:, h, :],
                 in0=io33[:], scalar1=slopes[h], scalar2=None, op0=ALU.mult)
            P.op("vector", "tensor_scalar", reads=["mb_pidx"], writes=["mb_nsl"], out=nsl[:, h, :],
                 in0=pidx[:], scalar1=-slopes[h], scalar2=None, op0=ALU.mult)
        pastm = sbt("mb_pastm", [128, 32, 32], F32)
        P.op("gpsimd", "memset", writes=["mb_pastm"], ap=pastm[:], constant=0.0)
        pm4 = pastm[:].rearrange("p (b e) n -> p b e n", e=2)[:, :, :, 0:16]
        P.op("gpsimd", "affine_select", reads=["mb_pastm"], writes=["mb_pastm"], out=pm4, in_=pm4,
             pattern=[[1, 16], [0, 2], [-1, 16]], compare_op=ALU.is_gt, fill=NEG, base=0, channel_multiplier=0)
        efull = sbt("mb_efull", [128, 128, 128], BF16)
        P.op("gpsimd", "memset", writes=["mb_efull"], ap=efull[:], constant=1.0)
        P.op("gpsimd", "affine_select", reads=["mb_efull"], writes=["mb_efull"], out=efull[:], in_=efull[:],
             pattern=[[-1, 128], [0, 128]], compare_op=ALU.is_equal, fill=0.0, base=0, channel_multiplier=1)
        mc = sbt("mb_mc", [128, 128], F32)
        P.op("gpsimd", "affine_select", reads=["ones_f"], writes=["mb_mc"], out=mc[:], in_=ones_f[:],
             pattern=[[1, 128]], compare_op=ALU.is_ge, fill=0.0, base=0, channel_multiplier=-1)
        v_all = sbt("mb_v", [128, 32, 512], BF16)
        v_src = v1.rearrange("(n p) d -> p n d", p=128)
        for i in range(4):
            P.dma("sync", v_all[:, i * 8:(i + 1) * 8, :], v_src[:, i * 8:(i + 1) * 8, :], reads=["v1"],
                  writes=["mb_v"])
        qT = [sbt(f"mb_q{i}", [128, T], BF16) for i in range(2)]
        kT = [sbt(f"mb_k{i}", [128, T], BF16) for i in range(2)]
        yst = [sbt(f"mb_y{i}", [128, T], BF16) for i in range(2)]
        km_f = sbt("mb_kmf", [128, 16], F32)
        km_b = sbt("mb_kmb", [128, 16], BF16)
        gm = sbt("mb_gm", [128, 32, 32], F32)
        ng = sbt("mb_ng", [128, 32, 32], F32)
        m8 = sbt("mb_m8", [128, 32, 8], F32)
        rt4 = sbt("mb_rt4", [128, 8, 128], BF16)
        w_t = [sbt(f"mb_w{i}", [128, 256], BF16) for i in range(3)]
        rz = [sbt(f"mb_rz{i}", [128, 256], F32) for i in range(2)]
        P.op("gpsimd", "memset", writes=["mb_gm"], ap=gm[:], constant=0.0)
        P.op("gpsimd", "memset", writes=["mb_ng"], ap=ng[:], constant=0.0)
        qk_rows = qk1.rearrange("(h p) t -> h p t", p=128)
        ym128 = ymix1.rearrange("(h p) t -> h p t", p=128)
        nw = 0
        for hd in range(4):
            hs = hd % 2
            P.dma("sync", qT[hs][:], qk_rows[hd], reads=["qk1"], writes=[f"mb_q{hs}"])
            P.dma("sync", kT[hs][:], qk_rows[4 + hd], reads=["qk1"], writes=[f"mb_k{hs}"])
            P.op("vector", "tensor_reduce", reads=[f"mb_k{hs}"], writes=["mb_kmf"], out=km_f[:],
                 in_=kT[hs][:].rearrange("p (n k) -> p n k", k=256), axis=AX.X, op=ALU.add)
            P.op("vector", "tensor_scalar", reads=["mb_kmf"], writes=["mb_kmb"], out=km_b[:], in0=km_f[:],
                 scalar1=1.0 / 256.0, scalar2=None, op0=ALU.mult)
            for i in range(32):
                mm(6, qT[hs][:, i * 128:(i + 1) * 128], km_b[:], True, True, [f"mb_q{hs}", "mb_kmb"],
                   out=psb[6][:, i * 16:(i + 1) * 16])
            P.op("vector", "tensor_tensor", reads=["psb6", "mb_pastm"], writes=["mb_gm"], out=gm[:, :, 0:16],
                 in0=psb[6][:].rearrange("p (i n) -> p i n", n=16), in1=pastm[:, :, 0:16], op=ALU.add)
            for i in range(32):
                P.op("vector", "max", reads=["mb_gm"], writes=["mb_m8"], out=m8[:, i, :], in_=gm[:, i, 0:16])
            P.op("vector", "tensor_tensor", reads=["mb_gm", "mb_m8"], writes=["mb_ng"], out=ng[:, :, 0:16],
                 in0=gm[:, :, 0:16], in1=m8[:, :, 2:3].to_broadcast([128, 32, 16]), op=ALU.is_ge)
            P.op("vector", "tensor_scalar", reads=["mb_ng"], writes=["mb_ng"], out=ng[:, :, 0:16],
                 in0=ng[:, :, 0:16], scalar1=-1.0, scalar2=-NEG, op0=ALU.add, op1=ALU.mult)
            P.op("vector", "tensor_tensor", reads=["mb_ng", "mb_pastm"], writes=["mb_ng"], out=ng[:, :, 0:16],
                 in0=ng[:, :, 0:16], in1=pastm[:, :, 0:16], op=ALU.add)
            P.op("vector", "memset", writes=["mb_ng"], ap=ng[:, :, 16:17], constant=0.0)
            ng4 = ng[:].rearrange("p (b e) n -> p b e n", e=2)
            for e_ in range(2):
                P.op("vector", "tensor_scalar", reads=["mb_ng", "mb_nsl"], writes=["mb_ng"],
                     out=ng4[:, :, e_, 0:17], in0=ng4[:, :, e_, 0:17], scalar1=nsl[:, hd, e_:e_ + 1],
                     scalar2=None, op0=ALU.add)
            for g in range(8):
                pi = 6 + (g // 4)
                P.op("tensor", "transpose", reads=["mb_ng", "ident_f"], writes=[f"psb{pi}"],
                     out=psb[pi][:, (g % 4) * 128:(g % 4 + 1) * 128],
                     in_=ng[:, 4 * g:4 * g + 4, :].rearrange("p a n -> p (a n)"), identity=ident_f[:])
                if g % 4 == 3:
                    evac(rt4[:, g - 3:g + 1, :].rearrange("p a t -> p (a t)"), psb[pi][:], [f"psb{pi}"], ["mb_rt4"])
            for b in range(16):
                po, pz = 2 + (b % 2), 4 + (b % 2)
                nkt = 2 * b + 2
                for kt in range(nkt):
                    ws = nw % 3
                    ps_ = nw % 2
                    nw += 1
                    own = kt >= 2 * b
                    c0 = 128 if kt == 2 * b + 1 else 0
                    cs = slice(c0, 256)
                    n_row = 16 if own else kt // 2
                    mm(ps_, kT[hs][:, kt * 128:(kt + 1) * 128], qT[hs][:, b * 256 + c0:(b + 1) * 256], True, False,
                       [f"mb_k{hs}", f"mb_q{hs}"], out=psb[ps_][:, cs])
                    for e_ in range(c0 // 128, 2):
                        il = 2 * (b % 2) + e_
                        mm(ps_, efull[:, il * 32 + n_row, :], rt4[:, b // 2, :], False, e_ == 1,
                           ["mb_efull", "mb_rt4"], out=psb[ps_][:, e_ * 128:(e_ + 1) * 128])
                    dd = 2 * b - kt + 1
                    P.op("scalar", "activation", reads=[f"psb{ps_}", "mb_biasT"], writes=[f"mb_w{ws}"],
                         out=w_t[ws][:, cs], in_=psb[ps_][:, cs], func=AF.Exp, bias=biasT[:, hd, dd:dd + 1])
                    if own:
                        P.op("vector", "tensor_tensor", reads=[f"mb_w{ws}", "mb_mc"], writes=[f"mb_w{ws}"],
                             out=w_t[ws][:, c0:c0 + 128], in0=w_t[ws][:, c0:c0 + 128], in1=mc[:], op=ALU.mult)
                    mm(po, v_all[:, kt, hd * 128:(hd + 1) * 128], w_t[ws][:, cs], kt == 0, kt == nkt - 1,
                       ["mb_v", f"mb_w{ws}"], out=psb[po][:, cs])
                    mm(pz, ones_bf[:], w_t[ws][:, cs], kt == 0, kt == nkt - 1, ["ones_bf", f"mb_w{ws}"],
                       out=psb[pz][:, cs])
                P.op("vector", "reciprocal", reads=[f"psb{pz}"], writes=[f"mb_rz{b % 2}"], out=rz[b % 2][:],
                     in_=psb[pz][:, 0:256])
                P.op("vector", "tensor_tensor", reads=[f"psb{po}", f"mb_rz{b % 2}"], writes=[f"mb_y{hs}"],
                     out=yst[hs][:, b * 256:(b + 1) * 256], in0=psb[po][:, 0:256], in1=rz[b % 2][:], op=ALU.mult)
            P.dma("sync", ym128[4 + hd], yst[hs][:], reads=[f"mb_y{hs}"], writes=["ymix1"])
        P.end_phase()

    if upto <= 6:
        P.finish()
        return nc

    INV_2PI = 1.0 / (2.0 * math.pi)
    MAGIC = 12582912.0
    SIN_SCALE = 6.283185
    with contextlib.ExitStack() as ph:
        def sbt(name, shape, dt):
            return ph.enter_context(nc.sbuf_tensor(name, list(shape), dt))

        def vop(eng, meth, reads, writes, **kw):
            P.op(eng, meth, reads=reads, writes=writes, **kw)

        Ts = sbt("s5_Ts", [128, 4, 2, 128], BF16)
        To = sbt("s5_To", [128, 16, 2, 32], BF16)
        phiT = sbt("s5_phiT", [128, 16], F32)
        rhoT = sbt("s5_rhoT", [128, 16], F32)
        negpi = sbt("s5_negpi", [128, 1], F32)
        pst = contextlib.ExitStack()
        sbt_outer = sbt

        def sbt(name, shape, dt):
            return pst.enter_context(nc.sbuf_tensor(name, list(shape), dt))
        lr = sbt("s5_lr", [128, 2, 64], F32)
        li = sbt("s5_li", [128, 2, 64], F32)
        ldt = sbt("s5_ldt", [128, 2], F32)
        bre = sbt("s5_bre", [128, 2, 64, 16], F32)
        bim = sbt("s5_bim", [128, 2, 64, 16], F32)
        cre = sbt("s5_cre", [128, 2, 16, 64], F32)
        cim = sbt("s5_cim", [128, 2, 16, 64], F32)
        for tl, key in ((lr, "s5_lr"), (li, "s5_li"), (ldt, "s5_ldt"), (bre, "s5_bre"), (bim, "s5_bim"),
                        (cre, "s5_cre"), (cim, "s5_cim")):
            P.op("gpsimd", "memset", writes=[key], ap=tl[:], constant=0.0)
        P.dma("sync", lr[0:16], cd_lam_re.rearrange("(a g) n -> a g n", g=2), writes=["s5_lr"])
        P.dma("sync", li[0:16], cd_lam_im.rearrange("(a g) n -> a g n", g=2), writes=["s5_li"])
        P.dma("sync", ldt[0:16], cd_log_dt.rearrange("(a g) -> a g", g=2), writes=["s5_ldt"])
        P.dma("sync", bre[0:16], cd_b_re.rearrange("(a g) n p -> a g n p", g=2), writes=["s5_bre"])
        P.dma("sync", bim[0:16], cd_b_im.rearrange("(a g) n p -> a g n p", g=2), writes=["s5_bim"])
        P.dma("sync", cre[0:16], cd_c_re.rearrange("(a g) p n -> a g p n", g=2), writes=["s5_cre"])
        P.dma("sync", cim[0:16], cd_c_im.rearrange("(a g) p n -> a g p n", g=2), writes=["s5_cim"])
        P.op("gpsimd", "memset", writes=["s5_negpi"], ap=negpi[:], constant=-0.5 * math.pi)
        dtt = sbt("s5_dt", [128, 2], F32)
        vop("scalar", "activation", ["s5_ldt"], ["s5_dt"], out=dtt[:], in_=ldt[:], func=AF.Exp)
        sm = {}
        for nm in ("lrdt", "lidt", "mag", "a1", "sinv", "cosv", "abre", "abim", "den", "t1", "t2", "fre", "fim"):
            sm[nm] = sbt("s5_" + nm, [128, 2, 64], F32)

        def k(nm):
            return "s5_" + nm
        dt_bc = dtt[:].unsqueeze(2).to_broadcast([128, 2, 64])
        vop("vector", "tensor_tensor", [k("lr"), k("dt")], [k("lrdt")], out=sm["lrdt"][:], in0=lr[:], in1=dt_bc, op=ALU.mult)
        vop("vector", "tensor_tensor", [k("li"), k("dt")], [k("lidt")], out=sm["lidt"][:], in0=li[:], in1=dt_bc, op=ALU.mult)
        vop("scalar", "activation", [k("lrdt")], [k("mag")], out=sm["mag"][:], in_=sm["lrdt"][:], func=AF.Exp)
        vop("vector", "tensor_scalar", [k("lidt")], [k("a1")], out=sm["a1"][:], in0=sm["lidt"][:], scalar1=INV_2PI,
            scalar2=MAGIC, op0=ALU.mult, op1=ALU.add)
        vop("vector", "tensor_scalar", [k("a1")], [k("a1")], out=sm["a1"][:], in0=sm["a1"][:], scalar1=-MAGIC,
            scalar2=None, op0=ALU.add)
        vop("vector", "scalar_tensor_tensor", [k("lidt"), k("a1")], [k("a1")], out=sm["a1"][:], in0=sm["lidt"][:],
            scalar=INV_2PI, in1=sm["a1"][:], op0=ALU.mult, op1=ALU.subtract)
        vop("scalar", "activation", [k("a1")], [k("sinv")], out=sm["sinv"][:], in_=sm["a1"][:], func=AF.Sin,
            scale=SIN_SCALE)
        vop("vector", "scalar_tensor_tensor", [k("a1")], [k("a1")], out=sm["a1"][:], in0=sm["a1"][:], scalar=-1.0,
            in1=sm["a1"][:], op0=ALU.mult, op1=ALU.max)
        vop("scalar", "activation", [k("a1"), k("negpi")], [k("cosv")], out=sm["cosv"][:], in_=sm["a1"][:],
            func=AF.Sin, scale=SIN_SCALE, bias=negpi[:])
        vop("vector", "scalar_tensor_tensor", [k("cosv"), k("mag")], [k("abre")], out=sm["abre"][:], in0=sm["cosv"][:],
            scalar=-1.0, in1=sm["mag"][:], op0=ALU.mult, op1=ALU.mult)
        vop("vector", "tensor_tensor", [k("sinv"), k("mag")], [k("abim")], out=sm["abim"][:], in0=sm["sinv"][:],
            in1=sm["mag"][:], op=ALU.mult)
        vop("vector", "tensor_tensor", [k("lr")], [k("den")], out=sm["den"][:], in0=lr[:], in1=lr[:], op=ALU.mult)
        vop("vector", "tensor_tensor", [k("li")], [k("t1")], out=sm["t1"][:], in0=li[:], in1=li[:], op=ALU.mult)
        vop("vector", "tensor_tensor", [k("den"), k("t1")], [k("den")], out=sm["den"][:], in0=sm["den"][:], in1=sm["t1"][:], op=ALU.add)
        vop("vector", "tensor_scalar", [k("den")], [k("den")], out=sm["den"][:], in0=sm["den"][:], scalar1=1e-30,
            scalar2=None, op0=ALU.max)
        vop("vector", "reciprocal", [k("den")], [k("den")], out=sm["den"][:], in_=sm["den"][:])
        vop("vector", "tensor_scalar", [k("abre")], [k("t2")], out=sm["t2"][:], in0=sm["abre"][:], scalar1=-1.0,
            scalar2=None, op0=ALU.add)
        vop("vector", "tensor_tensor", [k("t2"), k("lr")], [k("fre")], out=sm["fre"][:], in0=sm["t2"][:], in1=lr[:], op=ALU.mult)
        vop("vector", "tensor_tensor", [k("abim"), k("li")], [k("t1")], out=sm["t1"][:], in0=sm["abim"][:], in1=li[:], op=ALU.mult)
        vop("vector", "tensor_tensor", [k("fre"), k("t1")], [k("fre")], out=sm["fre"][:], in0=sm["fre"][:], in1=sm["t1"][:], op=ALU.add)
        vop("vector", "tensor_tensor", [k("fre"), k("den")], [k("fre")], out=sm["fre"][:], in0=sm["fre"][:], in1=sm["den"][:], op=ALU.mult)
        vop("vector", "tensor_tensor", [k("abim"), k("lr")], [k("fim")], out=sm["fim"][:], in0=sm["abim"][:], in1=lr[:], op=ALU.mult)
        vop("vector", "tensor_tensor", [k("t2"), k("li")], [k("t1")], out=sm["t1"][:], in0=sm["t2"][:], in1=li[:], op=ALU.mult)
        vop("vector", "tensor_tensor", [k("fim"), k("t1")], [k("fim")], out=sm["fim"][:], in0=sm["fim"][:], in1=sm["t1"][:], op=ALU.subtract)
        vop("vector", "tensor_tensor", [k("fim"), k("den")], [k("fim")], out=sm["fim"][:], in0=sm["fim"][:], in1=sm["den"][:], op=ALU.mult)
        bb = sbt("s5_bb", [128, 2, 2, 16, 64], BF16)
        tb1 = sbt("s5_tb1", [128, 2, 64, 16], F32)
        tb2 = sbt("s5_tb2", [128, 2, 64, 16], F32)
        fre_bc = sm["fre"][:].unsqueeze(3).to_broadcast([128, 2, 64, 16])
        fim_bc = sm["fim"][:].unsqueeze(3).to_broadcast([128, 2, 64, 16])
        vop("vector", "tensor_tensor", [k("bre"), k("fre")], [k("tb1")], out=tb1[:], in0=bre[:], in1=fre_bc, op=ALU.mult)
        vop("vector", "tensor_tensor", [k("bim"), k("fim")], [k("tb2")], out=tb2[:], in0=bim[:], in1=fim_bc, op=ALU.mult)
        vop("vector", "tensor_tensor", [k("tb1"), k("tb2")], [k("bb")], out=bb[:, 0].rearrange("a g p n -> a g n p"), in0=tb1[:], in1=tb2[:], op=ALU.subtract)
        vop("vector", "tensor_tensor", [k("bim"), k("fre")], [k("tb1")], out=tb1[:], in0=bim[:], in1=fre_bc, op=ALU.mult)
        vop("vector", "tensor_tensor", [k("bre"), k("fim")], [k("tb2")], out=tb2[:], in0=bre[:], in1=fim_bc, op=ALU.mult)
        vop("vector", "tensor_tensor", [k("tb1"), k("tb2")], [k("bb")], out=bb[:, 1].rearrange("a g p n -> a g n p"), in0=tb1[:], in1=tb2[:], op=ALU.add)
        P.dma("sync", s5wd, bb[0:16], reads=[k("bb")], writes=["s5wd"])
        P.op("gpsimd", "memset", writes=[k("Ts")], ap=Ts[:], constant=0.0)
        for pair in range(16):
            ct, q = pair // 4, pair % 4
            for gl in range(2):
                P.dma("sync", Ts[32 * q + 16 * gl:32 * q + 16 * gl + 16, ct, :, gl * 64:(gl + 1) * 64],
                      s5wd[pair, :, gl].rearrange("r p n -> p r n"), reads=["s5wd"], writes=[k("Ts")])
        To_raw = sbt("s5_Toraw", [128, 2, 16, 16], F32)
        cst = sbt("s5_cst", [128, 2, 16, 128], F32)
        for ri, ct_, ck in ((0, cre, k("cre")), (1, cim, k("cim"))):
            vop("vector", "tensor_copy", [ck], [k("cst")], out=cst[:, ri].rearrange("a p (g n) -> a p g n", g=2),
                in_=ct_[:].rearrange("a g p n -> a p g n"))
        for ri, ct_, ck in ((0, cst, k("cst")), (1, cst, k("cst"))):
            for p4 in range(4):
                pi = 1 + (p4 % 2)
                for pp in range(4):
                    p_ = p4 * 4 + pp
                    P.op("tensor", "transpose", reads=[ck, "ident_f"], writes=[f"psb{pi}"],
                         out=psb[pi][:, pp * 128:(pp + 1) * 128], in_=ct_[:, ri, p_, :], identity=ident_f[:])
                evac(To_raw[:, ri, p4 * 4:(p4 + 1) * 4, :],
                     psb[pi][:].rearrange("p (a b) -> p a b", b=128)[:, :, 0:16], [f"psb{pi}"], [k("Toraw")])
        hm = sbt("s5_hm", [128, 4], F32)
        P.op("gpsimd", "memset", writes=[k("hm")], ap=hm[:], constant=0.0)
        P.op("gpsimd", "memset", writes=[k("hm")], ap=hm[0:64, 0:1], constant=1.0)
        P.op("gpsimd", "memset", writes=[k("hm")], ap=hm[64:128, 1:2], constant=1.0)
        P.op("gpsimd", "memset", writes=[k("hm")], ap=hm[0:64, 2:3], constant=-1.0)
        P.op("gpsimd", "memset", writes=[k("hm")], ap=hm[64:128, 3:4], constant=-1.0)
        for ri in range(2):
            for glp in range(2):
                vop("vector", "tensor_scalar", [k("Toraw"), k("hm")], [k("To")],
                    out=To[:, :, ri, glp * 16:(glp + 1) * 16],
                    in0=To_raw[:, ri, :, :].rearrange("p a b -> p b a"),
                    scalar1=hm[:, 2 * ri + glp:2 * ri + glp + 1], scalar2=None, op0=ALU.mult)
        P.op("tensor", "transpose", reads=[k("lidt"), "ident_f"], writes=["psb1"], out=psb[1][:, 0:128],
             in_=sm["lidt"][:].rearrange("p a b -> p (a b)"), identity=ident_f[:])
        P.op("tensor", "transpose", reads=[k("lrdt"), "ident_f"], writes=["psb1"], out=psb[1][:, 128:256],
             in_=sm["lrdt"][:].rearrange("p a b -> p (a b)"), identity=ident_f[:])
        vop("vector", "tensor_scalar", ["psb1"], [k("phiT")], out=phiT[:], in0=psb[1][:, 0:16], scalar1=INV_2PI,
            scalar2=None, op0=ALU.mult)
        vop("scalar", "activation", ["psb1"], [k("rhoT")], out=rhoT[:], in_=psb[1][:, 128:144], func=AF.Exp)
        P.end_phase()
        pst.close()
        sbt = sbt_outer
        dvec = sbt("s5_d", [128, 4], F32)
        gb = sbt("s5_gb", [128, 4], F32)
        P.dma("sync", dvec[:], cd_d.rearrange("(c p) -> p c", p=128), writes=[k("d")], allow_slow_non_contiguous=True)
        P.dma("sync", gb[:], cd_glu_b.rearrange("(c p) -> p c", p=128), writes=[k("gb")], allow_slow_non_contiguous=True)
        gw = sbt("s5_gw", [128, 4, 512], BF16)
        P.dma("gpsimd", gw[:], cd_glu_w.rearrange("(c p) n -> p c n", p=128), writes=[k("gw")])
        rowm = sbt("s5_rowm", [128, 4], F32)
        P.op("gpsimd", "memset", writes=[k("rowm")], ap=rowm[:], constant=0.0)
        for q in range(4):
            P.op("gpsimd", "memset", writes=[k("rowm")], ap=rowm[32 * q:32 * q + 32, q:q + 1], constant=1.0)
        HT = 1024
        NH = T // HT
        iot = sbt("s5_iota", [128, NH, HT], F32)
        P.op("gpsimd", "iota", writes=[k("iota")], out=iot[:].rearrange("p a b -> p (a b)"), pattern=[[1, T]], base=0,
             channel_multiplier=0, allow_small_or_imprecise_dtypes=True)
        onesb = sbt("s5_ones", [128, HT], F32)
        P.op("gpsimd", "memset", writes=[k("ones")], ap=onesb[:], constant=1.0)
        u_f = sbt("s5_uf", [128, HT], F32)
        u_b = sbt("s5_ub", [128, HT], BF16)
        um = [sbt(f"s5_um{q}", [128, HT], BF16) for q in range(4)]
        ang = sbt("s5_ang", [128, HT], F32)
        nsn = sbt("s5_ns", [128, HT], F32)
        ncs = sbt("s5_nc", [128, HT], F32)
        rho_t = sbt("s5_rho", [128, HT], F32)
        wre = sbt("s5_wre", [128, HT], F32)
        wim = sbt("s5_wim", [128, HT], F32)
        xre = sbt("s5_xre", [128, HT], F32)
        xim = sbt("s5_xim", [128, HT], F32)
        ta = [sbt(f"s5_ta{i}", [128, 512], F32) for i in range(2)]
        tbb = [sbt(f"s5_tbb{i}", [128, 512], F32) for i in range(2)]
        Xa = [[sbt(f"s5_X{q}{ri}", [128, HT], BF16) for ri in range(2)] for q in range(4)]
        carry = sbt("s5_carry", [128, 16, 2], F32)
        yg = [sbt(f"s5_yg{c}", [128, T], BF16) for c in range(4)]
        g1 = [sbt(f"s5_g1{i}", [128, 512], F32) for i in range(2)]
        g2 = [sbt(f"s5_g2{i}", [128, 512], F32) for i in range(2)]
        u_rows = s5u.rearrange("(c p) t -> c p t", p=128)
        nt_ = 0
        for ct in range(4):
            for hf in range(NH):
                tsl = slice(hf * HT, (hf + 1) * HT)
                P.dma("sync", u_f[:], u_rows[ct][:, tsl], reads=["s5u"], writes=[k("uf")])
                vop("gpsimd", "tensor_copy", [k("uf")], [k("ub")], out=u_b[:], in_=u_f[:])
                for q in range(4):
                    vop("gpsimd", "tensor_scalar", [k("ub"), k("rowm")], [f"s5_um{q}"], out=um[q][:], in0=u_b[:],
                        scalar1=rowm[:, q:q + 1], scalar2=None, op0=ALU.mult)
                for q in range(4):
                    pair = ct * 4 + q
                    vop("vector", "tensor_scalar", [k("iota"), k("phiT")], [k("ang")], out=ang[:], in0=iot[:, hf, :],
                        scalar1=phiT[:, pair:pair + 1], scalar2=MAGIC, op0=ALU.mult, op1=ALU.add)
                    vop("vector", "tensor_scalar", [k("ang")], [k("ang")], out=ang[:], in0=ang[:], scalar1=-MAGIC,
                        scalar2=None, op0=ALU.add)
                    vop("vector", "scalar_tensor_tensor", [k("iota"), k("phiT"), k("ang")], [k("ang")], out=ang[:],
                        in0=iot[:, hf, :], scalar=phiT[:, pair:pair + 1], in1=ang[:], op0=ALU.mult, op1=ALU.subtract)
                    vop("scalar", "activation", [k("ang")], [k("ns")], out=nsn[:], in_=ang[:], func=AF.Sin,
                        scale=-SIN_SCALE)
                    vop("vector", "scalar_tensor_tensor", [k("ang")], [k("ang")], out=ang[:], in0=ang[:], scalar=-1.0,
                        in1=ang[:], op0=ALU.mult, op1=ALU.max)
                    vop("scalar", "activation", [k("ang"), k("negpi")], [k("nc")], out=ncs[:], in_=ang[:], func=AF.Sin,
                        scale=SIN_SCALE, bias=negpi[:])
                    vop("gpsimd", "tensor_scalar", [k("ones"), k("rhoT")], [k("rho")], out=rho_t[:], in0=onesb[:],
                        scalar1=rhoT[:, pair:pair + 1], scalar2=None, op0=ALU.mult)
                    for tt in range(HT // 512):
                        cs = slice(tt * 512, (tt + 1) * 512)
                        r = nt_ % 2
                        nt_ += 1
                        pr, pm = 2 * r, 2 * r + 1
                        mm(pr, Ts[:, ct, 0, :], um[q][:, cs], True, True, [k("Ts"), f"s5_um{q}"])
                        mm(pm, Ts[:, ct, 1, :], um[q][:, cs], True, True, [k("Ts"), f"s5_um{q}"])
                        vop("vector", "tensor_tensor", [f"psb{pr}", k("nc")], [f"s5_ta{r}"], out=ta[r][:], in0=psb[pr][:],
                            in1=ncs[:, cs], op=ALU.mult)
                        vop("vector", "tensor_tensor", [f"psb{pm}", k("ns")], [f"s5_tbb{r}"], out=tbb[r][:], in0=psb[pm][:],
                            in1=nsn[:, cs], op=ALU.mult)
                        vop("gpsimd", "tensor_tensor", [f"s5_ta{r}", f"s5_tbb{r}"], [k("wre")], out=wre[:, cs], in0=ta[r][:],
                            in1=tbb[r][:], op=ALU.add)
                        vop("vector", "tensor_tensor", [f"psb{pm}", k("nc")], [f"s5_ta{r}"], out=ta[r][:], in0=psb[pm][:],
                            in1=ncs[:, cs], op=ALU.mult)
                        vop("vector", "tensor_tensor", [f"psb{pr}", k("ns")], [f"s5_tbb{r}"], out=tbb[r][:], in0=psb[pr][:],
                            in1=nsn[:, cs], op=ALU.mult)
                        vop("gpsimd", "tensor_tensor", [f"s5_ta{r}", f"s5_tbb{r}"], [k("wim")], out=wim[:, cs], in0=ta[r][:],
                            in1=tbb[r][:], op=ALU.subtract)
                    ini_re = 0.0 if hf == 0 else carry[:, pair, 0:1]
                    ini_im = 0.0 if hf == 0 else carry[:, pair, 1:2]
                    vop("vector", "tensor_tensor_scan", [k("rho"), k("wre"), k("carry")], [k("xre")], out=xre[:],
                        data0=rho_t[:], data1=wre[:], initial=ini_re, op0=ALU.mult, op1=ALU.add)
                    vop("vector", "tensor_tensor_scan", [k("rho"), k("wim"), k("carry")], [k("xim")], out=xim[:],
                        data0=rho_t[:], data1=wim[:], initial=ini_im, op0=ALU.mult, op1=ALU.add)
                    if hf < NH - 1:
                        vop("gpsimd", "tensor_copy", [k("xre")], [k("carry")], out=carry[:, pair, 0:1], in_=xre[:, HT - 1:HT])
                        vop("gpsimd", "tensor_copy", [k("xim")], [k("carry")], out=carry[:, pair, 1:2], in_=xim[:, HT - 1:HT])
                    vop("gpsimd", "tensor_tensor", [k("xre"), k("nc")], [k("wre")], out=wre[:], in0=xre[:], in1=ncs[:], op=ALU.mult)
                    vop("vector", "tensor_tensor", [k("xim"), k("ns")], [k("wim")], out=wim[:], in0=xim[:], in1=nsn[:], op=ALU.mult)
                    vop("gpsimd", "tensor_tensor", [k("wre"), k("wim")], [f"s5_X{q}0"], out=Xa[q][0][:], in0=wre[:], in1=wim[:],
                        op=ALU.subtract)
                    vop("vector", "tensor_tensor", [k("xre"), k("ns")], [k("wre")], out=wre[:], in0=xre[:], in1=nsn[:], op=ALU.mult)
                    vop("gpsimd", "tensor_tensor", [k("xim"), k("nc")], [k("wim")], out=wim[:], in0=xim[:], in1=ncs[:], op=ALU.mult)
                    vop("vector", "tensor_tensor", [k("wre"), k("wim")], [f"s5_X{q}1"], out=Xa[q][1][:], in0=wre[:], in1=wim[:],
                        op=ALU.add)
                P.pe_drain()
                for tt in range(HT // 512):
                    cs = slice(tt * 512, (tt + 1) * 512)
                    py = 4 + (tt % 2)
                    for q in range(4):
                        for ri in range(2):
                            P.op("tensor", "matmul", reads=[k("To"), f"s5_X{q}{ri}"], writes=[f"psb{py}"],
                                 out=psb[py][32 * q:32 * q + 32, :], lhsT=To[:, ct * 4 + q, ri, :], rhs=Xa[q][ri][:, cs],
                                 start=(ri == 0), stop=(ri == 1), tile_position=(0, 32 * q))
                    r = tt % 2
                    vop("vector", "scalar_tensor_tensor", [k("uf"), k("d"), f"psb{py}"], [f"s5_g1{r}"], out=g1[r][:],
                        in0=u_f[:, cs], scalar=dvec[:, ct:ct + 1], in1=psb[py][:], op0=ALU.mult, op1=ALU.add)
                    vop("gpsimd", "tensor_tensor", [f"s5_g1{r}"], [f"s5_g2{r}"], out=g2[r][:], in0=g1[r][:], in1=g1[r][:], op=ALU.mult)
                    vop("gpsimd", "tensor_scalar", [f"s5_g2{r}"], [f"s5_g2{r}"], out=g2[r][:], in0=g2[r][:], scalar1=0.044715,
                        scalar2=1.0, op0=ALU.mult, op1=ALU.add)
                    vop("gpsimd", "tensor_tensor", [f"s5_g2{r}", f"s5_g1{r}"], [f"s5_g2{r}"], out=g2[r][:], in0=g2[r][:],
                        in1=g1[r][:], op=ALU.mult)
                    vop("scalar", "activation", [f"s5_g2{r}"], [f"s5_g2{r}"], out=g2[r][:], in_=g2[r][:], func=AF.Sigmoid,
                        scale=1.5957691216057308)
                    vop("vector", "tensor_tensor", [f"s5_g1{r}", f"s5_g2{r}"], [f"s5_yg{ct}"],
                        out=yg[ct][:, hf * HT + tt * 512:hf * HT + (tt + 1) * 512], in0=g1[r][:], in1=g2[r][:], op=ALU.mult)
                P.pe_drain()
        yo_s = [sbt(f"s5_yo{i}", [128, 4, 512], BF16) for i in range(2)]
        for tt in range(8):
            cs = slice(tt * 512, (tt + 1) * 512)
            s_ = tt % 2
            for oc in range(4):
                pi = oc
                for ct in range(4):
                    mm(pi, gw[:, ct, oc * 128:(oc + 1) * 128], yg[ct][:, cs], ct == 0, ct == 3, [k("gw"), f"s5_yg{ct}"])
                r = oc % 2
                vop("scalar", "activation", [f"psb{pi}", k("gb")], [f"s5_g1{r}"], out=g1[r][:], in_=psb[pi][:], func=AF.Sigmoid,
                    bias=gb[:, oc:oc + 1])
                vop("vector", "tensor_tensor", [f"s5_g1{r}", f"s5_yg{oc}"], [f"s5_yo{s_}"], out=yo_s[s_][:, oc, :], in0=g1[r][:],
                    in1=yg[oc][:, cs], op=ALU.mult)
            P.dma("sync", ymix1.rearrange("(c p) t -> p c t", p=128)[:, 0:4, cs], yo_s[s_][:], reads=[f"s5_yo{s_}"],
                  writes=["ymix1"])
        P.end_phase()

    if upto <= 7:
        P.finish()
        return nc

    out_ffn(1)
    P.finish()
    return nc


INPUT_ORDER = ["x", "ab_norm", "ab_w_in", "ab_conv_w", "ab_conv_b", "ab_gate_a_w", "ab_gate_a_b",
               "ab_gate_x_w", "ab_gate_x_b", "ab_lambda", "ab_w_out", "cd_norm", "cd_w_in",
               "cd_lam_re", "cd_lam_im", "cd_log_dt", "cd_b_re", "cd_b_im", "cd_c_re", "cd_c_im",
               "cd_d", "cd_glu_w", "cd_glu_b", "cd_w_out", "ffn_norm", "ffn_w_gate", "ffn_w_up",
               "ffn_w_down", "final_norm"]


def make_in_maps(inputs, cores):
    f = lambda a: np.ascontiguousarray(np.asarray(a, dtype=np.float32))
    shared = {
        "ab_norm": f(inputs["ab_norm"][0]), "ab_w_in": f(inputs["ab_w_in"][0]),
        "ab_conv_w": f(inputs["ab_conv_w"][0, :, 0, :]), "ab_conv_b": f(inputs["ab_conv_b"][0]),
        "ab_gate_a_w": f(inputs["ab_gate_a_w"][0]), "ab_gate_a_b": f(inputs["ab_gate_a_b"][0].reshape(512)),
        "ab_gate_x_w": f(inputs["ab_gate_x_w"][0]), "ab_gate_x_b": f(inputs["ab_gate_x_b"][0].reshape(512)),
        "ab_lambda": f(inputs["ab_lambda"][0]), "ab_w_out": f(inputs["ab_w_out"][0]),
        "cd_norm": f(inputs["cd_norm"][0]), "cd_w_in": f(inputs["cd_w_in"][0]),
        "cd_lam_re": f(inputs["cd_lam_re"][0]), "cd_lam_im": f(inputs["cd_lam_im"][0]),
        "cd_log_dt": f(inputs["cd_log_dt"][0]), "cd_b_re": f(inputs["cd_b_re"][0]),
        "cd_b_im": f(inputs["cd_b_im"][0]), "cd_c_re": f(inputs["cd_c_re"][0]),
        "cd_c_im": f(inputs["cd_c_im"][0]), "cd_d": f(inputs["cd_d"][0]),
        "cd_glu_w": f(inputs["cd_glu_w"][0]), "cd_glu_b": f(inputs["cd_glu_b"][0]),
        "cd_w_out": f(inputs["cd_w_out"][0]), "ffn_norm": f(inputs["ffn_norm"]),
        "ffn_w_gate": f(inputs["ffn_w_gate"]), "ffn_w_up": f(inputs["ffn_w_up"]),
        "ffn_w_down": f(inputs["ffn_w_down"]), "final_norm": f(inputs["final_norm"]),
    }
    maps = []
    for b in cores:
        m = dict(shared)
        m["x"] = f(inputs["x"][b])
        maps.append(m)
    return maps


def kernel(**inputs):
    nc = build()
    in_maps = make_in_maps(inputs, list(range(8)))
    res = run_bass_kernel_spmd(nc, in_maps, core_ids=list(range(8)))
    return np.stack([np.asarray(r["y"]) for r in res.results], axis=0).astype(np.float32)
```

```python
import contextlib
import math
import numpy as np
import concourse.bass as bass
import concourse.mybir as mybir
from concourse.bass_utils import run_bass_kernel_spmd

F32 = mybir.dt.float32
BF16 = mybir.dt.bfloat16
AF = mybir.ActivationFunctionType
ALU = mybir.AluOpType
AX = mybir.AxisListType

T = 4096
D = 1024
NT = T // 512
FH = 2816
NJ = FH // 128
EPS = 1e-6


class Prog:
    ENG = ("tensor", "vector", "scalar", "gpsimd", "sync")

    def __init__(self, nc):
        self.nc = nc
        self.es = contextlib.ExitStack()
        self.ops = {e: [] for e in self.ENG}
        self.sems = {}
        self.cnt = {}
        self.seen = {e: {} for e in self.ENG}
        self.lastw = {}
        self.readers = {}
        self.block = None
        for e in self.ENG:
            self._sem("e_" + e)

    def _sem(self, name):
        if name not in self.sems:
            self.sems[name] = self.es.enter_context(self.nc.semaphore(name))
            self.cnt[name] = 0
        return name

    def sb(self, name, shape, dt):
        return self.es.enter_context(self.nc.sbuf_tensor(name, list(shape), dt))

    def ps(self, name, shape, dt=F32):
        return self.es.enter_context(self.nc.psum_tensor(name, list(shape), dt))

    def _deps(self, eng, reads, writes):
        need = {}

        def add(tok):
            if tok is not None:
                need[tok[0]] = max(need.get(tok[0], 0), tok[1])

        for k in reads:
            add(self.lastw.get(k))
        for k in writes:
            add(self.lastw.get(k))
            for s, v in self.readers.get(k, {}).items():
                add((s, v))
        own = "e_" + eng
        for s, v in need.items():
            if eng == "tensor" and s == own:
                continue
            if self.seen[eng].get(s, 0) < v:
                self.ops[eng].append(("wait", s, v))
                self.seen[eng][s] = v

    def _commit(self, tok, reads, writes):
        for k in writes:
            self.lastw[k] = tok
            self.readers[k] = {}
        for k in reads:
            r = self.readers.setdefault(k, {})
            r[tok[0]] = max(r.get(tok[0], 0), tok[1])

    def op(self, eng, meth, reads=(), writes=(), **kw):
        self._deps(eng, reads, writes)
        s = "e_" + eng
        self.cnt[s] += 1
        tok = (s, self.cnt[s])
        self.ops[eng].append(("op", meth, kw, s, 1))
        self._commit(tok, reads, writes)

    def dma(self, q, out, in_, reads=(), writes=(), semkey=None, **kw):
        self._deps(q, reads, writes)
        s = self._sem("d_" + (semkey or writes[0]))
        self.cnt[s] += 16
        tok = (s, self.cnt[s])
        kw = dict(kw)
        kw["out"] = out
        kw["in_"] = in_
        self.ops[q].append(("op", "dma_start", kw, s, 16))
        self._commit(tok, reads, writes)

    def pe_drain(self):
        c = self.cnt["e_tensor"]
        if c > 0:
            self.ops["tensor"].append(("wait", "e_tensor", c))

    def barrier(self):
        for eng in self.ENG:
            for s, c in self.cnt.items():
                if c > 0 and self.seen[eng].get(s, 0) < c and not (eng == "tensor" and s == "e_tensor"):
                    self.ops[eng].append(("wait", s, c))
                    self.seen[eng][s] = c

    def flush(self):
        if self.block is None:
            self.block = self.nc.Block()
            self.blk = self.block.__enter__()
        for eng in self.ENG:
            items = self.ops[eng]
            self.ops[eng] = []
            if not items:
                continue

            def body(e, items=items):
                for it in items:
                    if it[0] == "wait":
                        e.wait_ge(self.sems[it[1]], it[2])
                    else:
                        getattr(e, it[1])(**it[2]).then_inc(self.sems[it[3]], it[4])
            getattr(self.blk, eng)(body)

    def end_phase(self):
        self.barrier()
        self.flush()

    def finish(self):
        for s, c in self.cnt.items():
            if s.startswith("d_") and c > 0 and self.seen["sync"].get(s, 0) < c:
                self.ops["sync"].append(("wait", s, c))
                self.seen["sync"][s] = c
        self.flush()
        self.block.__exit__(None, None, None)
        self.es.close()


def build(dbg=(), upto=99):
    nc = bass.Bass("TRN2", target_bir_lowering=False)
    P = Prog(nc)

    def dram(name, shape, dt, kind=None):
        if kind is None:
            kind = "ExternalOutput" if name in dbg else "Internal"
        return nc.dram_tensor(name, list(shape), dt, kind=kind).ap()

    def ext(name, shape):
        return dram(name, shape, F32, kind="ExternalInput")

    x_in = ext("x", [T, D])
    ab_norm = ext("ab_norm", [D])
    ab_w_in = ext("ab_w_in", [D, 2560])
    ab_conv_w = ext("ab_conv_w", [4, 512])
    ab_conv_b = ext("ab_conv_b", [512])
    ab_gate_a_w = ext("ab_gate_a_w", [8, 64, 64])
    ab_gate_a_b = ext("ab_gate_a_b", [512])
    ab_gate_x_w = ext("ab_gate_x_w", [8, 64, 64])
    ab_gate_x_b = ext("ab_gate_x_b", [512])
    ab_lambda = ext("ab_lambda", [512])
    ab_w_out = ext("ab_w_out", [D, D])
    cd_norm = ext("cd_norm", [D])
    cd_w_in = ext("cd_w_in", [D, 2048])
    cd_lam_re = ext("cd_lam_re", [32, 64])
    cd_lam_im = ext("cd_lam_im", [32, 64])
    cd_log_dt = ext("cd_log_dt", [32])
    cd_b_re = ext("cd_b_re", [32, 64, 16])
    cd_b_im = ext("cd_b_im", [32, 64, 16])
    cd_c_re = ext("cd_c_re", [32, 16, 64])
    cd_c_im = ext("cd_c_im", [32, 16, 64])
    cd_d = ext("cd_d", [512])
    cd_glu_w = ext("cd_glu_w", [512, 512])
    cd_glu_b = ext("cd_glu_b", [512])
    cd_w_out = ext("cd_w_out", [D, D])
    ffn_norm = ext("ffn_norm", [2, D])
    ffn_w_gate = ext("ffn_w_gate", [2, D, FH])
    ffn_w_up = ext("ffn_w_up", [2, D, FH])
    ffn_w_down = ext("ffn_w_down", [2, FH, D])
    final_norm = ext("final_norm", [D])
    y_out = dram("y", [T, D], F32, kind="ExternalOutput")

    w_in0_bf = dram("w_in0_bf", [D, 2560], BF16)
    w_out0_bf = dram("w_out0_bf", [D, D], BF16)
    w_in1_bf = dram("w_in1_bf", [D, 2048], BF16)
    w_out1_bf = dram("w_out1_bf", [D, D], BF16)
    wg_bf = [dram(f"wg_bf{l}", [NJ, 128, 8, 128], BF16) for l in range(2)]
    wu_bf = [dram(f"wu_bf{l}", [NJ, 128, 8, 128], BF16) for l in range(2)]
    wd_bf = [dram(f"wd_bf{l}", [FH, D], BF16) for l in range(2)]
    xT = [dram(f"xT{i}", [D, T], F32) for i in range(5)]
    rgin = dram("rgin", [1024, T], F32)
    qk0 = dram("qk0", [1024, T], BF16)
    v0 = dram("v0", [T, 512], BF16)
    ymix0 = dram("ymix0", [1024, T], BF16)
    s5u = dram("s5u", [512, T], F32)
    qk1 = dram("qk1", [1024, T], BF16)
    v1 = dram("v1", [T, 512], BF16)
    ymix1 = dram("ymix1", [1024, T], BF16)
    s5wd = dram("s5wd", [16, 8, 2, 16, 128], BF16)

    ones_bf = P.sb("ones_bf", [128, 128], BF16)
    ones_f = P.sb("ones_f", [128, 128], F32)
    ident_f = P.sb("ident_f", [128, 128], F32)
    P.op("gpsimd", "memset", writes=["ones_bf"], ap=ones_bf[:], constant=1.0)
    P.op("gpsimd", "memset", writes=["ones_f"], ap=ones_f[:], constant=1.0)
    P.op("gpsimd", "affine_select", reads=["ones_f"], writes=["ident_f"],
         out=ident_f[:], in_=ones_f[:], pattern=[[-1, 128]], compare_op=ALU.is_equal, fill=0.0,
         base=0, channel_multiplier=1)
    eps_c = P.sb("eps_c", [128, 1], F32)
    P.op("gpsimd", "memset", writes=["eps_c"], ap=eps_c[:], constant=EPS)
    gains = P.sb("gains", [128, 5, 8], F32)
    for i, g in enumerate([ab_norm, ffn_norm[0], cd_norm, ffn_norm[1], final_norm]):
        P.dma("sync", gains[:, i, :], g.rearrange("(c p) -> p c", p=128), writes=["gains"],
              allow_slow_non_contiguous=True)

    psb = [P.ps(f"psb{i}", [128, 512]) for i in range(8)]

    def cast_copy(dst, src, key, nsplit, axis_rows):
        n = axis_rows // nsplit
        for i in range(nsplit):
            P.dma("gpsimd", dst[i * n:(i + 1) * n], src[i * n:(i + 1) * n], writes=[key])

    cast_copy(w_in0_bf, ab_w_in, "w_in0_bf", 8, D)
    cast_copy(w_out0_bf, ab_w_out, "w_out0_bf", 4, D)

    def cast_ffn(l):
        for j in range(NJ):
            P.dma("gpsimd", wg_bf[l][j], ffn_w_gate[l].rearrange("(c p) n -> p c n", p=128)[:, :, j * 128:(j + 1) * 128],
                  writes=[f"wg_bf{l}"])
            P.dma("gpsimd", wu_bf[l][j], ffn_w_up[l].rearrange("(c p) n -> p c n", p=128)[:, :, j * 128:(j + 1) * 128],
                  writes=[f"wu_bf{l}"])
        cast_copy(wd_bf[l], ffn_w_down[l], f"wd_bf{l}", 11, FH)

    cast_ffn(0)
    cast_copy(w_in1_bf, cd_w_in, "w_in1_bf", 8, D)
    cast_copy(w_out1_bf, cd_w_out, "w_out1_bf", 4, D)
    cast_ffn(1)

    evac_rr = [0]

    def evac(out_ap, in_ap, reads, writes):
        evac_rr[0] ^= 1
        if evac_rr[0]:
            P.op("scalar", "copy", reads=reads, writes=writes, out=out_ap, in_=in_ap)
        else:
            P.op("vector", "tensor_copy", reads=reads, writes=writes, out=out_ap, in_=in_ap)

    def mm(pi, lhsT, rhs, start, stop, reads, out=None):
        P.op("tensor", "matmul", reads=reads, writes=[f"psb{pi}"],
             out=(psb[pi][:] if out is None else out), lhsT=lhsT, rhs=rhs, start=start, stop=stop)

    sq_t = P.sb("sq_t", [128, 8, 512], BF16)
    rstd_t = P.sb("rstd_t", [128, 512], F32)

    def rmsnorm_T(xt, xkey, gi, out_t, okey):
        P.op("scalar", "activation", reads=[xkey], writes=["sq_t"], out=sq_t[:], in_=xt[:], func=AF.Square)
        for c in range(8):
            mm(0, ones_bf[:], sq_t[:, c, :], c == 0, c == 7, ["sq_t", "ones_bf"])
        P.op("scalar", "activation", reads=["psb0", "eps_c"], writes=["rstd_t"],
             out=rstd_t[:], in_=psb[0][:], func=AF.Ln, scale=1.0 / D, bias=eps_c[:])
        P.op("scalar", "activation", reads=["rstd_t"], writes=["rstd_t"],
             out=rstd_t[:], in_=rstd_t[:], func=AF.Exp, scale=-0.5)
        for c in range(8):
            P.op("vector", "scalar_tensor_tensor", reads=[xkey, "rstd_t", "gains"], writes=[okey],
                 out=out_t[:, c, :], in0=xt[:, c, :], scalar=gains[:, gi, c:c + 1], in1=rstd_t[:],
                 op0=ALU.mult, op1=ALU.mult)

    def inproj(layer):
        ncol = 2560 if layer == 0 else 2048
        nf = 8 if layer == 0 else 4
        wsrc = w_in0_bf if layer == 0 else w_in1_bf
        wkey = "w_in0_bf" if layer == 0 else "w_in1_bf"
        f_dst, f_key = (rgin, "rgin") if layer == 0 else (s5u, "s5u")
        qk_dst, qk_key = (qk0, "qk0") if layer == 0 else (qk1, "qk1")
        v_dst, v_key = (v0, "v0") if layer == 0 else (v1, "v1")
        gi = 0 if layer == 0 else 2
        with contextlib.ExitStack() as ph:
            w_in = ph.enter_context(nc.sbuf_tensor(f"w_in_a{layer}", [128, 8, ncol], BF16))
            hw = ncol // 2
            for hh in range(2):
                P.dma("sync", w_in[:, :, hh * hw:(hh + 1) * hw],
                      wsrc.rearrange("(c p) n -> p c n", p=128)[:, :, hh * hw:(hh + 1) * hw],
                      reads=[wkey], writes=["w_in_a"])
            if layer == 0:
                xtok = [ph.enter_context(nc.sbuf_tensor(f"xtok{i}", [128, 4, D], F32)) for i in range(2)]
            xt_a = [ph.enter_context(nc.sbuf_tensor(f"xt_a{layer}{i}", [128, 8, 512], F32)) for i in range(2)]
            h_a = ph.enter_context(nc.sbuf_tensor(f"h_a{layer}", [128, 8, 512], BF16))
            st_rg = [ph.enter_context(nc.sbuf_tensor(f"st_rg{layer}{i}", [128, nf, 512], F32)) for i in range(2)]
            st_qk = [ph.enter_context(nc.sbuf_tensor(f"st_qk{layer}{i}", [128, 8, 512], BF16)) for i in range(2)]
            st_v = [ph.enter_context(nc.sbuf_tensor(f"st_v{layer}{i}", [128, 4, 512], BF16)) for i in range(2)]
            for it in range(NT):
                s = it % 2
                t0 = it * 512
                if layer == 0:
                    P.dma("sync", xtok[s][:], x_in[t0:t0 + 512, :].rearrange("(s p) d -> p s d", p=128),
                          writes=[f"xtok{s}"])
                    for c in range(8):
                        pi = 1 + (c % 2)
                        for sub in range(4):
                            P.op("tensor", "transpose", reads=[f"xtok{s}", "ident_f"], writes=[f"psb{pi}"],
                                 out=psb[pi][:, sub * 128:(sub + 1) * 128],
                                 in_=xtok[s][:, sub, c * 128:(c + 1) * 128], identity=ident_f[:])
                        evac(xt_a[s][:, c, :], psb[pi][:], [f"psb{pi}"], [f"xt_a{s}"])
                    P.dma("sync", xT[0].rearrange("(c p) t -> p c t", p=128)[:, :, t0:t0 + 512], xt_a[s][:],
                          reads=[f"xt_a{s}"], writes=["xT0"])
                else:
                    P.dma("sync", xt_a[s][:], xT[2].rearrange("(c p) t -> p c t", p=128)[:, :, t0:t0 + 512],
                          reads=["xT2"], writes=[f"xt_a{s}"])
                rmsnorm_T(xt_a[s], f"xt_a{s}", gi, h_a, "h_a")
                for m in range(nf + 8):
                    pi = 3 + (m % 5)
                    for k in range(8):
                        mm(pi, w_in[:, k, m * 128:(m + 1) * 128], h_a[:, k, :], k == 0, k == 7, ["w_in_a", "h_a"])
                    if m < nf:
                        evac(st_rg[s][:, m, :], psb[pi][:], [f"psb{pi}"], [f"st_rg{s}"])
                    elif layer == 1 and m < nf + 4:
                        P.op("vector", "tensor_scalar", reads=[f"psb{pi}"], writes=[f"st_qk{s}"],
                             out=st_qk[s][:, m - nf, :], in0=psb[pi][:], scalar1=128.0 ** -0.5, scalar2=None,
                             op0=ALU.mult)
                    else:
                        evac(st_qk[s][:, m - nf, :], psb[pi][:], [f"psb{pi}"], [f"st_qk{s}"])
                for sub in range(4):
                    pi = 3 + (sub % 5)
                    for k in range(8):
                        mm(pi, h_a[:, k, sub * 128:(sub + 1) * 128], w_in[:, k, ncol - 512:ncol], k == 0, k == 7,
                           ["w_in_a", "h_a"])
                    evac(st_v[s][:, sub, :], psb[pi][:], [f"psb{pi}"], [f"st_v{s}"])
                P.dma("sync", f_dst.rearrange("(c p) t -> p c t", p=128)[:, :, t0:t0 + 512], st_rg[s][:],
                      reads=[f"st_rg{s}"], writes=[f_key])
                P.dma("sync", qk_dst.rearrange("(c p) t -> p c t", p=128)[:, :, t0:t0 + 512], st_qk[s][:],
                      reads=[f"st_qk{s}"], writes=[qk_key])
                P.dma("sync", v_dst[t0:t0 + 512, :].rearrange("(s p) d -> p s d", p=128), st_v[s][:],
                      reads=[f"st_v{s}"], writes=[v_key])
            P.end_phase()

    def out_ffn(layer):
        wo_src, wo_key = (w_out0_bf, "w_out0_bf") if layer == 0 else (w_out1_bf, "w_out1_bf")
        ym_src, ym_key = (ymix0, "ymix0") if layer == 0 else (ymix1, "ymix1")
        x_src, x_key = xT[2 * layer], f"xT{2 * layer}"
        x_dst, x_dkey = xT[2 * layer + 2], f"xT{2 * layer + 2}"
        gi = 1 if layer == 0 else 3
        with contextlib.ExitStack() as ph:
            def sbt(name, shape, dt):
                return ph.enter_context(nc.sbuf_tensor(f"{name}_{layer}", list(shape), dt))
            wo = sbt("of_wo", [128, 8, D], BF16)
            wd = sbt("of_wd", [128, NJ, D], BF16)
            P.dma("sync", wo[:], wo_src.rearrange("(c p) n -> p c n", p=128), reads=[wo_key], writes=["of_wo"])
            for i in range(2):
                P.dma("sync", wd[:, i * 11:(i + 1) * 11, :],
                      wd_bf[layer].rearrange("(j p) n -> p j n", p=128)[:, i * 11:(i + 1) * 11, :],
                      reads=[f"wd_bf{layer}"], writes=["of_wd"])
            ym = [sbt(f"of_ym{i}", [128, 8, 512], BF16) for i in range(2)]
            xt = [sbt(f"of_x{i}", [128, 8, 512], F32) for i in range(2)]
            hT = sbt("of_h", [128, 8, 512], BF16)
            act = sbt("of_act", [128, NJ, 512], BF16)
            sg = [sbt(f"of_sg{i}", [128, 512], F32) for i in range(2)]
            wgu = [sbt(f"of_wgu{i}", [128, 2, 8, 128], BF16) for i in range(3)]
            if layer == 1:
                yo = sbt("of_yo", [128, 8, 512], F32)
                ytok = [sbt("of_ytok0", [128, 4, D], F32)] * 2
            nw = 0
            for it in range(NT):
                s = it % 2
                t0 = it * 512
                P.dma("sync", ym[s][:], ym_src.rearrange("(c p) t -> p c t", p=128)[:, :, t0:t0 + 512],
                      reads=[ym_key], writes=[f"of_ym{s}"])
                P.dma("sync", xt[s][:], x_src.rearrange("(c p) t -> p c t", p=128)[:, :, t0:t0 + 512],
                      reads=[x_key], writes=[f"of_x{s}"])
                for m in range(8):
                    pi = 1 + (m % 3)
                    for k in range(8):
                        mm(pi, wo[:, k, m * 128:(m + 1) * 128], ym[s][:, k, :], k == 0, k == 7,
                           ["of_wo", f"of_ym{s}"])
                    P.op("vector", "tensor_tensor", reads=[f"psb{pi}", f"of_x{s}"], writes=[f"of_x{s}"],
                         out=xt[s][:, m, :], in0=psb[pi][:], in1=xt[s][:, m, :], op=ALU.add)
                rmsnorm_T(xt[s], f"of_x{s}", gi, hT, "of_h")
                for j in range(NJ):
                    ws = nw % 3
                    nw += 1
                    P.dma("sync", wgu[ws][:, 0], wg_bf[layer][j], reads=[f"wg_bf{layer}"], writes=[f"of_wgu{ws}"])
                    P.dma("sync", wgu[ws][:, 1], wu_bf[layer][j], reads=[f"wu_bf{layer}"], writes=[f"of_wgu{ws}"])
                    pg, pu = 4 + 2 * (j % 2), 5 + 2 * (j % 2)
                    for k in range(8):
                        mm(pg, wgu[ws][:, 0, k, :], hT[:, k, :], k == 0, k == 7, [f"of_wgu{ws}", "of_h"])
                    for k in range(8):
                        mm(pu, wgu[ws][:, 1, k, :], hT[:, k, :], k == 0, k == 7, [f"of_wgu{ws}", "of_h"])
                    P.op("scalar", "activation", reads=[f"psb{pg}"], writes=[f"of_sg{j % 2}"], out=sg[j % 2][:],
                         in_=psb[pg][:], func=AF.Silu)
                    P.op("vector", "tensor_tensor", reads=[f"of_sg{j % 2}", f"psb{pu}"], writes=["of_act"],
                         out=act[:, j, :], in0=sg[j % 2][:], in1=psb[pu][:], op=ALU.mult)
                for m in range(8):
                    pi = 1 + (m % 3)
                    for j in range(NJ):
                        mm(pi, wd[:, j, m * 128:(m + 1) * 128], act[:, j, :], j == 0, j == NJ - 1,
                           ["of_wd", "of_act"])
                    P.op("vector", "tensor_tensor", reads=[f"psb{pi}", f"of_x{s}"], writes=[f"of_x{s}"],
                         out=xt[s][:, m, :], in0=psb[pi][:], in1=xt[s][:, m, :], op=ALU.add)
                if layer == 0 or "xT4" in dbg:
                    P.dma("sync", x_dst.rearrange("(c p) t -> p c t", p=128)[:, :, t0:t0 + 512], xt[s][:],
                          reads=[f"of_x{s}"], writes=[x_dkey])
                if layer == 1:
                    rmsnorm_T(xt[s], f"of_x{s}", 4, yo, "of_yo")
                    for sub in range(4):
                        for c in range(8):
                            pi = 1 + (sub % 3)
                            P.op("tensor", "transpose", reads=["of_yo", "ident_f"], writes=[f"psb{pi}"],
                                 out=psb[pi][:, (c % 4) * 128:(c % 4 + 1) * 128],
                                 in_=yo[:, c, sub * 128:(sub + 1) * 128], identity=ident_f[:])
                            if c % 4 == 3:
                                evac(ytok[s][:, sub, (c // 4) * 512:(c // 4 + 1) * 512], psb[pi][:], [f"psb{pi}"],
                                     ["of_ytok0"])
                                pi = 1 + ((sub + 1) % 3)
                    P.dma("sync", y_out[t0:t0 + 512, :].rearrange("(s p) d -> p s d", p=128), ytok[s][:],
                          reads=["of_ytok0"], writes=["y"])
            P.end_phase()

    inproj(0)

    if upto <= 1:
        P.finish()
        return nc

    with contextlib.ExitStack() as ph:
        def sbt(name, shape, dt):
            return ph.enter_context(nc.sbuf_tensor(name, list(shape), dt))
        convw = sbt("rg_convw", [128, 4, 4], F32)
        convb = sbt("rg_convb", [128, 4], F32)
        ba = sbt("rg_ba", [128, 4], F32)
        bx = sbt("rg_bx", [128, 4], F32)
        lam = sbt("rg_lam", [128, 4], F32)
        cvec = sbt("rg_cvec", [128, 4], F32)
        cvec2 = sbt("rg_cvec2", [128, 4], F32)
        wst = sbt("rg_wst", [128, 2, 4, 128], F32)
        wbd = sbt("rg_wbd", [128, 2, 4, 128], BF16)
        for j in range(4):
            P.dma("sync", convw[:, j, :], ab_conv_w[j].rearrange("(c p) -> p c", p=128), writes=["rg_convw"],
                  allow_slow_non_contiguous=True)
        for tl, src, key in ((convb, ab_conv_b, "rg_convb"), (ba, ab_gate_a_b, "rg_ba"),
                             (bx, ab_gate_x_b, "rg_bx"), (lam, ab_lambda, "rg_lam")):
            P.dma("sync", tl[:], src.rearrange("(c p) -> p c", p=128), writes=[key],
                  allow_slow_non_contiguous=True)
        P.op("gpsimd", "memset", writes=["rg_wst"], ap=wst[:], constant=0.0)
        for gi_, wsrc in enumerate((ab_gate_a_w, ab_gate_x_w)):
            for hd in range(8):
                cc, hl = hd // 2, hd % 2
                P.dma("sync", wst[hl * 64:(hl + 1) * 64, gi_, cc, hl * 64:(hl + 1) * 64], wsrc[hd],
                      writes=["rg_wst"])
        P.op("vector", "tensor_copy", reads=["rg_wst"], writes=["rg_wbd"], out=wbd[:], in_=wst[:])
        P.op("scalar", "activation", reads=["rg_lam"], writes=["rg_cvec"], out=cvec[:], in_=lam[:],
             func=AF.Exp, scale=-1.0)
        P.op("scalar", "activation", reads=["rg_cvec", "ones_f"], writes=["rg_cvec"], out=cvec[:], in_=cvec[:],
             func=AF.Ln, bias=ones_f[:, 0:1])
        P.op("vector", "tensor_scalar", reads=["rg_cvec"], writes=["rg_cvec2"], out=cvec2[:], in0=cvec[:],
             scalar1=-16.0, scalar2=None, op0=ALU.mult)
        P.op("vector", "tensor_scalar", reads=["rg_cvec"], writes=["rg_cvec"], out=cvec[:], in0=cvec[:],
             scalar1=-8.0, scalar2=None, op0=ALU.mult)
        B = [sbt(f"rgB{i}", [128, T], F32) for i in range(7)]
        xc_bf = sbt("rg_xcbf", [128, T], BF16)
        y_bf = sbt("rg_ybf", [128, T], BF16)
        rg_rows = rgin.rearrange("(c p) t -> c p t", p=128)
        ym_rows = ymix0.rearrange("(c p) t -> c p t", p=128)
        for cc in range(4):
            xr, gt, xc, rr, ii, a2, hh = B
            P.dma("sync", xr[:], rg_rows[cc], reads=["rgin"], writes=["rgB0"])
            P.dma("sync", gt[:], rg_rows[4 + cc], reads=["rgin"], writes=["rgB1"])
            P.op("vector", "tensor_scalar", reads=["rgB0", "rg_convw", "rg_convb"], writes=["rgB2"],
                 out=xc[:], in0=xr[:], scalar1=convw[:, 3, cc:cc + 1], scalar2=convb[:, cc:cc + 1],
                 op0=ALU.mult, op1=ALU.add)
            for j in (2, 1, 0):
                dl = 3 - j
                P.op("vector", "scalar_tensor_tensor", reads=["rgB0", "rgB2", "rg_convw"], writes=["rgB2"],
                     out=xc[:, dl:], in0=xr[:, 0:T - dl], scalar=convw[:, j, cc:cc + 1], in1=xc[:, dl:],
                     op0=ALU.mult, op1=ALU.add)
            P.op("gpsimd", "tensor_copy", reads=["rgB2"], writes=["rg_xcbf"], out=xc_bf[:], in_=xc[:])
            for tt in range(8):
                for gi_, (dst, dkey, bias) in enumerate(((rr, "rgB3", ba), (ii, "rgB4", bx))):
                    pi = (tt * 2 + gi_) % 4
                    mm(pi, wbd[:, gi_, cc, :], xc_bf[:, tt * 512:(tt + 1) * 512], True, True,
                       ["rg_wbd", "rg_xcbf"])
                    P.op("scalar", "activation", reads=[f"psb{pi}", "rg_ba", "rg_bx"], writes=[dkey],
                         out=dst[:, tt * 512:(tt + 1) * 512], in_=psb[pi][:], func=AF.Sigmoid,
                         bias=bias[:, cc:cc + 1])
            P.op("scalar", "activation", reads=["rgB3", "rg_cvec2"], writes=["rgB5"], out=a2[:], in_=rr[:],
                 func=AF.Exp, scale=cvec2[:, cc:cc + 1])
            P.op("scalar", "activation", reads=["rgB3", "rg_cvec"], writes=["rgB3"], out=rr[:], in_=rr[:],
                 func=AF.Exp, scale=cvec[:, cc:cc + 1])
            P.op("vector", "tensor_scalar", reads=["rgB5"], writes=["rgB5"], out=a2[:], in0=a2[:],
                 scalar1=-1.0, scalar2=1.0, op0=ALU.mult, op1=ALU.add)
            P.op("scalar", "activation", reads=["rgB5"], writes=["rgB5"], out=a2[:], in_=a2[:], func=AF.Sqrt)
            P.op("gpsimd", "tensor_tensor", reads=["rgB4", "rgB2"], writes=["rgB4"], out=ii[:], in0=ii[:],
                 in1=xc[:], op=ALU.mult)
            P.op("vector", "tensor_tensor", reads=["rgB4", "rgB5"], writes=["rgB4"], out=ii[:], in0=ii[:],
                 in1=a2[:], op=ALU.mult)
            P.op("vector", "tensor_tensor_scan", reads=["rgB3", "rgB4"], writes=["rgB6"], out=hh[:],
                 data0=rr[:], data1=ii[:], initial=0.0, op0=ALU.mult, op1=ALU.add)
            P.op("gpsimd", "tensor_tensor", reads=["rgB1"], writes=["rgB0"], out=xr[:], in0=gt[:], in1=gt[:],
                 op=ALU.mult)
            P.op("gpsimd", "tensor_scalar", reads=["rgB0"], writes=["rgB0"], out=xr[:], in0=xr[:],
                 scalar1=0.044715, scalar2=1.0, op0=ALU.mult, op1=ALU.add)
            P.op("gpsimd", "tensor_tensor", reads=["rgB0", "rgB1"], writes=["rgB0"], out=xr[:], in0=xr[:],
                 in1=gt[:], op=ALU.mult)
            P.op("scalar", "activation", reads=["rgB0"], writes=["rgB0"], out=xr[:], in_=xr[:],
                 func=AF.Sigmoid, scale=1.5957691216057308)
            P.op("vector", "tensor_tensor", reads=["rgB6", "rgB1"], writes=["rgB6"], out=hh[:], in0=hh[:],
                 in1=gt[:], op=ALU.mult)
            P.op("vector", "tensor_tensor", reads=["rgB6", "rgB0"], writes=["rg_ybf"], out=y_bf[:], in0=hh[:],
                 in1=xr[:], op=ALU.mult)
            P.dma("sync", ym_rows[cc], y_bf[:], reads=["rg_ybf"], writes=["ymix0"])
        P.end_phase()

    if upto <= 2:
        P.finish()
        return nc

    with contextlib.ExitStack() as ph:
        def sbt(name, shape, dt):
            return ph.enter_context(nc.sbuf_tensor(name, list(shape), dt))
        tri = sbt("sb_tri", [128, 128], BF16)
        mstr = sbt("sb_mstr", [128, 128], F32)
        P.op("gpsimd", "affine_select", reads=["ones_bf"], writes=["sb_tri"], out=tri[:], in_=ones_bf[:],
             pattern=[[-1, 128]], compare_op=ALU.is_ge, fill=0.0, base=0, channel_multiplier=1)
        P.op("gpsimd", "affine_select", reads=["ones_f"], writes=["sb_mstr"], out=mstr[:], in_=ones_f[:],
             pattern=[[1, 128]], compare_op=ALU.is_gt, fill=0.0, base=0, channel_multiplier=-1)
        v_all = sbt("sb_v", [128, 32, 512], BF16)
        v_src = v0.rearrange("(n p) d -> p n d", p=128)
        for i in range(4):
            P.dma("sync", v_all[:, i * 8:(i + 1) * 8, :], v_src[:, i * 8:(i + 1) * 8, :], reads=["v0"],
                  writes=["sb_v"])
        qT = [sbt(f"sb_q{i}", [128, T], BF16) for i in range(2)]
        kT = [sbt(f"sb_k{i}", [128, T], BF16) for i in range(2)]
        yst = [sbt(f"sb_y{i}", [128, T], BF16) for i in range(2)]
        for i in range(2):
            P.op("gpsimd", "memset", writes=[f"sb_q{i}"], ap=qT[i][:], constant=0.0)
            P.op("gpsimd", "memset", writes=[f"sb_k{i}"], ap=kT[i][:], constant=0.0)
        e_t = [sbt(f"sb_e{i}", [128, 512], F32) for i in range(3)]
        sp_t = [sbt(f"sb_sp{i}", [128, 512], BF16) for i in range(3)]
        en_t = [sbt(f"sb_en{i}", [128, 512], F32) for i in range(2)]
        w_t = [sbt(f"sb_w{i}", [128, 512], BF16) for i in range(2)]
        lacc_b = [sbt(f"sb_laccb{i}", [128, 512], BF16) for i in range(2)]
        qk_rows = qk0.rearrange("(h p) t -> h p t", p=64)
        ym128 = ymix0.rearrange("(h p) t -> h p t", p=128)
        items = []
        for hd in range(8):
            for qi in range(8):
                q0 = qi * 512
                kbs = list(range(q0 // 128 + 3, -1, -1))
                for bi, kb in enumerate(kbs):
                    items.append(dict(hd=hd, qi=qi, q0=q0, kb=kb, bi=bi, last=(kb == 0), idx=len(items)))

        def geom(it):
            c0 = max(0, it["kb"] * 128 - it["q0"])
            return c0, slice(c0, 512), slice(c0, c0 + 128), it["kb"] * 128 >= it["q0"]

        def st0(it):
            hd, hs, r = it["hd"], it["hd"] % 2, it["idx"] % 2
            if it["qi"] == 0 and it["bi"] == 0:
                P.dma("sync", qT[hs][0:64, :], qk_rows[hd], reads=["qk0"], writes=[f"sb_q{hs}"])
                P.dma("sync", kT[hs][0:64, :], qk_rows[8 + hd], reads=["qk0"], writes=[f"sb_k{hs}"])
            c0, cs, dg, diag = geom(it)
            mm(r, kT[hs][:, it["kb"] * 128:(it["kb"] + 1) * 128], qT[hs][:, it["q0"] + c0:it["q0"] + 512], True, True,
               [f"sb_k{hs}", f"sb_q{hs}"], out=psb[r][:, cs])

        def st1(it):
            r, r3 = it["idx"] % 2, it["idx"] % 3
            c0, cs, dg, diag = geom(it)
            P.op("scalar", "activation", reads=[f"psb{r}"], writes=[f"sb_e{r3}"], out=e_t[r3][:, cs],
                 in_=psb[r][:, cs], func=AF.Exp, scale=0.125)
            P.op("scalar", "activation", reads=[f"sb_e{r3}", "ones_f"], writes=[f"sb_sp{r3}"],
                 out=sp_t[r3][:, cs], in_=e_t[r3][:, cs], func=AF.Ln, bias=ones_f[:, 0:1])
            if diag:
                P.op("vector", "tensor_tensor", reads=[f"sb_sp{r3}", "sb_mstr"], writes=[f"sb_sp{r3}"],
                     out=sp_t[r3][:, dg], in0=sp_t[r3][:, dg], in1=mstr[:], op=ALU.mult)

        def st2(it):
            r, r3 = it["idx"] % 2, it["idx"] % 3
            lq = (it["hd"] * 8 + it["qi"]) % 2
            c0, cs, dg, diag = geom(it)
            pc = 2 + r
            if it["bi"] == 0:
                P.op("gpsimd", "memset", writes=[f"sb_laccb{lq}"], ap=lacc_b[lq][:], constant=0.0)
            mm(pc, tri[:], sp_t[r3][:, cs], True, it["bi"] == 0, ["sb_tri", f"sb_sp{r3}"], out=psb[pc][:, cs])
            if it["bi"] > 0:
                mm(pc, ones_bf[:], lacc_b[lq][:, cs], False, True, ["ones_bf", f"sb_laccb{lq}"], out=psb[pc][:, cs])
            if not it["last"]:
                P.op("vector", "tensor_tensor", reads=[f"sb_laccb{lq}", f"sb_sp{r3}"], writes=[f"sb_laccb{lq}"],
                     out=lacc_b[lq][:, cs], in0=lacc_b[lq][:, cs], in1=sp_t[r3][:, cs], op=ALU.add)

        def st3(it):
            r, r3 = it["idx"] % 2, it["idx"] % 3
            c0, cs, dg, diag = geom(it)
            pc = 2 + r
            P.op("scalar", "activation", reads=[f"psb{pc}"], writes=[f"sb_en{r}"], out=en_t[r][:, cs],
                 in_=psb[pc][:, cs], func=AF.Exp, scale=-1.0)
            P.op("vector", "tensor_tensor", reads=[f"sb_e{r3}", f"sb_en{r}"], writes=[f"sb_w{r}"],
                 out=w_t[r][:, cs], in0=e_t[r3][:, cs], in1=en_t[r][:, cs], op=ALU.mult)
            if diag:
                P.op("vector", "tensor_tensor", reads=[f"sb_w{r}", "sb_mstr"], writes=[f"sb_w{r}"],
                     out=w_t[r][:, dg], in0=w_t[r][:, dg], in1=mstr[:], op=ALU.mult)

        def st4(it):
            hd, r = it["hd"], it["idx"] % 2
            c0, cs, dg, diag = geom(it)
            po = 4 + (it["qi"] % 2)
            ys = (hd // 2) % 2
            hr = slice((hd % 2) * 64, (hd % 2) * 64 + 64)
            mm(po, v_all[:, it["kb"], (hd // 2) * 128:(hd // 2) * 128 + 128], w_t[r][:, cs], it["bi"] == 0, it["last"],
               ["sb_v", f"sb_w{r}"], out=psb[po][:, cs])
            if it["last"]:
                evac(yst[ys][hr, it["q0"]:it["q0"] + 512], psb[po][hr, :], [f"psb{po}"], [f"sb_y{ys}"])
                if it["qi"] == 7 and hd % 2 == 1:
                    P.dma("sync", ym128[4 + hd // 2], yst[ys][:], reads=[f"sb_y{ys}"], writes=["ymix0"])

        stages = [st0, st1, st2, st3, st4]
        for step in range(len(items) + len(stages) - 1):
            for j in range(len(stages) - 1, -1, -1):
                t_ = step - j
                if 0 <= t_ < len(items):
                    stages[j](items[t_])
        P.end_phase()

    if upto <= 3:
        P.finish()
        return nc

    out_ffn(0)
    if upto <= 4:
        P.finish()
        return nc

    inproj(1)
    if upto <= 5:
        P.finish()
        return nc

    NEG = -30000.0
    with contextlib.ExitStack() as ph:
        def sbt(name, shape, dt):
            return ph.enter_context(nc.sbuf_tensor(name, list(shape), dt))
        slopes = [2.0 ** (-2.0 * (h + 1)) for h in range(4)]
        io33 = sbt("mb_io33", [128, 33], F32)
        pidx = sbt("mb_pidx", [128, 2], F32)
        biasT = sbt("mb_biasT", [128, 4, 33], F32)
        nsl = sbt("mb_nsl", [128, 4, 2], F32)
        P.op("gpsimd", "iota", writes=["mb_io33"], out=io33[:], pattern=[[-128, 33]], base=128,
             channel_multiplier=1, allow_small_or_imprecise_dtypes=True)
        P.op("gpsimd", "iota", writes=["mb_pidx"], out=pidx[:], pattern=[[128, 2]], base=0,
             channel_multiplier=1, allow_small_or_imprecise_dtypes=True)
        for h in range(4):
            P.op("vector", "tensor_scalar", reads=["mb_io33"], writes=["mb_biasT"], out=biasT[:, h, :],
                 in0=io33[:], scalar1=slopes[h], scalar2=None, op0=ALU.mult)
            P.op("vector", "tensor_scalar", reads=["mb_pidx"], writes=["mb_nsl"], out=nsl[:, h, :],
                 in0=pidx[:], scalar1=-slopes[h], scalar2=None, op0=ALU.mult)
        pastm = sbt("mb_pastm", [128, 32, 32], F32)
        P.op("gpsimd", "memset", writes=["mb_pastm"], ap=pastm[:], constant=0.0)
        pm4 = pastm[:].rearrange("p (b e) n -> p b e n", e=2)[:, :, :, 0:16]
        P.op("gpsimd", "affine_select", reads=["mb_pastm"], writes=["mb_pastm"], out=pm4, in_=pm4,
             pattern=[[1, 16], [0, 2], [-1, 16]], compare_op=ALU.is_gt, fill=NEG, base=0, channel_multiplier=0)
        efull = sbt("mb_efull", [128, 128, 128], BF16)
        P.op("gpsimd", "memset", writes=["mb_efull"], ap=efull[:], constant=1.0)
        P.op("gpsimd", "affine_select", reads=["mb_efull"], writes=["mb_efull"], out=efull[:], in_=efull[:],
             pattern=[[-1, 128], [0, 128]], compare_op=ALU.is_equal, fill=0.0, base=0, channel_multiplier=1)
        mc = sbt("mb_mc", [128, 128], F32)
        P.op("gpsimd", "affine_select", reads=["ones_f"], writes=["mb_mc"], out=mc[:], in_=ones_f[:],
             pattern=[[1, 128]], compare_op=ALU.is_ge, fill=0.0, base=0, channel_multiplier=-1)
        v_all = sbt("mb_v", [128, 32, 512], BF16)
        v_src = v1.rearrange("(n p) d -> p n d", p=128)
        for i in range(4):
            P.dma("sync", v_all[:, i * 8:(i + 1) * 8, :], v_src[:, i * 8:(i + 1) * 8, :], reads=["v1"],
                  writes=["mb_v"])
        qT = [sbt(f"mb_q{i}", [128, T], BF16) for i in range(2)]
        kT = [sbt(f"mb_k{i}", [128, T], BF16) for i in range(2)]
        yst = [sbt(f"mb_y{i}", [128, T], BF16) for i in range(2)]
        km_f = sbt("mb_kmf", [128, 16], F32)
        km_b = sbt("mb_kmb", [128, 16], BF16)
        gm = sbt("mb_gm", [128, 32, 32], F32)
        ng = sbt("mb_ng", [128, 32, 32], F32)
        m8 = sbt("mb_m8", [128, 32, 8], F32)
        rt4 = [sbt(f"mb_rt4{i}", [128, 8, 128], BF16) for i in range(2)]
        w_t = [sbt(f"mb_w{i}", [128, 256], BF16) for i in range(3)]
        rz = [sbt(f"mb_rz{i}", [128, 256], F32) for i in range(2)]
        P.op("gpsimd", "memset", writes=["mb_gm"], ap=gm[:], constant=0.0)
        P.op("gpsimd", "memset", writes=["mb_ng"], ap=ng[:], constant=0.0)
        qk_rows = qk1.rearrange("(h p) t -> h p t", p=128)
        ym128 = ymix1.rearrange("(h p) t -> h p t", p=128)
        def gating(hd):
            hs = hd % 2
            P.dma("sync", qT[hs][:], qk_rows[hd], reads=["qk1"], writes=[f"mb_q{hs}"])
            P.dma("sync", kT[hs][:], qk_rows[4 + hd], reads=["qk1"], writes=[f"mb_k{hs}"])
            P.op("vector", "tensor_reduce", reads=[f"mb_k{hs}"], writes=["mb_kmf"], out=km_f[:],
                 in_=kT[hs][:].rearrange("p (n k) -> p n k", k=256), axis=AX.X, op=ALU.add)
            P.op("vector", "tensor_scalar", reads=["mb_kmf"], writes=["mb_kmb"], out=km_b[:], in0=km_f[:],
                 scalar1=1.0 / 256.0, scalar2=None, op0=ALU.mult)
            for i in range(32):
                mm(6, qT[hs][:, i * 128:(i + 1) * 128], km_b[:], True, True, [f"mb_q{hs}", "mb_kmb"],
                   out=psb[6][:, i * 16:(i + 1) * 16])
            P.op("vector", "tensor_tensor", reads=["psb6", "mb_pastm"], writes=["mb_gm"], out=gm[:, :, 0:16],
                 in0=psb[6][:].rearrange("p (i n) -> p i n", n=16), in1=pastm[:, :, 0:16], op=ALU.add)
            for i in range(32):
                P.op("vector", "max", reads=["mb_gm"], writes=["mb_m8"], out=m8[:, i, :], in_=gm[:, i, 0:16])
            P.op("vector", "tensor_tensor", reads=["mb_gm", "mb_m8"], writes=["mb_ng"], out=ng[:, :, 0:16],
                 in0=gm[:, :, 0:16], in1=m8[:, :, 2:3].to_broadcast([128, 32, 16]), op=ALU.is_ge)
            P.op("vector", "tensor_scalar", reads=["mb_ng"], writes=["mb_ng"], out=ng[:, :, 0:16],
                 in0=ng[:, :, 0:16], scalar1=-1.0, scalar2=-NEG, op0=ALU.add, op1=ALU.mult)
            P.op("vector", "tensor_tensor", reads=["mb_ng", "mb_pastm"], writes=["mb_ng"], out=ng[:, :, 0:16],
                 in0=ng[:, :, 0:16], in1=pastm[:, :, 0:16], op=ALU.add)
            P.op("vector", "memset", writes=["mb_ng"], ap=ng[:, :, 16:17], constant=0.0)
            ng4 = ng[:].rearrange("p (b e) n -> p b e n", e=2)
            for e_ in range(2):
                P.op("vector", "tensor_scalar", reads=["mb_ng", "mb_nsl"], writes=["mb_ng"],
                     out=ng4[:, :, e_, 0:17], in0=ng4[:, :, e_, 0:17], scalar1=nsl[:, hd, e_:e_ + 1],
                     scalar2=None, op0=ALU.add)
            for g in range(8):
                pi = 6 + (g // 4)
                P.op("tensor", "transpose", reads=["mb_ng", "ident_f"], writes=[f"psb{pi}"],
                     out=psb[pi][:, (g % 4) * 128:(g % 4 + 1) * 128],
                     in_=ng[:, 4 * g:4 * g + 4, :].rearrange("p a n -> p (a n)"), identity=ident_f[:])
                if g % 4 == 3:
                    evac(rt4[hs][:, g - 3:g + 1, :].rearrange("p a t -> p (a t)"), psb[pi][:], [f"psb{pi}"],
                         [f"mb_rt4{hs}"])

        items = []
        for hd in range(4):
            for b in range(16):
                for kt in range(2 * b + 2):
                    items.append(dict(hd=hd, b=b, kt=kt, nkt=2 * b + 2, idx=len(items)))

        def mgeom(it):
            c0 = 128 if it["kt"] == 2 * it["b"] + 1 else 0
            return c0, slice(c0, 256), it["kt"] >= 2 * it["b"]

        def m0(it):
            hd, hs, b, kt, r = it["hd"], it["hd"] % 2, it["b"], it["kt"], it["idx"] % 2
            if b == 8 and kt == 0 and hd < 3:
                gating(hd + 1)
            c0, cs, own = mgeom(it)
            n_row = 16 if own else kt // 2
            mm(r, kT[hs][:, kt * 128:(kt + 1) * 128], qT[hs][:, b * 256 + c0:(b + 1) * 256], True, False,
               [f"mb_k{hs}", f"mb_q{hs}"], out=psb[r][:, cs])
            for e_ in range(c0 // 128, 2):
                il = 2 * (b % 2) + e_
                mm(r, efull[:, il * 32 + n_row, :], rt4[hs][:, b // 2, :], False, e_ == 1,
                   ["mb_efull", f"mb_rt4{hs}"], out=psb[r][:, e_ * 128:(e_ + 1) * 128])

        def m1(it):
            hd, b, kt, r, ws = it["hd"], it["b"], it["kt"], it["idx"] % 2, it["idx"] % 3
            c0, cs, own = mgeom(it)
            dd = 2 * b - kt + 1
            P.op("scalar", "activation", reads=[f"psb{r}", "mb_biasT"], writes=[f"mb_w{ws}"],
                 out=w_t[ws][:, cs], in_=psb[r][:, cs], func=AF.Exp, bias=biasT[:, hd, dd:dd + 1])
            if own:
                P.op("vector", "tensor_tensor", reads=[f"mb_w{ws}", "mb_mc"], writes=[f"mb_w{ws}"],
                     out=w_t[ws][:, c0:c0 + 128], in0=w_t[ws][:, c0:c0 + 128], in1=mc[:], op=ALU.mult)

        def m2(it):
            hd, hs, b, kt, ws = it["hd"], it["hd"] % 2, it["b"], it["kt"], it["idx"] % 3
            c0, cs, own = mgeom(it)
            po, pz = 2 + (b % 2), 4 + (b % 2)
            last = kt == it["nkt"] - 1
            mm(po, v_all[:, kt, hd * 128:(hd + 1) * 128], w_t[ws][:, cs], kt == 0, last,
               ["mb_v", f"mb_w{ws}"], out=psb[po][:, cs])
            mm(pz, ones_bf[:], w_t[ws][:, cs], kt == 0, last, ["ones_bf", f"mb_w{ws}"], out=psb[pz][:, cs])
            if last:
                P.op("vector", "reciprocal", reads=[f"psb{pz}"], writes=[f"mb_rz{b % 2}"], out=rz[b % 2][:],
                     in_=psb[pz][:, 0:256])
                P.op("vector", "tensor_tensor", reads=[f"psb{po}", f"mb_rz{b % 2}"], writes=[f"mb_y{hs}"],
                     out=yst[hs][:, b * 256:(b + 1) * 256], in0=psb[po][:, 0:256], in1=rz[b % 2][:], op=ALU.mult)
                if b == 15:
                    P.dma("sync", ym128[4 + hd], yst[hs][:], reads=[f"mb_y{hs}"], writes=["ymix1"])

        gating(0)
        mst = [m0, m1, m2]
        for step in range(len(items) + len(mst) - 1):
            for j in range(len(mst) - 1, -1, -1):
                t_ = step - j
                if 0 <= t_ < len(items):
                    mst[j](items[t_])
        P.end_phase()

    if upto <= 6:
        P.finish()
        return nc

    INV_2PI = 1.0 / (2.0 * math.pi)
    MAGIC = 12582912.0
    SIN_SCALE = 6.283185
    with contextlib.ExitStack() as ph:
        def sbt(name, shape, dt):
            return ph.enter_context(nc.sbuf_tensor(name, list(shape), dt))

        def vop(eng, meth, reads, writes, **kw):
            P.op(eng, meth, reads=reads, writes=writes, **kw)

        Ts = sbt("s5_Ts", [128, 4, 8, 2, 128], BF16)
        To = sbt("s5_To", [128, 16, 8, 2, 32], BF16)
        Kt = sbt("s5_Kt", [128, 4, 8, 128], BF16)
        phiT = sbt("s5_phiT", [128, 16], F32)
        rhoT = sbt("s5_rhoT", [128, 16], F32)
        negpi = sbt("s5_negpi", [128, 1], F32)
        pst = contextlib.ExitStack()
        sbt_outer = sbt

        def sbt(name, shape, dt):
            return pst.enter_context(nc.sbuf_tensor(name, list(shape), dt))
        lr = sbt("s5_lr", [128, 2, 64], F32)
        li = sbt("s5_li", [128, 2, 64], F32)
        ldt = sbt("s5_ldt", [128, 2], F32)
        bre = sbt("s5_bre", [128, 2, 64, 16], F32)
        bim = sbt("s5_bim", [128, 2, 64, 16], F32)
        cst = sbt("s5_cst", [128, 2, 16, 128], F32)
        for tl, key in ((lr, "s5_lr"), (li, "s5_li"), (ldt, "s5_ldt"), (bre, "s5_bre"), (bim, "s5_bim"),
                        (cst, "s5_cst")):
            P.op("gpsimd", "memset", writes=[key], ap=tl[:], constant=0.0)
        P.dma("sync", lr[0:16], cd_lam_re.rearrange("(a g) n -> a g n", g=2), writes=["s5_lr"])
        P.dma("sync", li[0:16], cd_lam_im.rearrange("(a g) n -> a g n", g=2), writes=["s5_li"])
        P.dma("sync", ldt[0:16], cd_log_dt.rearrange("(a g) -> a g", g=2), writes=["s5_ldt"])
        P.dma("sync", bre[0:16], cd_b_re.rearrange("(a g) n p -> a g n p", g=2), writes=["s5_bre"])
        P.dma("sync", bim[0:16], cd_b_im.rearrange("(a g) n p -> a g n p", g=2), writes=["s5_bim"])
        for ri_, csrc in ((0, cd_c_re), (1, cd_c_im)):
            for g_ in range(2):
                P.dma("sync", cst[0:16, ri_, :, g_ * 64:(g_ + 1) * 64],
                      csrc.rearrange("(a g) p n -> a g p n", g=2)[:, g_], writes=["s5_cst"])
        P.op("gpsimd", "memset", writes=["s5_negpi"], ap=negpi[:], constant=-0.5 * math.pi)
        dtt = sbt("s5_dt", [128, 2], F32)
        vop("scalar", "activation", ["s5_ldt"], ["s5_dt"], out=dtt[:], in_=ldt[:], func=AF.Exp)
        sm = {}
        for nm in ("lrdt", "lidt", "mag", "a1", "sinv", "cosv", "abre", "abim", "den", "t1", "t2", "fre", "fim"):
            sm[nm] = sbt("s5_" + nm, [128, 2, 64], F32)

        def k(nm):
            return "s5_" + nm
        dt_bc = dtt[:].unsqueeze(2).to_broadcast([128, 2, 64])
        vop("vector", "tensor_tensor", [k("lr"), k("dt")], [k("lrdt")], out=sm["lrdt"][:], in0=lr[:], in1=dt_bc, op=ALU.mult)
        vop("vector", "tensor_tensor", [k("li"), k("dt")], [k("lidt")], out=sm["lidt"][:], in0=li[:], in1=dt_bc, op=ALU.mult)
        vop("scalar", "activation", [k("lrdt")], [k("mag")], out=sm["mag"][:], in_=sm["lrdt"][:], func=AF.Exp)
        vop("vector", "tensor_scalar", [k("lidt")], [k("a1")], out=sm["a1"][:], in0=sm["lidt"][:], scalar1=INV_2PI,
            scalar2=MAGIC, op0=ALU.mult, op1=ALU.add)
        vop("vector", "tensor_scalar", [k("a1")], [k("a1")], out=sm["a1"][:], in0=sm["a1"][:], scalar1=-MAGIC,
            scalar2=None, op0=ALU.add)
        vop("vector", "scalar_tensor_tensor", [k("lidt"), k("a1")], [k("a1")], out=sm["a1"][:], in0=sm["lidt"][:],
            scalar=INV_2PI, in1=sm["a1"][:], op0=ALU.mult, op1=ALU.subtract)
        vop("scalar", "activation", [k("a1")], [k("sinv")], out=sm["sinv"][:], in_=sm["a1"][:], func=AF.Sin,
            scale=SIN_SCALE)
        vop("vector", "scalar_tensor_tensor", [k("a1")], [k("a1")], out=sm["a1"][:], in0=sm["a1"][:], scalar=-1.0,
            in1=sm["a1"][:], op0=ALU.mult, op1=ALU.max)
        vop("scalar", "activation", [k("a1"), k("negpi")], [k("cosv")], out=sm["cosv"][:], in_=sm["a1"][:],
            func=AF.Sin, scale=SIN_SCALE, bias=negpi[:])
        vop("vector", "scalar_tensor_tensor", [k("cosv"), k("mag")], [k("abre")], out=sm["abre"][:], in0=sm["cosv"][:],
            scalar=-1.0, in1=sm["mag"][:], op0=ALU.mult, op1=ALU.mult)
        vop("vector", "tensor_tensor", [k("sinv"), k("mag")], [k("abim")], out=sm["abim"][:], in0=sm["sinv"][:],
            in1=sm["mag"][:], op=ALU.mult)
        vop("vector", "tensor_tensor", [k("lr")], [k("den")], out=sm["den"][:], in0=lr[:], in1=lr[:], op=ALU.mult)
        vop("vector", "tensor_tensor", [k("li")], [k("t1")], out=sm["t1"][:], in0=li[:], in1=li[:], op=ALU.mult)
        vop("vector", "tensor_tensor", [k("den"), k("t1")], [k("den")], out=sm["den"][:], in0=sm["den"][:], in1=sm["t1"][:], op=ALU.add)
        vop("vector", "tensor_scalar", [k("den")], [k("den")], out=sm["den"][:], in0=sm["den"][:], scalar1=1e-30,
            scalar2=None, op0=ALU.max)
        vop("vector", "reciprocal", [k("den")], [k("den")], out=sm["den"][:], in_=sm["den"][:])
        vop("vector", "tensor_scalar", [k("abre")], [k("t2")], out=sm["t2"][:], in0=sm["abre"][:], scalar1=-1.0,
            scalar2=None, op0=ALU.add)
        vop("vector", "tensor_tensor", [k("t2"), k("lr")], [k("fre")], out=sm["fre"][:], in0=sm["t2"][:], in1=lr[:], op=ALU.mult)
        vop("vector", "tensor_tensor", [k("abim"), k("li")], [k("t1")], out=sm["t1"][:], in0=sm["abim"][:], in1=li[:], op=ALU.mult)
        vop("vector", "tensor_tensor", [k("fre"), k("t1")], [k("fre")], out=sm["fre"][:], in0=sm["fre"][:], in1=sm["t1"][:], op=ALU.add)
        vop("vector", "tensor_tensor", [k("fre"), k("den")], [k("fre")], out=sm["fre"][:], in0=sm["fre"][:], in1=sm["den"][:], op=ALU.mult)
        vop("vector", "tensor_tensor", [k("abim"), k("lr")], [k("fim")], out=sm["fim"][:], in0=sm["abim"][:], in1=lr[:], op=ALU.mult)
        vop("vector", "tensor_tensor", [k("t2"), k("li")], [k("t1")], out=sm["t1"][:], in0=sm["t2"][:], in1=li[:], op=ALU.mult)
        vop("vector", "tensor_tensor", [k("fim"), k("t1")], [k("fim")], out=sm["fim"][:], in0=sm["fim"][:], in1=sm["t1"][:], op=ALU.subtract)
        vop("vector", "tensor_tensor", [k("fim"), k("den")], [k("fim")], out=sm["fim"][:], in0=sm["fim"][:], in1=sm["den"][:], op=ALU.mult)
        bbre = sbt("s5_bbre", [128, 2, 64, 16], F32)
        bbim = sbt("s5_bbim", [128, 2, 64, 16], F32)
        tb1 = sbt("s5_tb1", [128, 2, 64, 16], F32)
        tb2 = sbt("s5_tb2", [128, 2, 64, 16], F32)
        fre_bc = sm["fre"][:].unsqueeze(3).to_broadcast([128, 2, 64, 16])
        fim_bc = sm["fim"][:].unsqueeze(3).to_broadcast([128, 2, 64, 16])
        vop("vector", "tensor_tensor", [k("bre"), k("fre")], [k("tb1")], out=tb1[:], in0=bre[:], in1=fre_bc, op=ALU.mult)
        vop("vector", "tensor_tensor", [k("bim"), k("fim")], [k("tb2")], out=tb2[:], in0=bim[:], in1=fim_bc, op=ALU.mult)
        vop("vector", "tensor_tensor", [k("tb1"), k("tb2")], [k("bbre")], out=bbre[:], in0=tb1[:], in1=tb2[:], op=ALU.subtract)
        vop("vector", "tensor_tensor", [k("bim"), k("fre")], [k("tb1")], out=tb1[:], in0=bim[:], in1=fre_bc, op=ALU.mult)
        vop("vector", "tensor_tensor", [k("bre"), k("fim")], [k("tb2")], out=tb2[:], in0=bre[:], in1=fim_bc, op=ALU.mult)
        vop("vector", "tensor_tensor", [k("tb1"), k("tb2")], [k("bbim")], out=bbim[:], in0=tb1[:], in1=tb2[:], op=ALU.add)
        pw = sbt("s5_pw", [128, 9, 2, 128], F32)
        pt1 = sbt("s5_pt1", [128, 128], F32)
        pt2 = sbt("s5_pt2", [128, 128], F32)
        P.op("gpsimd", "memset", writes=[k("pw")], ap=pw[:, 0, 0, :], constant=1.0)
        P.op("gpsimd", "memset", writes=[k("pw")], ap=pw[:, 0, 1, :], constant=0.0)
        abre_f = sm["abre"][:].rearrange("p a b -> p (a b)")
        abim_f = sm["abim"][:].rearrange("p a b -> p (a b)")
        for kk in range(1, 9):
            vop("vector", "tensor_tensor", [k("pw"), k("abre")], [k("pt1")], out=pt1[:], in0=pw[:, kk - 1, 0, :], in1=abre_f, op=ALU.mult)
            vop("vector", "tensor_tensor", [k("pw"), k("abim")], [k("pt2")], out=pt2[:], in0=pw[:, kk - 1, 1, :], in1=abim_f, op=ALU.mult)
            vop("vector", "tensor_tensor", [k("pt1"), k("pt2")], [k("pw")], out=pw[:, kk, 0, :], in0=pt1[:], in1=pt2[:], op=ALU.subtract)
            vop("vector", "tensor_tensor", [k("pw"), k("abim")], [k("pt1")], out=pt1[:], in0=pw[:, kk - 1, 0, :], in1=abim_f, op=ALU.mult)
            vop("vector", "tensor_tensor", [k("pw"), k("abre")], [k("pt2")], out=pt2[:], in0=pw[:, kk - 1, 1, :], in1=abre_f, op=ALU.mult)
            vop("vector", "tensor_tensor", [k("pt1"), k("pt2")], [k("pw")], out=pw[:, kk, 1, :], in0=pt1[:], in1=pt2[:], op=ALU.add)
        hm = sbt("s5_hm", [128, 2], F32)
        P.op("gpsimd", "memset", writes=[k("hm")], ap=hm[:], constant=0.0)
        P.op("gpsimd", "memset", writes=[k("hm")], ap=hm[0:64, 0:1], constant=1.0)
        P.op("gpsimd", "memset", writes=[k("hm")], ap=hm[64:128, 1:2], constant=1.0)
        Wall = [sbt(f"s5_Wall{i}", [128, 2, 16, 128], BF16) for i in range(2)]
        Wf = [sbt(f"s5_Wf{i}", [128, 2, 16, 128], F32) for i in range(2)]
        raw = [sbt(f"s5_raw{i}", [128, 2, 16, 16], F32) for i in range(2)]
        WTb = sbt("s5_WTb", [128, 16, 8, 2, 32], BF16)
        ToC = sbt("s5_ToC", [128, 16, 2, 32], BF16)
        ntr = [0]

        def transpose_mask(src, skey, dst_fn, dkey):
            rw = ntr[0] % 2
            ntr[0] += 1
            for ri in range(2):
                for p4 in range(4):
                    pi = 1 + (p4 % 2)
                    for pp in range(4):
                        P.op("tensor", "transpose", reads=[skey, "ident_f"], writes=[f"psb{pi}"],
                             out=psb[pi][:, pp * 128:(pp + 1) * 128], in_=src[:, ri, p4 * 4 + pp, :], identity=ident_f[:])
                    evac(raw[rw][:, ri, p4 * 4:(p4 + 1) * 4, :],
                         psb[pi][:].rearrange("p (a b) -> p a b", b=128)[:, :, 0:16], [f"psb{pi}"], [f"s5_raw{rw}"])
            for ri in range(2):
                for glp in range(2):
                    vop("vector", "tensor_scalar", [f"s5_raw{rw}", k("hm")], [dkey], out=dst_fn(ri, glp),
                        in0=raw[rw][:, ri, :, :].rearrange("p a b -> p b a"), scalar1=hm[:, glp:glp + 1], scalar2=None,
                        op0=ALU.mult)

        for kk in range(8):
            wf = Wf[kk % 2]
            wkey = f"s5_Wf{kk % 2}"
            pre_bc = pw[:, kk, 0, :].rearrange("p (g n) -> p g n", g=2).unsqueeze(3).to_broadcast([128, 2, 64, 16])
            pim_bc = pw[:, kk, 1, :].rearrange("p (g n) -> p g n", g=2).unsqueeze(3).to_broadcast([128, 2, 64, 16])
            vop("vector", "tensor_tensor", [k("bbre"), k("pw")], [k("tb1")], out=tb1[:], in0=bbre[:], in1=pre_bc, op=ALU.mult)
            vop("vector", "tensor_tensor", [k("bbim"), k("pw")], [k("tb2")], out=tb2[:], in0=bbim[:], in1=pim_bc, op=ALU.mult)
            vop("vector", "tensor_tensor", [k("tb1"), k("tb2")], [wkey], out=wf[:, 0].rearrange("a p (g n) -> a g n p", g=2),
                in0=tb1[:], in1=tb2[:], op=ALU.subtract)
            vop("vector", "tensor_tensor", [k("bbim"), k("pw")], [k("tb1")], out=tb1[:], in0=bbim[:], in1=pre_bc, op=ALU.mult)
            vop("vector", "tensor_tensor", [k("bbre"), k("pw")], [k("tb2")], out=tb2[:], in0=bbre[:], in1=pim_bc, op=ALU.mult)
            vop("vector", "tensor_tensor", [k("tb1"), k("tb2")], [wkey], out=wf[:, 1].rearrange("a p (g n) -> a g n p", g=2),
                in0=tb1[:], in1=tb2[:], op=ALU.add)
            vop("gpsimd", "tensor_copy", [wkey], [f"s5_Wall{kk % 2}"], out=Wall[kk % 2][:], in_=wf[:])
            P.dma("sync", s5wd[:, kk], Wall[kk % 2][0:16], reads=[f"s5_Wall{kk % 2}"], writes=["s5wd"])
            transpose_mask(wf, wkey, lambda ri, glp, kk=kk: WTb[:, :, kk, ri, glp * 16:(glp + 1) * 16], k("WTb"))
        P.op("gpsimd", "memset", writes=[k("Ts")], ap=Ts[:], constant=0.0)
        nd = 0
        for pair in range(16):
            ct, q = pair // 4, pair % 4
            for gl in range(2):
                for j in range(8):
                    P.dma("sync" if nd % 2 == 0 else "gpsimd",
                          Ts[32 * q + 16 * gl:32 * q + 16 * gl + 16, ct, j, :, gl * 64:(gl + 1) * 64],
                          s5wd[pair, 7 - j, :, :, gl * 64:(gl + 1) * 64].rearrange("r p n -> p r n"),
                          reads=["s5wd"], writes=[k("Ts")])
                    nd += 1
        tcl = [t_[:].rearrange("a g n p -> a (g n p)").rearrange("a (x y) -> a x y", y=128) for t_ in (tb1, tb2)]
        for i in range(-1, 8):
            wf = Wf[i % 2]
            wkey = f"s5_Wf{i % 2}"
            pre_bc = pw[:, i + 1, 0, :].unsqueeze(1).to_broadcast([128, 16, 128])
            pim_bc = pw[:, i + 1, 1, :].unsqueeze(1).to_broadcast([128, 16, 128])
            vop("vector", "tensor_tensor", [k("cst"), k("pw")], [k("tb1")], out=tcl[0], in0=cst[:, 0], in1=pre_bc, op=ALU.mult)
            vop("vector", "tensor_tensor", [k("cst"), k("pw")], [k("tb2")], out=tcl[1], in0=cst[:, 1], in1=pim_bc, op=ALU.mult)
            vop("vector", "tensor_tensor", [k("tb1"), k("tb2")], [wkey], out=wf[:, 0], in0=tcl[0], in1=tcl[1], op=ALU.subtract)
            vop("vector", "tensor_tensor", [k("cst"), k("pw")], [k("tb1")], out=tcl[0], in0=cst[:, 0], in1=pim_bc, op=ALU.mult)
            vop("vector", "tensor_tensor", [k("cst"), k("pw")], [k("tb2")], out=tcl[1], in0=cst[:, 1], in1=pre_bc, op=ALU.mult)
            vop("vector", "scalar_tensor_tensor", [k("tb1"), k("tb2")], [wkey], out=wf[:, 1], in0=tcl[0], scalar=-1.0,
                in1=tcl[1], op0=ALU.mult, op1=ALU.subtract)
            if i < 0:
                transpose_mask(wf, wkey, lambda ri, glp: ToC[:, :, ri, glp * 16:(glp + 1) * 16], k("ToC"))
            else:
                transpose_mask(wf, wkey, lambda ri, glp, i=i: To[:, :, i, ri, glp * 16:(glp + 1) * 16], k("To"))
        P.op("gpsimd", "memset", writes=[k("Kt")], ap=Kt[:], constant=0.0)
        for ct in range(4):
            pk = 3 + (ct % 2)
            P.pe_drain()
            for q in range(4):
                pair = ct * 4 + q
                for tau in range(8):
                    for ri in range(2):
                        P.op("tensor", "matmul", reads=[k("WTb"), k("ToC")], writes=[f"psb{pk}"],
                             out=psb[pk][32 * q:32 * q + 32, tau * 32:(tau + 1) * 32], lhsT=WTb[:, pair, tau, ri, :],
                             rhs=ToC[:, pair, ri, :], start=(ri == 0), stop=(ri == 1), tile_position=(0, 32 * q))
            P.pe_drain()
            for q in range(4):
                evac(Kt[32 * q:32 * q + 32, ct, :, 32 * q:32 * q + 32],
                     psb[pk][32 * q:32 * q + 32, 0:256].rearrange("p (a b) -> p a b", b=32), [f"psb{pk}"], [k("Kt")])
        P.op("tensor", "transpose", reads=[k("lidt"), "ident_f"], writes=["psb1"], out=psb[1][:, 0:128],
             in_=sm["lidt"][:].rearrange("p a b -> p (a b)"), identity=ident_f[:])
        P.op("tensor", "transpose", reads=[k("lrdt"), "ident_f"], writes=["psb1"], out=psb[1][:, 128:256],
             in_=sm["lrdt"][:].rearrange("p a b -> p (a b)"), identity=ident_f[:])
        vop("vector", "tensor_scalar", ["psb1"], [k("phiT")], out=phiT[:], in0=psb[1][:, 0:16], scalar1=8.0 * INV_2PI,
            scalar2=None, op0=ALU.mult)
        vop("scalar", "activation", ["psb1"], [k("rhoT")], out=rhoT[:], in_=psb[1][:, 128:144], func=AF.Exp, scale=8.0)
        P.end_phase()
        pst.close()
        sbt = sbt_outer
        dvec = sbt("s5_d", [128, 4], F32)
        gb = sbt("s5_gb", [128, 4], F32)
        P.dma("sync", dvec[:], cd_d.rearrange("(c p) -> p c", p=128), writes=[k("d")], allow_slow_non_contiguous=True)
        P.dma("sync", gb[:], cd_glu_b.rearrange("(c p) -> p c", p=128), writes=[k("gb")], allow_slow_non_contiguous=True)
        gw = sbt("s5_gw", [128, 4, 512], BF16)
        P.dma("gpsimd", gw[:], cd_glu_w.rearrange("(c p) n -> p c n", p=128), writes=[k("gw")])
        NCH = T // 8
        iot = sbt("s5_iota", [128, NCH], F32)
        P.op("gpsimd", "iota", writes=[k("iota")], out=iot[:], pattern=[[1, NCH]], base=0,
             channel_multiplier=0, allow_small_or_imprecise_dtypes=True)
        yg = [sbt(f"s5_yg{c}", [128, T], BF16) for c in range(4)]
        g1 = [sbt(f"s5_g1{i}", [128, 512], F32) for i in range(2)]
        g2 = [sbt(f"s5_g2{i}", [128, 512], F32) for i in range(2)]
        mst = contextlib.ExitStack()

        def sbt(name, shape, dt):
            return mst.enter_context(nc.sbuf_tensor(name, list(shape), dt))
        u_f = [sbt(f"s5_uf{i}", [128, 512], F32) for i in range(2)]
        u_b = [sbt(f"s5_ub{i}", [128, 512], BF16) for i in range(2)]
        um = [sbt(f"s5_um{q}", [128, T], BF16) for q in range(4)]
        for q in range(4):
            P.op("gpsimd", "memset", writes=[f"s5_um{q}"], ap=um[q][:], constant=0.0)
        ang = [sbt(f"s5_ang{i}", [128, NCH], F32) for i in range(2)]
        nsn = [sbt(f"s5_ns{i}", [128, NCH], F32) for i in range(3)]
        ncs = [sbt(f"s5_nc{i}", [128, NCH], F32) for i in range(3)]
        rho_t = [sbt(f"s5_rho{i}", [128, NCH], F32) for i in range(3)]
        wre = [sbt(f"s5_wre{i}", [128, NCH], F32) for i in range(2)]
        wim = [sbt(f"s5_wim{i}", [128, NCH], F32) for i in range(2)]
        xre = sbt("s5_xre", [128, NCH], F32)
        xim = sbt("s5_xim", [128, NCH], F32)
        ta = sbt("s5_ta", [128, NCH], F32)
        tbb = sbt("s5_tbb", [128, NCH], F32)
        tc_ = sbt("s5_tc", [128, NCH], F32)
        Xa = [[[sbt(f"s5_X{c}{q}{ri}", [128, NCH], BF16) for ri in range(2)] for q in range(4)] for c in range(2)]
        for c in range(2):
            for q in range(4):
                for ri in range(2):
                    P.op("gpsimd", "memset", writes=[f"s5_X{c}{q}{ri}"], ap=Xa[c][q][ri][:], constant=0.0)
        u_rows = s5u.rearrange("(c p) t -> c p t", p=128)
        items = []
        for ct in range(4):
            for q in range(4):
                items.append(dict(ct=ct, q=q, pair=ct * 4 + q, idx=len(items)))

        def sA(it):
            ct, q, pair, p = it["ct"], it["q"], it["pair"], it["idx"]
            if q == 0:
                for qq in range(4):
                    P.dma("gpsimd", um[qq][32 * qq:32 * qq + 32, :], u_rows[ct][32 * qq:32 * qq + 32, :], reads=["s5u"],
                          writes=[f"s5_um{qq}"])
            sa, s3 = p % 2, p % 3
            vop("vector", "tensor_scalar", [k("iota"), k("phiT")], [f"s5_ang{sa}"], out=ang[sa][:], in0=iot[:],
                scalar1=phiT[:, pair:pair + 1], scalar2=MAGIC, op0=ALU.mult, op1=ALU.add)
            vop("vector", "tensor_scalar", [f"s5_ang{sa}"], [f"s5_ang{sa}"], out=ang[sa][:], in0=ang[sa][:], scalar1=-MAGIC,
                scalar2=None, op0=ALU.add)
            vop("vector", "scalar_tensor_tensor", [k("iota"), k("phiT"), f"s5_ang{sa}"], [f"s5_ang{sa}"], out=ang[sa][:],
                in0=iot[:], scalar=phiT[:, pair:pair + 1], in1=ang[sa][:], op0=ALU.mult, op1=ALU.subtract)
            vop("scalar", "activation", [f"s5_ang{sa}"], [f"s5_ns{s3}"], out=nsn[s3][:], in_=ang[sa][:], func=AF.Sin,
                scale=-SIN_SCALE)
            vop("vector", "scalar_tensor_tensor", [f"s5_ang{sa}"], [f"s5_ang{sa}"], out=ang[sa][:], in0=ang[sa][:], scalar=-1.0,
                in1=ang[sa][:], op0=ALU.mult, op1=ALU.max)
            vop("scalar", "activation", [f"s5_ang{sa}", k("negpi")], [f"s5_nc{s3}"], out=ncs[s3][:], in_=ang[sa][:],
                func=AF.Sin, scale=SIN_SCALE, bias=negpi[:])
            vop("scalar", "activation", [k("iota"), k("rhoT")], [f"s5_rho{s3}"], out=rho_t[s3][:], in_=iot[:],
                func=AF.Identity, scale=0.0, bias=rhoT[:, pair:pair + 1])

        def sB(it):
            ct, q, pair, p = it["ct"], it["q"], it["pair"], it["idx"]
            r, s3 = p % 2, p % 3
            pr, pm = 2 * r, 2 * r + 1
            for ri, pb in ((0, pr), (1, pm)):
                for j in range(8):
                    mm(pb, Ts[:, ct, j, ri, :], um[q][:, j:T:8], j == 0, j == 7, [k("Ts"), f"s5_um{q}"])
            vop("vector", "tensor_tensor", [f"psb{pr}", f"s5_nc{s3}"], [k("ta")], out=ta[:], in0=psb[pr][:], in1=ncs[s3][:], op=ALU.mult)
            vop("vector", "tensor_tensor", [f"psb{pm}", f"s5_ns{s3}"], [k("tbb")], out=tbb[:], in0=psb[pm][:], in1=nsn[s3][:], op=ALU.mult)
            vop("gpsimd", "tensor_tensor", [k("ta"), k("tbb")], [f"s5_wre{r}"], out=wre[r][:], in0=ta[:], in1=tbb[:], op=ALU.add)
            vop("vector", "tensor_tensor", [f"psb{pm}", f"s5_nc{s3}"], [k("tc")], out=tc_[:], in0=psb[pm][:], in1=ncs[s3][:], op=ALU.mult)
            vop("vector", "tensor_tensor", [f"psb{pr}", f"s5_ns{s3}"], [k("tbb")], out=tbb[:], in0=psb[pr][:], in1=nsn[s3][:], op=ALU.mult)
            vop("gpsimd", "tensor_tensor", [k("tc"), k("tbb")], [f"s5_wim{r}"], out=wim[r][:], in0=tc_[:], in1=tbb[:], op=ALU.subtract)

        def sC(it):
            ct, q, pair, p = it["ct"], it["q"], it["pair"], it["idx"]
            r, s3, xc = p % 2, p % 3, ct % 2
            vop("vector", "tensor_tensor_scan", [f"s5_rho{s3}", f"s5_wre{r}"], [k("xre")], out=xre[:],
                data0=rho_t[s3][:], data1=wre[r][:], initial=0.0, op0=ALU.mult, op1=ALU.add)
            vop("vector", "tensor_tensor_scan", [f"s5_rho{s3}", f"s5_wim{r}"], [k("xim")], out=xim[:],
                data0=rho_t[s3][:], data1=wim[r][:], initial=0.0, op0=ALU.mult, op1=ALU.add)
            n1 = NCH - 1
            vop("gpsimd", "tensor_tensor", [k("xre"), f"s5_nc{s3}"], [k("ta")], out=ta[:], in0=xre[:], in1=ncs[s3][:], op=ALU.mult)
            vop("vector", "tensor_tensor", [k("xim"), f"s5_ns{s3}"], [k("tc")], out=tc_[:], in0=xim[:], in1=nsn[s3][:], op=ALU.mult)
            vop("gpsimd", "tensor_tensor", [k("ta"), k("tc")], [f"s5_X{xc}{q}0"], out=Xa[xc][q][0][:, 1:NCH], in0=ta[:, 0:n1],
                in1=tc_[:, 0:n1], op=ALU.subtract)
            vop("vector", "tensor_tensor", [k("xre"), f"s5_ns{s3}"], [k("ta")], out=ta[:], in0=xre[:], in1=nsn[s3][:], op=ALU.mult)
            vop("gpsimd", "tensor_tensor", [k("xim"), f"s5_nc{s3}"], [k("tc")], out=tc_[:], in0=xim[:], in1=ncs[s3][:], op=ALU.mult)
            vop("vector", "tensor_tensor", [k("ta"), k("tc")], [f"s5_X{xc}{q}1"], out=Xa[xc][q][1][:, 1:NCH], in0=ta[:, 0:n1],
                in1=tc_[:, 0:n1], op=ALU.add)
            if q == 3:
                for tt in range(8):
                    t0 = tt * 512
                    py = 4 + (tt % 2)
                    us = tt % 2
                    P.dma("sync", u_f[us][:], u_rows[ct][:, t0:t0 + 512], reads=["s5u"], writes=[f"s5_uf{us}"])
                    P.dma("gpsimd", u_b[us][:], u_rows[ct][:, t0:t0 + 512], reads=["s5u"], writes=[f"s5_ub{us}"])
                    first = True
                    for tau in range(8):
                        for i in range(tau, 8):
                            mm(py, Kt[:, ct, tau, :], u_b[us][:, i - tau:512:8], first, False, [k("Kt"), f"s5_ub{us}"],
                               out=psb[py][:, i:512:8])
                            first = False
                    P.pe_drain()
                    for i in range(8):
                        for qq in range(4):
                            for ri in range(2):
                                P.op("tensor", "matmul", reads=[k("To"), f"s5_X{xc}{qq}{ri}"], writes=[f"psb{py}"],
                                     out=psb[py][32 * qq:32 * qq + 32, i:512:8], lhsT=To[:, ct * 4 + qq, i, ri, :],
                                     rhs=Xa[xc][qq][ri][:, tt * 64:(tt + 1) * 64], start=False,
                                     stop=(i == 7 and qq == 3 and ri == 1), tile_position=(0, 32 * qq))
                    P.pe_drain()
                    rr = tt % 2
                    vop("vector", "scalar_tensor_tensor", [f"s5_uf{us}", k("d"), f"psb{py}"], [f"s5_g1{rr}"], out=g1[rr][:],
                        in0=u_f[us][:], scalar=dvec[:, ct:ct + 1], in1=psb[py][:], op0=ALU.mult, op1=ALU.add)
                    vop("scalar", "activation", [f"s5_g1{rr}"], [f"s5_g2{rr}"], out=g2[rr][:], in_=g1[rr][:], func=AF.Square,
                        scale=math.sqrt(0.044715))
                    vop("vector", "scalar_tensor_tensor", [f"s5_g2{rr}", f"s5_g1{rr}"], [f"s5_g2{rr}"], out=g2[rr][:],
                        in0=g2[rr][:], scalar=1.0, in1=g1[rr][:], op0=ALU.add, op1=ALU.mult)
                    vop("scalar", "activation", [f"s5_g2{rr}"], [f"s5_g2{rr}"], out=g2[rr][:], in_=g2[rr][:], func=AF.Sigmoid,
                        scale=1.5957691216057308)
                    vop("vector", "tensor_tensor", [f"s5_g1{rr}", f"s5_g2{rr}"], [f"s5_yg{ct}"], out=yg[ct][:, t0:t0 + 512],
                        in0=g1[rr][:], in1=g2[rr][:], op=ALU.mult)

        sst = [sA, sB, sC]
        for step in range(len(items) + len(sst) - 1):
            for j in range(len(sst) - 1, -1, -1):
                t_ = step - j
                if 0 <= t_ < len(items):
                    sst[j](items[t_])
        P.end_phase()
        mst.close()
        sbt = sbt_outer
        yo_s = [sbt(f"s5_yo{i}", [128, 4, 512], BF16) for i in range(2)]
        for tt in range(8):
            cs = slice(tt * 512, (tt + 1) * 512)
            s_ = tt % 2
            for oc in range(4):
                pi = oc
                for ct in range(4):
                    mm(pi, gw[:, ct, oc * 128:(oc + 1) * 128], yg[ct][:, cs], ct == 0, ct == 3, [k("gw"), f"s5_yg{ct}"])
                r = oc % 2
                vop("scalar", "activation", [f"psb{pi}", k("gb")], [f"s5_g1{r}"], out=g1[r][:], in_=psb[pi][:], func=AF.Sigmoid,
                    bias=gb[:, oc:oc + 1])
                vop("vector", "tensor_tensor", [f"s5_g1{r}", f"s5_yg{oc}"], [f"s5_yo{s_}"], out=yo_s[s_][:, oc, :], in0=g1[r][:],
                    in1=yg[oc][:, cs], op=ALU.mult)
            P.dma("sync", ymix1.rearrange("(c p) t -> p c t", p=128)[:, 0:4, cs], yo_s[s_][:], reads=[f"s5_yo{s_}"],
                  writes=["ymix1"])
        P.end_phase()

    if upto <= 7:
        P.finish()
        return nc

    out_ffn(1)
    P.finish()
    return nc


INPUT_ORDER = ["x", "ab_norm", "ab_w_in", "ab_conv_w", "ab_conv_b", "ab_gate_a_w", "ab_gate_a_b",
               "ab_gate_x_w", "ab_gate_x_b", "ab_lambda", "ab_w_out", "cd_norm", "cd_w_in",
               "cd_lam_re", "cd_lam_im", "cd_log_dt", "cd_b_re", "cd_b_im", "cd_c_re", "cd_c_im",
               "cd_d", "cd_glu_w", "cd_glu_b", "cd_w_out", "ffn_norm", "ffn_w_gate", "ffn_w_up",
               "ffn_w_down", "final_norm"]


def make_in_maps(inputs, cores):
    f = lambda a: np.ascontiguousarray(np.asarray(a, dtype=np.float32))
    shared = {
        "ab_norm": f(inputs["ab_norm"][0]), "ab_w_in": f(inputs["ab_w_in"][0]),
        "ab_conv_w": f(inputs["ab_conv_w"][0, :, 0, :]), "ab_conv_b": f(inputs["ab_conv_b"][0]),
        "ab_gate_a_w": f(inputs["ab_gate_a_w"][0]), "ab_gate_a_b": f(inputs["ab_gate_a_b"][0].reshape(512)),
        "ab_gate_x_w": f(inputs["ab_gate_x_w"][0]), "ab_gate_x_b": f(inputs["ab_gate_x_b"][0].reshape(512)),
        "ab_lambda": f(inputs["ab_lambda"][0]), "ab_w_out": f(inputs["ab_w_out"][0]),
        "cd_norm": f(inputs["cd_norm"][0]), "cd_w_in": f(inputs["cd_w_in"][0]),
        "cd_lam_re": f(inputs["cd_lam_re"][0]), "cd_lam_im": f(inputs["cd_lam_im"][0]),
        "cd_log_dt": f(inputs["cd_log_dt"][0]), "cd_b_re": f(inputs["cd_b_re"][0]),
        "cd_b_im": f(inputs["cd_b_im"][0]), "cd_c_re": f(inputs["cd_c_re"][0]),
        "cd_c_im": f(inputs["cd_c_im"][0]), "cd_d": f(inputs["cd_d"][0]),
        "cd_glu_w": f(inputs["cd_glu_w"][0]), "cd_glu_b": f(inputs["cd_glu_b"][0]),
        "cd_w_out": f(inputs["cd_w_out"][0]), "ffn_norm": f(inputs["ffn_norm"]),
        "ffn_w_gate": f(inputs["ffn_w_gate"]), "ffn_w_up": f(inputs["ffn_w_up"]),
        "ffn_w_down": f(inputs["ffn_w_down"]), "final_norm": f(inputs["final_norm"]),
    }
    maps = []
    for b in cores:
        m = dict(shared)
        m["x"] = f(inputs["x"][b])
        maps.append(m)
    return maps


def kernel(**inputs):
    nc = build()
    in_maps = make_in_maps(inputs, list(range(8)))
    res = run_bass_kernel_spmd(nc, in_maps, core_ids=list(range(8)))
    return np.stack([np.asarray(r["y"]) for r in res.results], axis=0).astype(np.float32)
```

```python
import contextlib
import math
import numpy as np
import concourse.bass as bass
import concourse.mybir as mybir
from concourse.bass_utils import run_bass_kernel_spmd

F32 = mybir.dt.float32
BF16 = mybir.dt.bfloat16
AF = mybir.ActivationFunctionType
ALU = mybir.AluOpType
AX = mybir.AxisListType

T = 4096
D = 1024
NT = T // 512
FH = 2816
NJ = FH // 128
EPS = 1e-6


class Prog:
    ENG = ("tensor", "vector", "scalar", "gpsimd", "sync")

    def __init__(self, nc):
        self.nc = nc
        self.es = contextlib.ExitStack()
        self.ops = {e: [] for e in self.ENG}
        self.sems = {}
        self.cnt = {}
        self.seen = {e: {} for e in self.ENG}
        self.lastw = {}
        self.readers = {}
        self.block = None
        for e in self.ENG:
            self._sem("e_" + e)

    def _sem(self, name):
        if name not in self.sems:
            self.sems[name] = self.es.enter_context(self.nc.semaphore(name))
            self.cnt[name] = 0
        return name

    def sb(self, name, shape, dt):
        return self.es.enter_context(self.nc.sbuf_tensor(name, list(shape), dt))

    def ps(self, name, shape, dt=F32):
        return self.es.enter_context(self.nc.psum_tensor(name, list(shape), dt))

    def _deps(self, eng, reads, writes):
        need = {}

        def add(tok):
            if tok is not None:
                need[tok[0]] = max(need.get(tok[0], 0), tok[1])

        for k in reads:
            add(self.lastw.get(k))
        for k in writes:
            add(self.lastw.get(k))
            for s, v in self.readers.get(k, {}).items():
                add((s, v))
        own = "e_" + eng
        for s, v in need.items():
            if eng == "tensor" and s == own:
                continue
            if self.seen[eng].get(s, 0) < v:
                self.ops[eng].append(("wait", s, v))
                self.seen[eng][s] = v

    def _commit(self, tok, reads, writes):
        for k in writes:
            self.lastw[k] = tok
            self.readers[k] = {}
        for k in reads:
            r = self.readers.setdefault(k, {})
            r[tok[0]] = max(r.get(tok[0], 0), tok[1])

    def op(self, eng, meth, reads=(), writes=(), **kw):
        self._deps(eng, reads, writes)
        s = "e_" + eng
        self.cnt[s] += 1
        tok = (s, self.cnt[s])
        self.ops[eng].append(("op", meth, kw, s, 1))
        self._commit(tok, reads, writes)

    def dma(self, q, out, in_, reads=(), writes=(), semkey=None, **kw):
        self._deps(q, reads, writes)
        s = self._sem("d_" + (semkey or writes[0]))
        self.cnt[s] += 16
        tok = (s, self.cnt[s])
        kw = dict(kw)
        kw["out"] = out
        kw["in_"] = in_
        self.ops[q].append(("op", "dma_start", kw, s, 16))
        self._commit(tok, reads, writes)

    def pe_drain(self):
        c = self.cnt["e_tensor"]
        if c > 0:
            self.ops["tensor"].append(("wait", "e_tensor", c))

    def barrier(self):
        for eng in self.ENG:
            for s, c in self.cnt.items():
                if c > 0 and self.seen[eng].get(s, 0) < c and not (eng == "tensor" and s == "e_tensor"):
                    self.ops[eng].append(("wait", s, c))
                    self.seen[eng][s] = c

    def flush(self):
        if self.block is None:
            self.block = self.nc.Block()
            self.blk = self.block.__enter__()
        for eng in self.ENG:
            items = self.ops[eng]
            self.ops[eng] = []
            if not items:
                continue

            def body(e, items=items):
                for it in items:
                    if it[0] == "wait":
                        e.wait_ge(self.sems[it[1]], it[2])
                    else:
                        getattr(e, it[1])(**it[2]).then_inc(self.sems[it[3]], it[4])
            getattr(self.blk, eng)(body)

    def end_phase(self):
        self.barrier()
        self.flush()

    def finish(self):
        for s, c in self.cnt.items():
            if s.startswith("d_") and c > 0 and self.seen["sync"].get(s, 0) < c:
                self.ops["sync"].append(("wait", s, c))
                self.seen["sync"][s] = c
        self.flush()
        self.block.__exit__(None, None, None)
        self.es.close()


def build(dbg=(), upto=99):
    nc = bass.Bass("TRN2", target_bir_lowering=False)
    P = Prog(nc)

    def dram(name, shape, dt, kind=None):
        if kind is None:
            kind = "ExternalOutput" if name in dbg else "Internal"
        return nc.dram_tensor(name, list(shape), dt, kind=kind).ap()

    def ext(name, shape):
        return dram(name, shape, F32, kind="ExternalInput")

    x_in = ext("x", [T, D])
    ab_norm = ext("ab_norm", [D])
    ab_w_in = ext("ab_w_in", [D, 2560])
    ab_conv_w = ext("ab_conv_w", [4, 512])
    ab_conv_b = ext("ab_conv_b", [512])
    ab_gate_a_w = ext("ab_gate_a_w", [8, 64, 64])
    ab_gate_a_b = ext("ab_gate_a_b", [512])
    ab_gate_x_w = ext("ab_gate_x_w", [8, 64, 64])
    ab_gate_x_b = ext("ab_gate_x_b", [512])
    ab_lambda = ext("ab_lambda", [512])
    ab_w_out = ext("ab_w_out", [D, D])
    cd_norm = ext("cd_norm", [D])
    cd_w_in = ext("cd_w_in", [D, 2048])
    cd_lam_re = ext("cd_lam_re", [32, 64])
    cd_lam_im = ext("cd_lam_im", [32, 64])
    cd_log_dt = ext("cd_log_dt", [32])
    cd_b_re = ext("cd_b_re", [32, 64, 16])
    cd_b_im = ext("cd_b_im", [32, 64, 16])
    cd_c_re = ext("cd_c_re", [32, 16, 64])
    cd_c_im = ext("cd_c_im", [32, 16, 64])
    cd_d = ext("cd_d", [512])
    cd_glu_w = ext("cd_glu_w", [512, 512])
    cd_glu_b = ext("cd_glu_b", [512])
    cd_w_out = ext("cd_w_out", [D, D])
    ffn_norm = ext("ffn_norm", [2, D])
    ffn_w_gate = ext("ffn_w_gate", [2, D, FH])
    ffn_w_up = ext("ffn_w_up", [2, D, FH])
    ffn_w_down = ext("ffn_w_down", [2, FH, D])
    final_norm = ext("final_norm", [D])
    y_out = dram("y", [T, D], F32, kind="ExternalOutput")

    w_in0_bf = dram("w_in0_bf", [D, 2560], BF16)
    w_out0_bf = dram("w_out0_bf", [D, D], BF16)
    w_in1_bf = dram("w_in1_bf", [D, 2048], BF16)
    w_out1_bf = dram("w_out1_bf", [D, D], BF16)
    wg_bf = [dram(f"wg_bf{l}", [NJ, 128, 8, 128], BF16) for l in range(2)]
    wu_bf = [dram(f"wu_bf{l}", [NJ, 128, 8, 128], BF16) for l in range(2)]
    wd_bf = [dram(f"wd_bf{l}", [FH, D], BF16) for l in range(2)]
    xT = [dram(f"xT{i}", [D, T], F32) for i in range(5)]
    rgin = dram("rgin", [1024, T], F32)
    qk0 = dram("qk0", [1024, T], BF16)
    v0 = dram("v0", [T, 512], BF16)
    ymix0 = dram("ymix0", [1024, T], BF16)
    s5u = dram("s5u", [512, T], F32)
    qk1 = dram("qk1", [1024, T], BF16)
    v1 = dram("v1", [T, 512], BF16)
    ymix1 = dram("ymix1", [1024, T], BF16)
    s5wd = dram("s5wd", [16, 8, 2, 16, 128], BF16)

    ones_bf = P.sb("ones_bf", [128, 128], BF16)
    ones_f = P.sb("ones_f", [128, 128], F32)
    ident_f = P.sb("ident_f", [128, 128], F32)
    P.op("gpsimd", "memset", writes=["ones_bf"], ap=ones_bf[:], constant=1.0)
    P.op("gpsimd", "memset", writes=["ones_f"], ap=ones_f[:], constant=1.0)
    P.op("gpsimd", "affine_select", reads=["ones_f"], writes=["ident_f"],
         out=ident_f[:], in_=ones_f[:], pattern=[[-1, 128]], compare_op=ALU.is_equal, fill=0.0,
         base=0, channel_multiplier=1)
    eps_c = P.sb("eps_c", [128, 1], F32)
    P.op("gpsimd", "memset", writes=["eps_c"], ap=eps_c[:], constant=EPS)
    gains = P.sb("gains", [128, 5, 8], F32)
    for i, g in enumerate([ab_norm, ffn_norm[0], cd_norm, ffn_norm[1], final_norm]):
        P.dma("sync", gains[:, i, :], g.rearrange("(c p) -> p c", p=128), writes=["gains"],
              allow_slow_non_contiguous=True)

    psb = [P.ps(f"psb{i}", [128, 512]) for i in range(8)]

    def cast_copy(dst, src, key, nsplit, axis_rows):
        n = axis_rows // nsplit
        for i in range(nsplit):
            P.dma("gpsimd", dst[i * n:(i + 1) * n], src[i * n:(i + 1) * n], writes=[key])

    cast_copy(w_in0_bf, ab_w_in, "w_in0_bf", 8, D)
    cast_copy(w_out0_bf, ab_w_out, "w_out0_bf", 4, D)

    def cast_ffn(l):
        for j in range(NJ):
            P.dma("gpsimd", wg_bf[l][j], ffn_w_gate[l].rearrange("(c p) n -> p c n", p=128)[:, :, j * 128:(j + 1) * 128],
                  writes=[f"wg_bf{l}"])
            P.dma("gpsimd", wu_bf[l][j], ffn_w_up[l].rearrange("(c p) n -> p c n", p=128)[:, :, j * 128:(j + 1) * 128],
                  writes=[f"wu_bf{l}"])
        cast_copy(wd_bf[l], ffn_w_down[l], f"wd_bf{l}", 11, FH)

    cast_ffn(0)
    cast_copy(w_in1_bf, cd_w_in, "w_in1_bf", 8, D)
    cast_copy(w_out1_bf, cd_w_out, "w_out1_bf", 4, D)
    cast_ffn(1)

    evac_rr = [0]

    def evac(out_ap, in_ap, reads, writes):
        evac_rr[0] ^= 1
        if evac_rr[0]:
            P.op("scalar", "copy", reads=reads, writes=writes, out=out_ap, in_=in_ap)
        else:
            P.op("vector", "tensor_copy", reads=reads, writes=writes, out=out_ap, in_=in_ap)

    def mm(pi, lhsT, rhs, start, stop, reads, out=None):
        P.op("tensor", "matmul", reads=reads, writes=[f"psb{pi}"],
             out=(psb[pi][:] if out is None else out), lhsT=lhsT, rhs=rhs, start=start, stop=stop)

    sq_t = P.sb("sq_t", [128, 8, 512], BF16)
    rstd_t = P.sb("rstd_t", [128, 512], F32)

    def rmsnorm_T(xt, xkey, gi, out_t, okey):
        P.op("scalar", "activation", reads=[xkey], writes=["sq_t"], out=sq_t[:], in_=xt[:], func=AF.Square)
        for c in range(8):
            mm(0, ones_bf[:], sq_t[:, c, :], c == 0, c == 7, ["sq_t", "ones_bf"])
        P.op("scalar", "activation", reads=["psb0", "eps_c"], writes=["rstd_t"],
             out=rstd_t[:], in_=psb[0][:], func=AF.Ln, scale=1.0 / D, bias=eps_c[:])
        P.op("scalar", "activation", reads=["rstd_t"], writes=["rstd_t"],
             out=rstd_t[:], in_=rstd_t[:], func=AF.Exp, scale=-0.5)
        for c in range(8):
            P.op("vector", "scalar_tensor_tensor", reads=[xkey, "rstd_t", "gains"], writes=[okey],
                 out=out_t[:, c, :], in0=xt[:, c, :], scalar=gains[:, gi, c:c + 1], in1=rstd_t[:],
                 op0=ALU.mult, op1=ALU.mult)

    def inproj(layer):
        ncol = 2560 if layer == 0 else 2048
        nf = 8 if layer == 0 else 4
        wsrc = w_in0_bf if layer == 0 else w_in1_bf
        wkey = "w_in0_bf" if layer == 0 else "w_in1_bf"
        f_dst, f_key = (rgin, "rgin") if layer == 0 else (s5u, "s5u")
        qk_dst, qk_key = (qk0, "qk0") if layer == 0 else (qk1, "qk1")
        v_dst, v_key = (v0, "v0") if layer == 0 else (v1, "v1")
        gi = 0 if layer == 0 else 2
        with contextlib.ExitStack() as ph:
            w_in = ph.enter_context(nc.sbuf_tensor(f"w_in_a{layer}", [128, 8, ncol], BF16))
            hw = ncol // 2
            for hh in range(2):
                P.dma("sync", w_in[:, :, hh * hw:(hh + 1) * hw],
                      wsrc.rearrange("(c p) n -> p c n", p=128)[:, :, hh * hw:(hh + 1) * hw],
                      reads=[wkey], writes=["w_in_a"])
            if layer == 0:
                xtok = [ph.enter_context(nc.sbuf_tensor(f"xtok{i}", [128, 4, D], F32)) for i in range(2)]
            xt_a = [ph.enter_context(nc.sbuf_tensor(f"xt_a{layer}{i}", [128, 8, 512], F32)) for i in range(2)]
            h_a = [ph.enter_context(nc.sbuf_tensor(f"h_a{layer}{i}", [128, 8, 512], BF16)) for i in range(2)]
            st_rg = [ph.enter_context(nc.sbuf_tensor(f"st_rg{layer}{i}", [128, nf, 512], F32)) for i in range(2)]
            st_qk = [ph.enter_context(nc.sbuf_tensor(f"st_qk{layer}{i}", [128, 8, 512], BF16)) for i in range(2)]
            st_v = [ph.enter_context(nc.sbuf_tensor(f"st_v{layer}{i}", [128, 4, 512], BF16)) for i in range(2)]

            def load_norm(it):
                s = it % 2
                t0 = it * 512
                if layer == 0:
                    P.dma("sync", xtok[s][:], x_in[t0:t0 + 512, :].rearrange("(s p) d -> p s d", p=128),
                          writes=[f"xtok{s}"])
                    for c in range(8):
                        pi = 1 + (c % 2)
                        for sub in range(4):
                            P.op("tensor", "transpose", reads=[f"xtok{s}", "ident_f"], writes=[f"psb{pi}"],
                                 out=psb[pi][:, sub * 128:(sub + 1) * 128],
                                 in_=xtok[s][:, sub, c * 128:(c + 1) * 128], identity=ident_f[:])
                        evac(xt_a[s][:, c, :], psb[pi][:], [f"psb{pi}"], [f"xt_a{s}"])
                    P.dma("sync", xT[0].rearrange("(c p) t -> p c t", p=128)[:, :, t0:t0 + 512], xt_a[s][:],
                          reads=[f"xt_a{s}"], writes=["xT0"])
                else:
                    P.dma("sync", xt_a[s][:], xT[2].rearrange("(c p) t -> p c t", p=128)[:, :, t0:t0 + 512],
                          reads=["xT2"], writes=[f"xt_a{s}"])
                rmsnorm_T(xt_a[s], f"xt_a{s}", gi, h_a[s], f"h_a{s}")

            def project(it):
                s = it % 2
                t0 = it * 512
                hh, hk = h_a[s], f"h_a{s}"
                for m in range(nf + 8):
                    pi = 3 + (m % 5)
                    for k in range(8):
                        mm(pi, w_in[:, k, m * 128:(m + 1) * 128], hh[:, k, :], k == 0, k == 7, ["w_in_a", hk])
                    if m < nf:
                        evac(st_rg[s][:, m, :], psb[pi][:], [f"psb{pi}"], [f"st_rg{s}"])
                    elif layer == 1 and m < nf + 4:
                        P.op("vector", "tensor_scalar", reads=[f"psb{pi}"], writes=[f"st_qk{s}"],
                             out=st_qk[s][:, m - nf, :], in0=psb[pi][:], scalar1=128.0 ** -0.5, scalar2=None,
                             op0=ALU.mult)
                    else:
                        evac(st_qk[s][:, m - nf, :], psb[pi][:], [f"psb{pi}"], [f"st_qk{s}"])
                for sub in range(4):
                    pi = 3 + (sub % 5)
                    for k in range(8):
                        mm(pi, hh[:, k, sub * 128:(sub + 1) * 128], w_in[:, k, ncol - 512:ncol], k == 0, k == 7,
                           ["w_in_a", hk])
                    evac(st_v[s][:, sub, :], psb[pi][:], [f"psb{pi}"], [f"st_v{s}"])
                P.dma("sync", f_dst.rearrange("(c p) t -> p c t", p=128)[:, :, t0:t0 + 512], st_rg[s][:],
                      reads=[f"st_rg{s}"], writes=[f_key])
                P.dma("sync", qk_dst.rearrange("(c p) t -> p c t", p=128)[:, :, t0:t0 + 512], st_qk[s][:],
                      reads=[f"st_qk{s}"], writes=[qk_key])
                P.dma("sync", v_dst[t0:t0 + 512, :].rearrange("(s p) d -> p s d", p=128), st_v[s][:],
                      reads=[f"st_v{s}"], writes=[v_key])

            load_norm(0)
            for it in range(NT):
                if it + 1 < NT:
                    load_norm(it + 1)
                project(it)
            P.end_phase()

    def out_ffn(layer):
        wo_src, wo_key = (w_out0_bf, "w_out0_bf") if layer == 0 else (w_out1_bf, "w_out1_bf")
        ym_src, ym_key = (ymix0, "ymix0") if layer == 0 else (ymix1, "ymix1")
        x_src, x_key = xT[2 * layer], f"xT{2 * layer}"
        x_dst, x_dkey = xT[2 * layer + 2], f"xT{2 * layer + 2}"
        gi = 1 if layer == 0 else 3
        with contextlib.ExitStack() as ph:
            def sbt(name, shape, dt):
                return ph.enter_context(nc.sbuf_tensor(f"{name}_{layer}", list(shape), dt))
            wo = sbt("of_wo", [128, 8, D], BF16)
            wd = sbt("of_wd", [128, NJ, D], BF16)
            P.dma("sync", wo[:], wo_src.rearrange("(c p) n -> p c n", p=128), reads=[wo_key], writes=["of_wo"])
            for i in range(2):
                P.dma("sync", wd[:, i * 11:(i + 1) * 11, :],
                      wd_bf[layer].rearrange("(j p) n -> p j n", p=128)[:, i * 11:(i + 1) * 11, :],
                      reads=[f"wd_bf{layer}"], writes=["of_wd"])
            ym = [sbt(f"of_ym{i}", [128, 8, 512], BF16) for i in range(2)]
            xt = [sbt(f"of_x{i}", [128, 8, 512], F32) for i in range(2)]
            hT = sbt("of_h", [128, 8, 512], BF16)
            act = sbt("of_act", [128, NJ, 512], BF16)
            sg = [sbt(f"of_sg{i}", [128, 512], F32) for i in range(2)]
            wgu = [sbt(f"of_wgu{i}", [128, 2, 8, 128], BF16) for i in range(3)]
            if layer == 1:
                yo = sbt("of_yo", [128, 8, 512], F32)
                ytok = [sbt("of_ytok0", [128, 4, D], F32)] * 2
            nw = 0
            for it in range(NT):
                s = it % 2
                t0 = it * 512
                P.dma("sync", ym[s][:], ym_src.rearrange("(c p) t -> p c t", p=128)[:, :, t0:t0 + 512],
                      reads=[ym_key], writes=[f"of_ym{s}"])
                P.dma("sync", xt[s][:], x_src.rearrange("(c p) t -> p c t", p=128)[:, :, t0:t0 + 512],
                      reads=[x_key], writes=[f"of_x{s}"])
                for m in range(8):
                    pi = 1 + (m % 3)
                    for k in range(8):
                        mm(pi, wo[:, k, m * 128:(m + 1) * 128], ym[s][:, k, :], k == 0, k == 7,
                           ["of_wo", f"of_ym{s}"])
                    P.op("vector", "tensor_tensor", reads=[f"psb{pi}", f"of_x{s}"], writes=[f"of_x{s}"],
                         out=xt[s][:, m, :], in0=psb[pi][:], in1=xt[s][:, m, :], op=ALU.add)
                rmsnorm_T(xt[s], f"of_x{s}", gi, hT, "of_h")
                for j in range(NJ):
                    ws = nw % 3
                    nw += 1
                    P.dma("sync", wgu[ws][:, 0], wg_bf[layer][j], reads=[f"wg_bf{layer}"], writes=[f"of_wgu{ws}"])
                    P.dma("sync", wgu[ws][:, 1], wu_bf[layer][j], reads=[f"wu_bf{layer}"], writes=[f"of_wgu{ws}"])
                    pg, pu = 4 + 2 * (j % 2), 5 + 2 * (j % 2)
                    for k in range(8):
                        mm(pg, wgu[ws][:, 0, k, :], hT[:, k, :], k == 0, k == 7, [f"of_wgu{ws}", "of_h"])
                    for k in range(8):
                        mm(pu, wgu[ws][:, 1, k, :], hT[:, k, :], k == 0, k == 7, [f"of_wgu{ws}", "of_h"])
                    P.op("scalar", "activation", reads=[f"psb{pg}"], writes=[f"of_sg{j % 2}"], out=sg[j % 2][:],
                         in_=psb[pg][:], func=AF.Silu)
                    P.op("vector", "tensor_tensor", reads=[f"of_sg{j % 2}", f"psb{pu}"], writes=["of_act"],
                         out=act[:, j, :], in0=sg[j % 2][:], in1=psb[pu][:], op=ALU.mult)
                for m in range(8):
                    pi = 1 + (m % 3)
                    for j in range(NJ):
                        mm(pi, wd[:, j, m * 128:(m + 1) * 128], act[:, j, :], j == 0, j == NJ - 1,
                           ["of_wd", "of_act"])
                    P.op("vector", "tensor_tensor", reads=[f"psb{pi}", f"of_x{s}"], writes=[f"of_x{s}"],
                         out=xt[s][:, m, :], in0=psb[pi][:], in1=xt[s][:, m, :], op=ALU.add)
                if layer == 0 or "xT4" in dbg:
                    P.dma("sync", x_dst.rearrange("(c p) t -> p c t", p=128)[:, :, t0:t0 + 512], xt[s][:],
                          reads=[f"of_x{s}"], writes=[x_dkey])
                if layer == 1:
                    rmsnorm_T(xt[s], f"of_x{s}", 4, yo, "of_yo")
                    for sub in range(4):
                        for c in range(8):
                            pi = 1 + (sub % 3)
                            P.op("tensor", "transpose", reads=["of_yo", "ident_f"], writes=[f"psb{pi}"],
                                 out=psb[pi][:, (c % 4) * 128:(c % 4 + 1) * 128],
                                 in_=yo[:, c, sub * 128:(sub + 1) * 128], identity=ident_f[:])
                            if c % 4 == 3:
                                evac(ytok[s][:, sub, (c // 4) * 512:(c // 4 + 1) * 512], psb[pi][:], [f"psb{pi}"],
                                     ["of_ytok0"])
                                pi = 1 + ((sub + 1) % 3)
                    P.dma("sync", y_out[t0:t0 + 512, :].rearrange("(s p) d -> p s d", p=128), ytok[s][:],
                          reads=["of_ytok0"], writes=["y"])
            P.end_phase()

    inproj(0)

    if upto <= 1:
        P.finish()
        return nc

    with contextlib.ExitStack() as ph:
        def sbt(name, shape, dt):
            return ph.enter_context(nc.sbuf_tensor(name, list(shape), dt))
        convw = sbt("rg_convw", [128, 4, 4], F32)
        convb = sbt("rg_convb", [128, 4], F32)
        ba = sbt("rg_ba", [128, 4], F32)
        bx = sbt("rg_bx", [128, 4], F32)
        lam = sbt("rg_lam", [128, 4], F32)
        cvec = sbt("rg_cvec", [128, 4], F32)
        cvec2 = sbt("rg_cvec2", [128, 4], F32)
        wst = sbt("rg_wst", [128, 2, 4, 128], F32)
        wbd = sbt("rg_wbd", [128, 2, 4, 128], BF16)
        for j in range(4):
            P.dma("sync", convw[:, j, :], ab_conv_w[j].rearrange("(c p) -> p c", p=128), writes=["rg_convw"],
                  allow_slow_non_contiguous=True)
        for tl, src, key in ((convb, ab_conv_b, "rg_convb"), (ba, ab_gate_a_b, "rg_ba"),
                             (bx, ab_gate_x_b, "rg_bx"), (lam, ab_lambda, "rg_lam")):
            P.dma("sync", tl[:], src.rearrange("(c p) -> p c", p=128), writes=[key],
                  allow_slow_non_contiguous=True)
        P.op("gpsimd", "memset", writes=["rg_wst"], ap=wst[:], constant=0.0)
        for gi_, wsrc in enumerate((ab_gate_a_w, ab_gate_x_w)):
            for hd in range(8):
                cc, hl = hd // 2, hd % 2
                P.dma("sync", wst[hl * 64:(hl + 1) * 64, gi_, cc, hl * 64:(hl + 1) * 64], wsrc[hd],
                      writes=["rg_wst"])
        P.op("vector", "tensor_copy", reads=["rg_wst"], writes=["rg_wbd"], out=wbd[:], in_=wst[:])
        P.op("scalar", "activation", reads=["rg_lam"], writes=["rg_cvec"], out=cvec[:], in_=lam[:],
             func=AF.Exp, scale=-1.0)
        P.op("scalar", "activation", reads=["rg_cvec", "ones_f"], writes=["rg_cvec"], out=cvec[:], in_=cvec[:],
             func=AF.Ln, bias=ones_f[:, 0:1])
        P.op("vector", "tensor_scalar", reads=["rg_cvec"], writes=["rg_cvec2"], out=cvec2[:], in0=cvec[:],
             scalar1=-16.0, scalar2=None, op0=ALU.mult)
        P.op("vector", "tensor_scalar", reads=["rg_cvec"], writes=["rg_cvec"], out=cvec[:], in0=cvec[:],
             scalar1=-8.0, scalar2=None, op0=ALU.mult)
        B = [sbt(f"rgB{i}", [128, T], F32) for i in range(7)]
        xc_bf = sbt("rg_xcbf", [128, T], BF16)
        y_bf = sbt("rg_ybf", [128, T], BF16)
        rg_rows = rgin.rearrange("(c p) t -> c p t", p=128)
        ym_rows = ymix0.rearrange("(c p) t -> c p t", p=128)
        for cc in range(4):
            xr, gt, xc, rr, ii, a2, hh = B
            P.dma("sync", xr[:], rg_rows[cc], reads=["rgin"], writes=["rgB0"])
            P.dma("sync", gt[:], rg_rows[4 + cc], reads=["rgin"], writes=["rgB1"])
            P.op("vector", "tensor_scalar", reads=["rgB0", "rg_convw", "rg_convb"], writes=["rgB2"],
                 out=xc[:], in0=xr[:], scalar1=convw[:, 3, cc:cc + 1], scalar2=convb[:, cc:cc + 1],
                 op0=ALU.mult, op1=ALU.add)
            for j in (2, 1, 0):
                dl = 3 - j
                P.op("vector", "scalar_tensor_tensor", reads=["rgB0", "rgB2", "rg_convw"], writes=["rgB2"],
                     out=xc[:, dl:], in0=xr[:, 0:T - dl], scalar=convw[:, j, cc:cc + 1], in1=xc[:, dl:],
                     op0=ALU.mult, op1=ALU.add)
            P.op("gpsimd", "tensor_copy", reads=["rgB2"], writes=["rg_xcbf"], out=xc_bf[:], in_=xc[:])
            for tt in range(8):
                for gi_, (dst, dkey, bias) in enumerate(((rr, "rgB3", ba), (ii, "rgB4", bx))):
                    pi = (tt * 2 + gi_) % 4
                    mm(pi, wbd[:, gi_, cc, :], xc_bf[:, tt * 512:(tt + 1) * 512], True, True,
                       ["rg_wbd", "rg_xcbf"])
                    P.op("scalar", "activation", reads=[f"psb{pi}", "rg_ba", "rg_bx"], writes=[dkey],
                         out=dst[:, tt * 512:(tt + 1) * 512], in_=psb[pi][:], func=AF.Sigmoid,
                         bias=bias[:, cc:cc + 1])
            P.op("scalar", "activation", reads=["rgB3", "rg_cvec2"], writes=["rgB5"], out=a2[:], in_=rr[:],
                 func=AF.Exp, scale=cvec2[:, cc:cc + 1])
            P.op("scalar", "activation", reads=["rgB3", "rg_cvec"], writes=["rgB3"], out=rr[:], in_=rr[:],
                 func=AF.Exp, scale=cvec[:, cc:cc + 1])
            P.op("vector", "tensor_scalar", reads=["rgB5"], writes=["rgB5"], out=a2[:], in0=a2[:],
                 scalar1=-1.0, scalar2=1.0, op0=ALU.mult, op1=ALU.add)
            P.op("scalar", "activation", reads=["rgB5"], writes=["rgB5"], out=a2[:], in_=a2[:], func=AF.Sqrt)
            P.op("gpsimd", "tensor_tensor", reads=["rgB4", "rgB2"], writes=["rgB4"], out=ii[:], in0=ii[:],
                 in1=xc[:], op=ALU.mult)
            P.op("vector", "tensor_tensor", reads=["rgB4", "rgB5"], writes=["rgB4"], out=ii[:], in0=ii[:],
                 in1=a2[:], op=ALU.mult)
            P.op("vector", "tensor_tensor_scan", reads=["rgB3", "rgB4"], writes=["rgB6"], out=hh[:],
                 data0=rr[:], data1=ii[:], initial=0.0, op0=ALU.mult, op1=ALU.add)
            P.op("gpsimd", "tensor_tensor", reads=["rgB1"], writes=["rgB0"], out=xr[:], in0=gt[:], in1=gt[:],
                 op=ALU.mult)
            P.op("gpsimd", "tensor_scalar", reads=["rgB0"], writes=["rgB0"], out=xr[:], in0=xr[:],
                 scalar1=0.044715, scalar2=1.0, op0=ALU.mult, op1=ALU.add)
            P.op("gpsimd", "tensor_tensor", reads=["rgB0", "rgB1"], writes=["rgB0"], out=xr[:], in0=xr[:],
                 in1=gt[:], op=ALU.mult)
            P.op("scalar", "activation", reads=["rgB0"], writes=["rgB0"], out=xr[:], in_=xr[:],
                 func=AF.Sigmoid, scale=1.5957691216057308)
            P.op("vector", "tensor_tensor", reads=["rgB6", "rgB1"], writes=["rgB6"], out=hh[:], in0=hh[:],
                 in1=gt[:], op=ALU.mult)
            P.op("vector", "tensor_tensor", reads=["rgB6", "rgB0"], writes=["rg_ybf"], out=y_bf[:], in0=hh[:],
                 in1=xr[:], op=ALU.mult)
            P.dma("sync", ym_rows[cc], y_bf[:], reads=["rg_ybf"], writes=["ymix0"])
        P.end_phase()

    if upto <= 2:
        P.finish()
        return nc

    with contextlib.ExitStack() as ph:
        def sbt(name, shape, dt):
            return ph.enter_context(nc.sbuf_tensor(name, list(shape), dt))
        tri = sbt("sb_tri", [128, 128], BF16)
        mstr = sbt("sb_mstr", [128, 128], F32)
        P.op("gpsimd", "affine_select", reads=["ones_bf"], writes=["sb_tri"], out=tri[:], in_=ones_bf[:],
             pattern=[[-1, 128]], compare_op=ALU.is_ge, fill=0.0, base=0, channel_multiplier=1)
        P.op("gpsimd", "affine_select", reads=["ones_f"], writes=["sb_mstr"], out=mstr[:], in_=ones_f[:],
             pattern=[[1, 128]], compare_op=ALU.is_gt, fill=0.0, base=0, channel_multiplier=-1)
        v_all = sbt("sb_v", [128, 32, 512], BF16)
        v_src = v0.rearrange("(n p) d -> p n d", p=128)
        for i in range(4):
            P.dma("sync", v_all[:, i * 8:(i + 1) * 8, :], v_src[:, i * 8:(i + 1) * 8, :], reads=["v0"],
                  writes=["sb_v"])
        qT = [sbt(f"sb_q{i}", [128, T], BF16) for i in range(2)]
        kT = [sbt(f"sb_k{i}", [128, T], BF16) for i in range(2)]
        yst = [sbt(f"sb_y{i}", [128, T], BF16) for i in range(2)]
        for i in range(2):
            P.op("gpsimd", "memset", writes=[f"sb_q{i}"], ap=qT[i][:], constant=0.0)
            P.op("gpsimd", "memset", writes=[f"sb_k{i}"], ap=kT[i][:], constant=0.0)
        e_t = [sbt(f"sb_e{i}", [128, 512], F32) for i in range(3)]
        sp_t = [sbt(f"sb_sp{i}", [128, 512], BF16) for i in range(3)]
        en_t = [sbt(f"sb_en{i}", [128, 512], F32) for i in range(2)]
        w_t = [sbt(f"sb_w{i}", [128, 512], BF16) for i in range(2)]
        lacc_b = [sbt(f"sb_laccb{i}", [128, 512], BF16) for i in range(2)]
        qk_rows = qk0.rearrange("(h p) t -> h p t", p=64)
        ym128 = ymix0.rearrange("(h p) t -> h p t", p=128)
        items = []
        for hd in range(8):
            for qi in range(8):
                q0 = qi * 512
                kbs = list(range(q0 // 128 + 3, -1, -1))
                for bi, kb in enumerate(kbs):
                    items.append(dict(hd=hd, qi=qi, q0=q0, kb=kb, bi=bi, last=(kb == 0), idx=len(items)))

        def geom(it):
            c0 = max(0, it["kb"] * 128 - it["q0"])
            return c0, slice(c0, 512), slice(c0, c0 + 128), it["kb"] * 128 >= it["q0"]

        def st0(it):
            hd, hs, r = it["hd"], it["hd"] % 2, it["idx"] % 2
            if it["qi"] == 0 and it["bi"] == 0:
                P.dma("sync", qT[hs][0:64, :], qk_rows[hd], reads=["qk0"], writes=[f"sb_q{hs}"])
                P.dma("sync", kT[hs][0:64, :], qk_rows[8 + hd], reads=["qk0"], writes=[f"sb_k{hs}"])
            c0, cs, dg, diag = geom(it)
            mm(r, kT[hs][:, it["kb"] * 128:(it["kb"] + 1) * 128], qT[hs][:, it["q0"] + c0:it["q0"] + 512], True, True,
               [f"sb_k{hs}", f"sb_q{hs}"], out=psb[r][:, cs])

        def st1(it):
            r, r3 = it["idx"] % 2, it["idx"] % 3
            c0, cs, dg, diag = geom(it)
            P.op("scalar", "activation", reads=[f"psb{r}"], writes=[f"sb_e{r3}"], out=e_t[r3][:, cs],
                 in_=psb[r][:, cs], func=AF.Exp, scale=0.125)
            P.op("scalar", "activation", reads=[f"sb_e{r3}", "ones_f"], writes=[f"sb_sp{r3}"],
                 out=sp_t[r3][:, cs], in_=e_t[r3][:, cs], func=AF.Ln, bias=ones_f[:, 0:1])
            if diag:
                P.op("vector", "tensor_tensor", reads=[f"sb_sp{r3}", "sb_mstr"], writes=[f"sb_sp{r3}"],
                     out=sp_t[r3][:, dg], in0=sp_t[r3][:, dg], in1=mstr[:], op=ALU.mult)

        def st2(it):
            r, r3 = it["idx"] % 2, it["idx"] % 3
            lq = (it["hd"] * 8 + it["qi"]) % 2
            c0, cs, dg, diag = geom(it)
            pc = 2 + r
            if it["bi"] == 0:
                P.op("vector", "memset", writes=[f"sb_laccb{lq}"], ap=lacc_b[lq][:], constant=0.0)
            mm(pc, tri[:], sp_t[r3][:, cs], True, it["bi"] == 0, ["sb_tri", f"sb_sp{r3}"], out=psb[pc][:, cs])
            if it["bi"] > 0:
                mm(pc, ones_bf[:], lacc_b[lq][:, cs], False, True, ["ones_bf", f"sb_laccb{lq}"], out=psb[pc][:, cs])
            if not it["last"]:
                P.op("vector", "tensor_tensor", reads=[f"sb_laccb{lq}", f"sb_sp{r3}"], writes=[f"sb_laccb{lq}"],
                     out=lacc_b[lq][:, cs], in0=lacc_b[lq][:, cs], in1=sp_t[r3][:, cs], op=ALU.add)

        def st3(it):
            r, r3 = it["idx"] % 2, it["idx"] % 3
            c0, cs, dg, diag = geom(it)
            pc = 2 + r
            P.op("scalar", "activation", reads=[f"psb{pc}"], writes=[f"sb_en{r}"], out=en_t[r][:, cs],
                 in_=psb[pc][:, cs], func=AF.Exp, scale=-1.0)
            P.op("vector", "tensor_tensor", reads=[f"sb_e{r3}", f"sb_en{r}"], writes=[f"sb_w{r}"],
                 out=w_t[r][:, cs], in0=e_t[r3][:, cs], in1=en_t[r][:, cs], op=ALU.mult)
            if diag:
                P.op("vector", "tensor_tensor", reads=[f"sb_w{r}", "sb_mstr"], writes=[f"sb_w{r}"],
                     out=w_t[r][:, dg], in0=w_t[r][:, dg], in1=mstr[:], op=ALU.mult)

        def st4(it):
            hd, r = it["hd"], it["idx"] % 2
            c0, cs, dg, diag = geom(it)
            po = 4 + (it["qi"] % 2)
            ys = (hd // 2) % 2
            hr = slice((hd % 2) * 64, (hd % 2) * 64 + 64)
            mm(po, v_all[:, it["kb"], (hd // 2) * 128:(hd // 2) * 128 + 128], w_t[r][:, cs], it["bi"] == 0, it["last"],
               ["sb_v", f"sb_w{r}"], out=psb[po][:, cs])
            if it["last"]:
                evac(yst[ys][hr, it["q0"]:it["q0"] + 512], psb[po][hr, :], [f"psb{po}"], [f"sb_y{ys}"])
                if it["qi"] == 7 and hd % 2 == 1:
                    P.dma("sync", ym128[4 + hd // 2], yst[ys][:], reads=[f"sb_y{ys}"], writes=["ymix0"])

        stages = [st0, st1, st2, st3, st4]
        for step in range(len(items) + len(stages) - 1):
            for j in range(len(stages) - 1, -1, -1):
                t_ = step - j
                if 0 <= t_ < len(items):
                    stages[j](items[t_])
        P.end_phase()

    if upto <= 3:
        P.finish()
        return nc

    out_ffn(0)
    if upto <= 4:
        P.finish()
        return nc

    inproj(1)
    if upto <= 5:
        P.finish()
        return nc

    NEG = -30000.0
    with contextlib.ExitStack() as ph:
        def sbt(name, shape, dt):
            return ph.enter_context(nc.sbuf_tensor(name, list(shape), dt))
        slopes = [2.0 ** (-2.0 * (h + 1)) for h in range(4)]
        io33 = sbt("mb_io33", [128, 33], F32)
        pidx = sbt("mb_pidx", [128, 2], F32)
        biasT = sbt("mb_biasT", [128, 4, 33], F32)
        nsl = sbt("mb_nsl", [128, 4, 2], F32)
        P.op("gpsimd", "iota", writes=["mb_io33"], out=io33[:], pattern=[[-128, 33]], base=128,
             channel_multiplier=1, allow_small_or_imprecise_dtypes=True)
        P.op("gpsimd", "iota", writes=["mb_pidx"], out=pidx[:], pattern=[[128, 2]], base=0,
             channel_multiplier=1, allow_small_or_imprecise_dtypes=True)
        for h in range(4):
            P.op("vector", "tensor_scalar", reads=["mb_io33"], writes=["mb_biasT"], out=biasT[:, h, :],
                 in0=io33[:], scalar1=slopes[h], scalar2=None, op0=ALU.mult)
            P.op("vector", "tensor_scalar", reads=["mb_pidx"], writes=["mb_nsl"], out=nsl[:, h, :],
                 in0=pidx[:], scalar1=-slopes[h], scalar2=None, op0=ALU.mult)
        pastm = sbt("mb_pastm", [128, 32, 32], F32)
        P.op("gpsimd", "memset", writes=["mb_pastm"], ap=pastm[:], constant=0.0)
        pm4 = pastm[:].rearrange("p (b e) n -> p b e n", e=2)[:, :, :, 0:16]
        P.op("gpsimd", "affine_select", reads=["mb_pastm"], writes=["mb_pastm"], out=pm4, in_=pm4,
             pattern=[[1, 16], [0, 2], [-1, 16]], compare_op=ALU.is_gt, fill=NEG, base=0, channel_multiplier=0)
        efull = sbt("mb_efull", [128, 128, 128], BF16)
        P.op("gpsimd", "memset", writes=["mb_efull"], ap=efull[:], constant=1.0)
        P.op("gpsimd", "affine_select", reads=["mb_efull"], writes=["mb_efull"], out=efull[:], in_=efull[:],
             pattern=[[-1, 128], [0, 128]], compare_op=ALU.is_equal, fill=0.0, base=0, channel_multiplier=1)
        mc = sbt("mb_mc", [128, 128], F32)
        P.op("gpsimd", "affine_select", reads=["ones_f"], writes=["mb_mc"], out=mc[:], in_=ones_f[:],
             pattern=[[1, 128]], compare_op=ALU.is_ge, fill=0.0, base=0, channel_multiplier=-1)
        v_all = sbt("mb_v", [128, 32, 512], BF16)
        v_src = v1.rearrange("(n p) d -> p n d", p=128)
        for i in range(4):
            P.dma("sync", v_all[:, i * 8:(i + 1) * 8, :], v_src[:, i * 8:(i + 1) * 8, :], reads=["v1"],
                  writes=["mb_v"])
        qT = [sbt(f"mb_q{i}", [128, T], BF16) for i in range(2)]
        kT = [sbt(f"mb_k{i}", [128, T], BF16) for i in range(2)]
        yst = [sbt(f"mb_y{i}", [128, T], BF16) for i in range(2)]
        km_f = sbt("mb_kmf", [128, 16], F32)
        km_b = sbt("mb_kmb", [128, 16], BF16)
        gm = sbt("mb_gm", [128, 32, 32], F32)
        ng = sbt("mb_ng", [128, 32, 32], F32)
        m8 = sbt("mb_m8", [128, 32, 8], F32)
        rt4 = [sbt(f"mb_rt4{i}", [128, 8, 128], BF16) for i in range(2)]
        w_t = [sbt(f"mb_w{i}", [128, 256], BF16) for i in range(3)]
        rz = [sbt(f"mb_rz{i}", [128, 256], F32) for i in range(2)]
        P.op("gpsimd", "memset", writes=["mb_gm"], ap=gm[:], constant=0.0)
        P.op("gpsimd", "memset", writes=["mb_ng"], ap=ng[:], constant=0.0)
        qk_rows = qk1.rearrange("(h p) t -> h p t", p=128)
        ym128 = ymix1.rearrange("(h p) t -> h p t", p=128)
        def gating(hd):
            hs = hd % 2
            P.dma("sync", qT[hs][:], qk_rows[hd], reads=["qk1"], writes=[f"mb_q{hs}"])
            P.dma("sync", kT[hs][:], qk_rows[4 + hd], reads=["qk1"], writes=[f"mb_k{hs}"])
            P.op("vector", "tensor_reduce", reads=[f"mb_k{hs}"], writes=["mb_kmf"], out=km_f[:],
                 in_=kT[hs][:].rearrange("p (n k) -> p n k", k=256), axis=AX.X, op=ALU.add)
            P.op("vector", "tensor_scalar", reads=["mb_kmf"], writes=["mb_kmb"], out=km_b[:], in0=km_f[:],
                 scalar1=1.0 / 256.0, scalar2=None, op0=ALU.mult)
            for i in range(32):
                mm(6, qT[hs][:, i * 128:(i + 1) * 128], km_b[:], True, True, [f"mb_q{hs}", "mb_kmb"],
                   out=psb[6][:, i * 16:(i + 1) * 16])
            P.op("vector", "tensor_tensor", reads=["psb6", "mb_pastm"], writes=["mb_gm"], out=gm[:, :, 0:16],
                 in0=psb[6][:].rearrange("p (i n) -> p i n", n=16), in1=pastm[:, :, 0:16], op=ALU.add)
            for i in range(32):
                P.op("vector", "max", reads=["mb_gm"], writes=["mb_m8"], out=m8[:, i, :], in_=gm[:, i, 0:16])
            P.op("vector", "tensor_tensor", reads=["mb_gm", "mb_m8"], writes=["mb_ng"], out=ng[:, :, 0:16],
                 in0=gm[:, :, 0:16], in1=m8[:, :, 2:3].to_broadcast([128, 32, 16]), op=ALU.is_ge)
            P.op("vector", "tensor_scalar", reads=["mb_ng"], writes=["mb_ng"], out=ng[:, :, 0:16],
                 in0=ng[:, :, 0:16], scalar1=-1.0, scalar2=-NEG, op0=ALU.add, op1=ALU.mult)
            P.op("vector", "tensor_tensor", reads=["mb_ng", "mb_pastm"], writes=["mb_ng"], out=ng[:, :, 0:16],
                 in0=ng[:, :, 0:16], in1=pastm[:, :, 0:16], op=ALU.add)
            P.op("vector", "memset", writes=["mb_ng"], ap=ng[:, :, 16:17], constant=0.0)
            ng4 = ng[:].rearrange("p (b e) n -> p b e n", e=2)
            for e_ in range(2):
                P.op("vector", "tensor_scalar", reads=["mb_ng", "mb_nsl"], writes=["mb_ng"],
                     out=ng4[:, :, e_, 0:17], in0=ng4[:, :, e_, 0:17], scalar1=nsl[:, hd, e_:e_ + 1],
                     scalar2=None, op0=ALU.add)
            for g in range(8):
                pi = 6 + (g // 4)
                P.op("tensor", "transpose", reads=["mb_ng", "ident_f"], writes=[f"psb{pi}"],
                     out=psb[pi][:, (g % 4) * 128:(g % 4 + 1) * 128],
                     in_=ng[:, 4 * g:4 * g + 4, :].rearrange("p a n -> p (a n)"), identity=ident_f[:])
                if g % 4 == 3:
                    evac(rt4[hs][:, g - 3:g + 1, :].rearrange("p a t -> p (a t)"), psb[pi][:], [f"psb{pi}"],
                         [f"mb_rt4{hs}"])

        items = []
        for hd in range(4):
            for b in range(16):
                for kt in range(2 * b + 2):
                    items.append(dict(hd=hd, b=b, kt=kt, nkt=2 * b + 2, idx=len(items)))

        def mgeom(it):
            c0 = 128 if it["kt"] == 2 * it["b"] + 1 else 0
            return c0, slice(c0, 256), it["kt"] >= 2 * it["b"]

        def m0(it):
            hd, hs, b, kt, r = it["hd"], it["hd"] % 2, it["b"], it["kt"], it["idx"] % 2
            if b == 8 and kt == 0 and hd < 3:
                gating(hd + 1)
            c0, cs, own = mgeom(it)
            n_row = 16 if own else kt // 2
            mm(r, kT[hs][:, kt * 128:(kt + 1) * 128], qT[hs][:, b * 256 + c0:(b + 1) * 256], True, False,
               [f"mb_k{hs}", f"mb_q{hs}"], out=psb[r][:, cs])
            for e_ in range(c0 // 128, 2):
                il = 2 * (b % 2) + e_
                mm(r, efull[:, il * 32 + n_row, :], rt4[hs][:, b // 2, :], False, e_ == 1,
                   ["mb_efull", f"mb_rt4{hs}"], out=psb[r][:, e_ * 128:(e_ + 1) * 128])

        def m1(it):
            hd, b, kt, r, ws = it["hd"], it["b"], it["kt"], it["idx"] % 2, it["idx"] % 3
            c0, cs, own = mgeom(it)
            dd = 2 * b - kt + 1
            P.op("scalar", "activation", reads=[f"psb{r}", "mb_biasT"], writes=[f"mb_w{ws}"],
                 out=w_t[ws][:, cs], in_=psb[r][:, cs], func=AF.Exp, bias=biasT[:, hd, dd:dd + 1])
            if own:
                P.op("vector", "tensor_tensor", reads=[f"mb_w{ws}", "mb_mc"], writes=[f"mb_w{ws}"],
                     out=w_t[ws][:, c0:c0 + 128], in0=w_t[ws][:, c0:c0 + 128], in1=mc[:], op=ALU.mult)

        def m2(it):
            hd, hs, b, kt, ws = it["hd"], it["hd"] % 2, it["b"], it["kt"], it["idx"] % 3
            c0, cs, own = mgeom(it)
            po, pz = 2 + (b % 2), 4 + (b % 2)
            last = kt == it["nkt"] - 1
            mm(po, v_all[:, kt, hd * 128:(hd + 1) * 128], w_t[ws][:, cs], kt == 0, last,
               ["mb_v", f"mb_w{ws}"], out=psb[po][:, cs])
            mm(pz, ones_bf[:], w_t[ws][:, cs], kt == 0, last, ["ones_bf", f"mb_w{ws}"], out=psb[pz][:, cs])
            if last:
                P.op("vector", "reciprocal", reads=[f"psb{pz}"], writes=[f"mb_rz{b % 2}"], out=rz[b % 2][:],
                     in_=psb[pz][:, 0:256])
                P.op("vector", "tensor_tensor", reads=[f"psb{po}", f"mb_rz{b % 2}"], writes=[f"mb_y{hs}"],
                     out=yst[hs][:, b * 256:(b + 1) * 256], in0=psb[po][:, 0:256], in1=rz[b % 2][:], op=ALU.mult)
                if b == 15:
                    P.dma("sync", ym128[4 + hd], yst[hs][:], reads=[f"mb_y{hs}"], writes=["ymix1"])

        gating(0)
        mst = [m0, m1, m2]
        for step in range(len(items) + len(mst) - 1):
            for j in range(len(mst) - 1, -1, -1):
                t_ = step - j
                if 0 <= t_ < len(items):
                    mst[j](items[t_])
        P.end_phase()

    if upto <= 6:
        P.finish()
        return nc

    INV_2PI = 1.0 / (2.0 * math.pi)
    MAGIC = 12582912.0
    SIN_SCALE = 6.283185
    with contextlib.ExitStack() as ph:
        def sbt(name, shape, dt):
            return ph.enter_context(nc.sbuf_tensor(name, list(shape), dt))

        def vop(eng, meth, reads, writes, **kw):
            P.op(eng, meth, reads=reads, writes=writes, **kw)

        Ts = sbt("s5_Ts", [128, 4, 8, 2, 128], BF16)
        To = sbt("s5_To", [128, 16, 8, 2, 32], BF16)
        Kt = sbt("s5_Kt", [128, 4, 8, 128], BF16)
        phiT = sbt("s5_phiT", [128, 16], F32)
        rhoT = sbt("s5_rhoT", [128, 16], F32)
        negpi = sbt("s5_negpi", [128, 1], F32)
        pst = contextlib.ExitStack()
        sbt_outer = sbt

        def sbt(name, shape, dt):
            return pst.enter_context(nc.sbuf_tensor(name, list(shape), dt))
        lr = sbt("s5_lr", [128, 2, 64], F32)
        li = sbt("s5_li", [128, 2, 64], F32)
        ldt = sbt("s5_ldt", [128, 2], F32)
        bre = sbt("s5_bre", [128, 2, 64, 16], F32)
        bim = sbt("s5_bim", [128, 2, 64, 16], F32)
        cst = sbt("s5_cst", [128, 2, 16, 128], F32)
        for tl, key in ((lr, "s5_lr"), (li, "s5_li"), (ldt, "s5_ldt"), (bre, "s5_bre"), (bim, "s5_bim"),
                        (cst, "s5_cst")):
            P.op("gpsimd", "memset", writes=[key], ap=tl[:], constant=0.0)
        P.dma("sync", lr[0:16], cd_lam_re.rearrange("(a g) n -> a g n", g=2), writes=["s5_lr"])
        P.dma("sync", li[0:16], cd_lam_im.rearrange("(a g) n -> a g n", g=2), writes=["s5_li"])
        P.dma("sync", ldt[0:16], cd_log_dt.rearrange("(a g) -> a g", g=2), writes=["s5_ldt"])
        P.dma("sync", bre[0:16], cd_b_re.rearrange("(a g) n p -> a g n p", g=2), writes=["s5_bre"])
        P.dma("sync", bim[0:16], cd_b_im.rearrange("(a g) n p -> a g n p", g=2), writes=["s5_bim"])
        for ri_, csrc in ((0, cd_c_re), (1, cd_c_im)):
            for g_ in range(2):
                P.dma("sync", cst[0:16, ri_, :, g_ * 64:(g_ + 1) * 64],
                      csrc.rearrange("(a g) p n -> a g p n", g=2)[:, g_], writes=["s5_cst"])
        P.op("gpsimd", "memset", writes=["s5_negpi"], ap=negpi[:], constant=-0.5 * math.pi)
        dtt = sbt("s5_dt", [128, 2], F32)
        vop("scalar", "activation", ["s5_ldt"], ["s5_dt"], out=dtt[:], in_=ldt[:], func=AF.Exp)
        sm = {}
        for nm in ("lrdt", "lidt", "mag", "a1", "sinv", "cosv", "abre", "abim", "den", "t1", "t2", "fre", "fim"):
            sm[nm] = sbt("s5_" + nm, [128, 2, 64], F32)

        def k(nm):
            return "s5_" + nm
        dt_bc = dtt[:].unsqueeze(2).to_broadcast([128, 2, 64])
        vop("vector", "tensor_tensor", [k("lr"), k("dt")], [k("lrdt")], out=sm["lrdt"][:], in0=lr[:], in1=dt_bc, op=ALU.mult)
        vop("vector", "tensor_tensor", [k("li"), k("dt")], [k("lidt")], out=sm["lidt"][:], in0=li[:], in1=dt_bc, op=ALU.mult)
        vop("scalar", "activation", [k("lrdt")], [k("mag")], out=sm["mag"][:], in_=sm["lrdt"][:], func=AF.Exp)
        vop("vector", "tensor_scalar", [k("lidt")], [k("a1")], out=sm["a1"][:], in0=sm["lidt"][:], scalar1=INV_2PI,
            scalar2=MAGIC, op0=ALU.mult, op1=ALU.add)
        vop("vector", "tensor_scalar", [k("a1")], [k("a1")], out=sm["a1"][:], in0=sm["a1"][:], scalar1=-MAGIC,
            scalar2=None, op0=ALU.add)
        vop("vector", "scalar_tensor_tensor", [k("lidt"), k("a1")], [k("a1")], out=sm["a1"][:], in0=sm["lidt"][:],
            scalar=INV_2PI, in1=sm["a1"][:], op0=ALU.mult, op1=ALU.subtract)
        vop("scalar", "activation", [k("a1")], [k("sinv")], out=sm["sinv"][:], in_=sm["a1"][:], func=AF.Sin,
            scale=SIN_SCALE)
        vop("vector", "scalar_tensor_tensor", [k("a1")], [k("a1")], out=sm["a1"][:], in0=sm["a1"][:], scalar=-1.0,
            in1=sm["a1"][:], op0=ALU.mult, op1=ALU.max)
        vop("scalar", "activation", [k("a1"), k("negpi")], [k("cosv")], out=sm["cosv"][:], in_=sm["a1"][:],
            func=AF.Sin, scale=SIN_SCALE, bias=negpi[:])
        vop("vector", "scalar_tensor_tensor", [k("cosv"), k("mag")], [k("abre")], out=sm["abre"][:], in0=sm["cosv"][:],
            scalar=-1.0, in1=sm["mag"][:], op0=ALU.mult, op1=ALU.mult)
        vop("vector", "tensor_tensor", [k("sinv"), k("mag")], [k("abim")], out=sm["abim"][:], in0=sm["sinv"][:],
            in1=sm["mag"][:], op=ALU.mult)
        vop("vector", "tensor_tensor", [k("lr")], [k("den")], out=sm["den"][:], in0=lr[:], in1=lr[:], op=ALU.mult)
        vop("vector", "tensor_tensor", [k("li")], [k("t1")], out=sm["t1"][:], in0=li[:], in1=li[:], op=ALU.mult)
        vop("vector", "tensor_tensor", [k("den"), k("t1")], [k("den")], out=sm["den"][:], in0=sm["den"][:], in1=sm["t1"][:], op=ALU.add)
        vop("vector", "tensor_scalar", [k("den")], [k("den")], out=sm["den"][:], in0=sm["den"][:], scalar1=1e-30,
            scalar2=None, op0=ALU.max)
        vop("vector", "reciprocal", [k("den")], [k("den")], out=sm["den"][:], in_=sm["den"][:])
        vop("vector", "tensor_scalar", [k("abre")], [k("t2")], out=sm["t2"][:], in0=sm["abre"][:], scalar1=-1.0,
            scalar2=None, op0=ALU.add)
        vop("vector", "tensor_tensor", [k("t2"), k("lr")], [k("fre")], out=sm["fre"][:], in0=sm["t2"][:], in1=lr[:], op=ALU.mult)
        vop("vector", "tensor_tensor", [k("abim"), k("li")], [k("t1")], out=sm["t1"][:], in0=sm["abim"][:], in1=li[:], op=ALU.mult)
        vop("vector", "tensor_tensor", [k("fre"), k("t1")], [k("fre")], out=sm["fre"][:], in0=sm["fre"][:], in1=sm["t1"][:], op=ALU.add)
        vop("vector", "tensor_tensor", [k("fre"), k("den")], [k("fre")], out=sm["fre"][:], in0=sm["fre"][:], in1=sm["den"][:], op=ALU.mult)
        vop("vector", "tensor_tensor", [k("abim"), k("lr")], [k("fim")], out=sm["fim"][:], in0=sm["abim"][:], in1=lr[:], op=ALU.mult)
        vop("vector", "tensor_tensor", [k("t2"), k("li")], [k("t1")], out=sm["t1"][:], in0=sm["t2"][:], in1=li[:], op=ALU.mult)
        vop("vector", "tensor_tensor", [k("fim"), k("t1")], [k("fim")], out=sm["fim"][:], in0=sm["fim"][:], in1=sm["t1"][:], op=ALU.subtract)
        vop("vector", "tensor_tensor", [k("fim"), k("den")], [k("fim")], out=sm["fim"][:], in0=sm["fim"][:], in1=sm["den"][:], op=ALU.mult)
        bbre = sbt("s5_bbre", [128, 2, 64, 16], F32)
        bbim = sbt("s5_bbim", [128, 2, 64, 16], F32)
        tb1 = sbt("s5_tb1", [128, 2, 64, 16], F32)
        tb2 = sbt("s5_tb2", [128, 2, 64, 16], F32)
        fre_bc = sm["fre"][:].unsqueeze(3).to_broadcast([128, 2, 64, 16])
        fim_bc = sm["fim"][:].unsqueeze(3).to_broadcast([128, 2, 64, 16])
        vop("vector", "tensor_tensor", [k("bre"), k("fre")], [k("tb1")], out=tb1[:], in0=bre[:], in1=fre_bc, op=ALU.mult)
        vop("vector", "tensor_tensor", [k("bim"), k("fim")], [k("tb2")], out=tb2[:], in0=bim[:], in1=fim_bc, op=ALU.mult)
        vop("vector", "tensor_tensor", [k("tb1"), k("tb2")], [k("bbre")], out=bbre[:], in0=tb1[:], in1=tb2[:], op=ALU.subtract)
        vop("vector", "tensor_tensor", [k("bim"), k("fre")], [k("tb1")], out=tb1[:], in0=bim[:], in1=fre_bc, op=ALU.mult)
        vop("vector", "tensor_tensor", [k("bre"), k("fim")], [k("tb2")], out=tb2[:], in0=bre[:], in1=fim_bc, op=ALU.mult)
        vop("vector", "tensor_tensor", [k("tb1"), k("tb2")], [k("bbim")], out=bbim[:], in0=tb1[:], in1=tb2[:], op=ALU.add)
        pw = sbt("s5_pw", [128, 9, 2, 128], F32)
        pt1 = sbt("s5_pt1", [128, 128], F32)
        pt2 = sbt("s5_pt2", [128, 128], F32)
        P.op("gpsimd", "memset", writes=[k("pw")], ap=pw[:, 0, 0, :], constant=1.0)
        P.op("gpsimd", "memset", writes=[k("pw")], ap=pw[:, 0, 1, :], constant=0.0)
        abre_f = sm["abre"][:].rearrange("p a b -> p (a b)")
        abim_f = sm["abim"][:].rearrange("p a b -> p (a b)")
        for kk in range(1, 9):
            vop("vector", "tensor_tensor", [k("pw"), k("abre")], [k("pt1")], out=pt1[:], in0=pw[:, kk - 1, 0, :], in1=abre_f, op=ALU.mult)
            vop("vector", "tensor_tensor", [k("pw"), k("abim")], [k("pt2")], out=pt2[:], in0=pw[:, kk - 1, 1, :], in1=abim_f, op=ALU.mult)
            vop("vector", "tensor_tensor", [k("pt1"), k("pt2")], [k("pw")], out=pw[:, kk, 0, :], in0=pt1[:], in1=pt2[:], op=ALU.subtract)
            vop("vector", "tensor_tensor", [k("pw"), k("abim")], [k("pt1")], out=pt1[:], in0=pw[:, kk - 1, 0, :], in1=abim_f, op=ALU.mult)
            vop("vector", "tensor_tensor", [k("pw"), k("abre")], [k("pt2")], out=pt2[:], in0=pw[:, kk - 1, 1, :], in1=abre_f, op=ALU.mult)
            vop("vector", "tensor_tensor", [k("pt1"), k("pt2")], [k("pw")], out=pw[:, kk, 1, :], in0=pt1[:], in1=pt2[:], op=ALU.add)
        hm = sbt("s5_hm", [128, 2], F32)
        P.op("gpsimd", "memset", writes=[k("hm")], ap=hm[:], constant=0.0)
        P.op("gpsimd", "memset", writes=[k("hm")], ap=hm[0:64, 0:1], constant=1.0)
        P.op("gpsimd", "memset", writes=[k("hm")], ap=hm[64:128, 1:2], constant=1.0)
        Wall = [sbt(f"s5_Wall{i}", [128, 2, 16, 128], BF16) for i in range(2)]
        Wf = [sbt(f"s5_Wf{i}", [128, 2, 16, 128], F32) for i in range(2)]
        raw = [sbt(f"s5_raw{i}", [128, 2, 16, 16], F32) for i in range(2)]
        WTb = sbt("s5_WTb", [128, 16, 8, 2, 32], BF16)
        ToC = sbt("s5_ToC", [128, 16, 2, 32], BF16)
        ntr = [0]

        def transpose_mask(src, skey, dst_fn, dkey):
            rw = ntr[0] % 2
            ntr[0] += 1
            for ri in range(2):
                for p4 in range(4):
                    pi = 1 + (p4 % 2)
                    for pp in range(4):
                        P.op("tensor", "transpose", reads=[skey, "ident_f"], writes=[f"psb{pi}"],
                             out=psb[pi][:, pp * 128:(pp + 1) * 128], in_=src[:, ri, p4 * 4 + pp, :], identity=ident_f[:])
                    evac(raw[rw][:, ri, p4 * 4:(p4 + 1) * 4, :],
                         psb[pi][:].rearrange("p (a b) -> p a b", b=128)[:, :, 0:16], [f"psb{pi}"], [f"s5_raw{rw}"])
            for ri in range(2):
                for glp in range(2):
                    vop("vector", "tensor_scalar", [f"s5_raw{rw}", k("hm")], [dkey], out=dst_fn(ri, glp),
                        in0=raw[rw][:, ri, :, :].rearrange("p a b -> p b a"), scalar1=hm[:, glp:glp + 1], scalar2=None,
                        op0=ALU.mult)

        for kk in range(8):
            wf = Wf[kk % 2]
            wkey = f"s5_Wf{kk % 2}"
            pre_bc = pw[:, kk, 0, :].rearrange("p (g n) -> p g n", g=2).unsqueeze(3).to_broadcast([128, 2, 64, 16])
            pim_bc = pw[:, kk, 1, :].rearrange("p (g n) -> p g n", g=2).unsqueeze(3).to_broadcast([128, 2, 64, 16])
            vop("vector", "tensor_tensor", [k("bbre"), k("pw")], [k("tb1")], out=tb1[:], in0=bbre[:], in1=pre_bc, op=ALU.mult)
            vop("vector", "tensor_tensor", [k("bbim"), k("pw")], [k("tb2")], out=tb2[:], in0=bbim[:], in1=pim_bc, op=ALU.mult)
            vop("vector", "tensor_tensor", [k("tb1"), k("tb2")], [wkey], out=wf[:, 0].rearrange("a p (g n) -> a g n p", g=2),
                in0=tb1[:], in1=tb2[:], op=ALU.subtract)
            vop("vector", "tensor_tensor", [k("bbim"), k("pw")], [k("tb1")], out=tb1[:], in0=bbim[:], in1=pre_bc, op=ALU.mult)
            vop("vector", "tensor_tensor", [k("bbre"), k("pw")], [k("tb2")], out=tb2[:], in0=bbre[:], in1=pim_bc, op=ALU.mult)
            vop("vector", "tensor_tensor", [k("tb1"), k("tb2")], [wkey], out=wf[:, 1].rearrange("a p (g n) -> a g n p", g=2),
                in0=tb1[:], in1=tb2[:], op=ALU.add)
            vop("gpsimd", "tensor_copy", [wkey], [f"s5_Wall{kk % 2}"], out=Wall[kk % 2][:], in_=wf[:])
            P.dma("sync", s5wd[:, 7 - kk], Wall[kk % 2][0:16], reads=[f"s5_Wall{kk % 2}"], writes=["s5wd"])
            transpose_mask(wf, wkey, lambda ri, glp, kk=kk: WTb[:, :, kk, ri, glp * 16:(glp + 1) * 16], k("WTb"))
        P.op("gpsimd", "memset", writes=[k("Ts")], ap=Ts[:], constant=0.0)
        for pair in range(16):
            ct, q = pair // 4, pair % 4
            for gl in range(2):
                P.dma("sync" if (pair + gl) % 2 == 0 else "gpsimd",
                      Ts[32 * q + 16 * gl:32 * q + 16 * gl + 16, ct, :, :, gl * 64:(gl + 1) * 64].rearrange("p j r n -> p (j r) n"),
                      s5wd[pair, :, :, :, gl * 64:(gl + 1) * 64].rearrange("j r p n -> p (j r) n"),
                      reads=["s5wd"], writes=[k("Ts")])
        tcl = [t_[:].rearrange("a g n p -> a (g n p)").rearrange("a (x y) -> a x y", y=128) for t_ in (tb1, tb2)]
        for i in range(-1, 8):
            wf = Wf[i % 2]
            wkey = f"s5_Wf{i % 2}"
            pre_bc = pw[:, i + 1, 0, :].unsqueeze(1).to_broadcast([128, 16, 128])
            pim_bc = pw[:, i + 1, 1, :].unsqueeze(1).to_broadcast([128, 16, 128])
            vop("vector", "tensor_tensor", [k("cst"), k("pw")], [k("tb1")], out=tcl[0], in0=cst[:, 0], in1=pre_bc, op=ALU.mult)
            vop("vector", "tensor_tensor", [k("cst"), k("pw")], [k("tb2")], out=tcl[1], in0=cst[:, 1], in1=pim_bc, op=ALU.mult)
            vop("vector", "tensor_tensor", [k("tb1"), k("tb2")], [wkey], out=wf[:, 0], in0=tcl[0], in1=tcl[1], op=ALU.subtract)
            vop("vector", "tensor_tensor", [k("cst"), k("pw")], [k("tb1")], out=tcl[0], in0=cst[:, 0], in1=pim_bc, op=ALU.mult)
            vop("vector", "tensor_tensor", [k("cst"), k("pw")], [k("tb2")], out=tcl[1], in0=cst[:, 1], in1=pre_bc, op=ALU.mult)
            vop("vector", "scalar_tensor_tensor", [k("tb1"), k("tb2")], [wkey], out=wf[:, 1], in0=tcl[0], scalar=-1.0,
                in1=tcl[1], op0=ALU.mult, op1=ALU.subtract)
            if i < 0:
                transpose_mask(wf, wkey, lambda ri, glp: ToC[:, :, ri, glp * 16:(glp + 1) * 16], k("ToC"))
            else:
                transpose_mask(wf, wkey, lambda ri, glp, i=i: To[:, :, i, ri, glp * 16:(glp + 1) * 16], k("To"))
        P.op("gpsimd", "memset", writes=[k("Kt")], ap=Kt[:], constant=0.0)
        for ct in range(4):
            pk = 3 + (ct % 2)
            P.pe_drain()
            for q in range(4):
                pair = ct * 4 + q
                for tau in range(8):
                    for ri in range(2):
                        P.op("tensor", "matmul", reads=[k("WTb"), k("ToC")], writes=[f"psb{pk}"],
                             out=psb[pk][32 * q:32 * q + 32, tau * 32:(tau + 1) * 32], lhsT=WTb[:, pair, tau, ri, :],
                             rhs=ToC[:, pair, ri, :], start=(ri == 0), stop=(ri == 1), tile_position=(0, 32 * q))
            P.pe_drain()
            for q in range(4):
                evac(Kt[32 * q:32 * q + 32, ct, :, 32 * q:32 * q + 32],
                     psb[pk][32 * q:32 * q + 32, 0:256].rearrange("p (a b) -> p a b", b=32), [f"psb{pk}"], [k("Kt")])
        P.op("tensor", "transpose", reads=[k("lidt"), "ident_f"], writes=["psb1"], out=psb[1][:, 0:128],
             in_=sm["lidt"][:].rearrange("p a b -> p (a b)"), identity=ident_f[:])
        P.op("tensor", "transpose", reads=[k("lrdt"), "ident_f"], writes=["psb1"], out=psb[1][:, 128:256],
             in_=sm["lrdt"][:].rearrange("p a b -> p (a b)"), identity=ident_f[:])
        vop("vector", "tensor_scalar", ["psb1"], [k("phiT")], out=phiT[:], in0=psb[1][:, 0:16], scalar1=8.0 * INV_2PI,
            scalar2=None, op0=ALU.mult)
        vop("scalar", "activation", ["psb1"], [k("rhoT")], out=rhoT[:], in_=psb[1][:, 128:144], func=AF.Exp, scale=8.0)
        P.end_phase()
        pst.close()
        sbt = sbt_outer
        dvec = sbt("s5_d", [128, 4], F32)
        gb = sbt("s5_gb", [128, 4], F32)
        P.dma("sync", dvec[:], cd_d.rearrange("(c p) -> p c", p=128), writes=[k("d")], allow_slow_non_contiguous=True)
        P.dma("sync", gb[:], cd_glu_b.rearrange("(c p) -> p c", p=128), writes=[k("gb")], allow_slow_non_contiguous=True)
        gw = sbt("s5_gw", [128, 4, 512], BF16)
        P.dma("gpsimd", gw[:], cd_glu_w.rearrange("(c p) n -> p c n", p=128), writes=[k("gw")])
        NCH = T // 8
        iot = sbt("s5_iota", [128, NCH], F32)
        P.op("gpsimd", "iota", writes=[k("iota")], out=iot[:], pattern=[[1, NCH]], base=0,
             channel_multiplier=0, allow_small_or_imprecise_dtypes=True)
        yg = [sbt(f"s5_yg{c}", [128, T], BF16) for c in range(4)]
        g1 = [sbt(f"s5_g1{i}", [128, 512], F32) for i in range(2)]
        g2 = [sbt(f"s5_g2{i}", [128, 512], F32) for i in range(2)]
        mst = contextlib.ExitStack()

        def sbt(name, shape, dt):
            return mst.enter_context(nc.sbuf_tensor(name, list(shape), dt))
        u_b = [sbt(f"s5_ub{i}", [128, T], BF16) for i in range(2)]
        um = [sbt(f"s5_um{q}", [128, T], BF16) for q in range(4)]
        for q in range(4):
            P.op("gpsimd", "memset", writes=[f"s5_um{q}"], ap=um[q][:], constant=0.0)
        ang = [sbt(f"s5_ang{i}", [128, NCH], F32) for i in range(2)]
        nsn = [sbt(f"s5_ns{i}", [128, NCH], F32) for i in range(3)]
        ncs = [sbt(f"s5_nc{i}", [128, NCH], F32) for i in range(3)]
        rho_t = [sbt(f"s5_rho{i}", [128, NCH], F32) for i in range(3)]
        wre = [sbt(f"s5_wre{i}", [128, NCH], F32) for i in range(2)]
        wim = [sbt(f"s5_wim{i}", [128, NCH], F32) for i in range(2)]
        xre = sbt("s5_xre", [128, NCH], F32)
        xim = sbt("s5_xim", [128, NCH], F32)
        ta = sbt("s5_ta", [128, NCH], F32)
        tbb = sbt("s5_tbb", [128, NCH], F32)
        tc_ = sbt("s5_tc", [128, NCH], F32)
        Xa = [[[sbt(f"s5_X{c}{q}{ri}", [128, NCH], BF16) for ri in range(2)] for q in range(4)] for c in range(2)]
        for c in range(2):
            for q in range(4):
                for ri in range(2):
                    P.op("gpsimd", "memset", writes=[f"s5_X{c}{q}{ri}"], ap=Xa[c][q][ri][:], constant=0.0)
        u_rows = s5u.rearrange("(c p) t -> c p t", p=128)
        items = []
        for ct in range(4):
            for q in range(4):
                items.append(dict(ct=ct, q=q, pair=ct * 4 + q, idx=len(items)))

        def sA(it):
            ct, q, pair, p = it["ct"], it["q"], it["pair"], it["idx"]
            if q == 0:
                P.dma("gpsimd", u_b[ct % 2][:], u_rows[ct], reads=["s5u"], writes=[f"s5_ub{ct % 2}"])
                for qq in range(4):
                    P.dma("gpsimd", um[qq][32 * qq:32 * qq + 32, :], u_rows[ct][32 * qq:32 * qq + 32, :], reads=["s5u"],
                          writes=[f"s5_um{qq}"])
            sa, s3 = p % 2, p % 3
            vop("vector", "tensor_scalar", [k("iota"), k("phiT")], [f"s5_ang{sa}"], out=ang[sa][:], in0=iot[:],
                scalar1=phiT[:, pair:pair + 1], scalar2=MAGIC, op0=ALU.mult, op1=ALU.add)
            vop("vector", "tensor_scalar", [f"s5_ang{sa}"], [f"s5_ang{sa}"], out=ang[sa][:], in0=ang[sa][:], scalar1=-MAGIC,
                scalar2=None, op0=ALU.add)
            vop("vector", "scalar_tensor_tensor", [k("iota"), k("phiT"), f"s5_ang{sa}"], [f"s5_ang{sa}"], out=ang[sa][:],
                in0=iot[:], scalar=phiT[:, pair:pair + 1], in1=ang[sa][:], op0=ALU.mult, op1=ALU.subtract)
            vop("scalar", "activation", [f"s5_ang{sa}"], [f"s5_ns{s3}"], out=nsn[s3][:], in_=ang[sa][:], func=AF.Sin,
                scale=-SIN_SCALE)
            vop("vector", "scalar_tensor_tensor", [f"s5_ang{sa}"], [f"s5_ang{sa}"], out=ang[sa][:], in0=ang[sa][:], scalar=-1.0,
                in1=ang[sa][:], op0=ALU.mult, op1=ALU.max)
            vop("scalar", "activation", [f"s5_ang{sa}", k("negpi")], [f"s5_nc{s3}"], out=ncs[s3][:], in_=ang[sa][:],
                func=AF.Sin, scale=SIN_SCALE, bias=negpi[:])
            vop("scalar", "activation", [k("iota"), k("rhoT")], [f"s5_rho{s3}"], out=rho_t[s3][:], in_=iot[:],
                func=AF.Identity, scale=0.0, bias=rhoT[:, pair:pair + 1])

        def sB(it):
            ct, q, pair, p = it["ct"], it["q"], it["pair"], it["idx"]
            r, s3 = p % 2, p % 3
            pr, pm = 2 * r, 2 * r + 1
            for ri, pb in ((0, pr), (1, pm)):
                for j in range(8):
                    mm(pb, Ts[:, ct, j, ri, :], um[q][:, j:T:8], j == 0, j == 7, [k("Ts"), f"s5_um{q}"])
            vop("vector", "tensor_tensor", [f"psb{pr}", f"s5_nc{s3}"], [k("ta")], out=ta[:], in0=psb[pr][:], in1=ncs[s3][:], op=ALU.mult)
            vop("vector", "tensor_tensor", [f"psb{pm}", f"s5_ns{s3}"], [k("tbb")], out=tbb[:], in0=psb[pm][:], in1=nsn[s3][:], op=ALU.mult)
            vop("gpsimd", "tensor_tensor", [k("ta"), k("tbb")], [f"s5_wre{r}"], out=wre[r][:], in0=ta[:], in1=tbb[:], op=ALU.add)
            vop("vector", "tensor_tensor", [f"psb{pm}", f"s5_nc{s3}"], [k("tc")], out=tc_[:], in0=psb[pm][:], in1=ncs[s3][:], op=ALU.mult)
            vop("vector", "tensor_tensor", [f"psb{pr}", f"s5_ns{s3}"], [k("tbb")], out=tbb[:], in0=psb[pr][:], in1=nsn[s3][:], op=ALU.mult)
            vop("gpsimd", "tensor_tensor", [k("tc"), k("tbb")], [f"s5_wim{r}"], out=wim[r][:], in0=tc_[:], in1=tbb[:], op=ALU.subtract)

        def sC(it):
            ct, q, pair, p = it["ct"], it["q"], it["pair"], it["idx"]
            r, s3, xc = p % 2, p % 3, ct % 2
            vop("vector", "tensor_tensor_scan", [f"s5_rho{s3}", f"s5_wre{r}"], [k("xre")], out=xre[:],
                data0=rho_t[s3][:], data1=wre[r][:], initial=0.0, op0=ALU.mult, op1=ALU.add)
            vop("vector", "tensor_tensor_scan", [f"s5_rho{s3}", f"s5_wim{r}"], [k("xim")], out=xim[:],
                data0=rho_t[s3][:], data1=wim[r][:], initial=0.0, op0=ALU.mult, op1=ALU.add)
            n1 = NCH - 1
            vop("gpsimd", "tensor_tensor", [k("xre"), f"s5_nc{s3}"], [k("ta")], out=ta[:], in0=xre[:], in1=ncs[s3][:], op=ALU.mult)
            vop("vector", "tensor_tensor", [k("xim"), f"s5_ns{s3}"], [k("tc")], out=tc_[:], in0=xim[:], in1=nsn[s3][:], op=ALU.mult)
            vop("gpsimd", "tensor_tensor", [k("ta"), k("tc")], [f"s5_X{xc}{q}0"], out=Xa[xc][q][0][:, 1:NCH], in0=ta[:, 0:n1],
                in1=tc_[:, 0:n1], op=ALU.subtract)
            vop("vector", "tensor_tensor", [k("xre"), f"s5_ns{s3}"], [k("ta")], out=ta[:], in0=xre[:], in1=nsn[s3][:], op=ALU.mult)
            vop("gpsimd", "tensor_tensor", [k("xim"), f"s5_nc{s3}"], [k("tc")], out=tc_[:], in0=xim[:], in1=ncs[s3][:], op=ALU.mult)
            vop("vector", "tensor_tensor", [k("ta"), k("tc")], [f"s5_X{xc}{q}1"], out=Xa[xc][q][1][:, 1:NCH], in0=ta[:, 0:n1],
                in1=tc_[:, 0:n1], op=ALU.add)
            if q == 3:
                cb = ct % 2
                for half in range(2):
                    ilist = list(range(half * 4, half * 4 + 4))
                    for i in ilist:
                        py = 4 + (i % 4)
                        for tau in range(i + 1):
                            mm(py, Kt[:, ct, tau, :], u_b[cb][:, i - tau:T:8], tau == 0, False, [k("Kt"), f"s5_ub{cb}"])
                    P.pe_drain()
                    for i in ilist:
                        py = 4 + (i % 4)
                        for qq in range(4):
                            for ri in range(2):
                                P.op("tensor", "matmul", reads=[k("To"), f"s5_X{xc}{qq}{ri}"], writes=[f"psb{py}"],
                                     out=psb[py][32 * qq:32 * qq + 32, :], lhsT=To[:, ct * 4 + qq, i, ri, :],
                                     rhs=Xa[xc][qq][ri][:], start=False, stop=(qq == 3 and ri == 1),
                                     tile_position=(0, 32 * qq))
                    P.pe_drain()
                    for i in ilist:
                        py = 4 + (i % 4)
                        rr = i % 2
                        vop("vector", "scalar_tensor_tensor", [f"s5_ub{cb}", k("d"), f"psb{py}"], [f"s5_g1{rr}"], out=g1[rr][:],
                            in0=u_b[cb][:, i:T:8], scalar=dvec[:, ct:ct + 1], in1=psb[py][:], op0=ALU.mult, op1=ALU.add)
                        vop("scalar", "activation", [f"s5_g1{rr}"], [f"s5_g2{rr}"], out=g2[rr][:], in_=g1[rr][:], func=AF.Square,
                            scale=math.sqrt(0.044715))
                        vop("vector", "scalar_tensor_tensor", [f"s5_g2{rr}", f"s5_g1{rr}"], [f"s5_g2{rr}"], out=g2[rr][:],
                            in0=g2[rr][:], scalar=1.0, in1=g1[rr][:], op0=ALU.add, op1=ALU.mult)
                        vop("scalar", "activation", [f"s5_g2{rr}"], [f"s5_g2{rr}"], out=g2[rr][:], in_=g2[rr][:], func=AF.Sigmoid,
                            scale=1.5957691216057308)
                        vop("vector", "tensor_tensor", [f"s5_g1{rr}", f"s5_g2{rr}"], [f"s5_yg{ct}"], out=yg[ct][:, i:T:8],
                            in0=g1[rr][:], in1=g2[rr][:], op=ALU.mult)

        sst = [sA, sB, sC]
        for step in range(len(items) + len(sst) - 1):
            for j in range(len(sst) - 1, -1, -1):
                t_ = step - j
                if 0 <= t_ < len(items):
                    sst[j](items[t_])
        P.end_phase()
        mst.close()
        sbt = sbt_outer
        yo_s = [sbt(f"s5_yo{i}", [128, 4, 512], BF16) for i in range(2)]
        for tt in range(8):
            cs = slice(tt * 512, (tt + 1) * 512)
            s_ = tt % 2
            for oc in range(4):
                pi = oc
                for ct in range(4):
                    mm(pi, gw[:, ct, oc * 128:(oc + 1) * 128], yg[ct][:, cs], ct == 0, ct == 3, [k("gw"), f"s5_yg{ct}"])
                r = oc % 2
                vop("scalar", "activation", [f"psb{pi}", k("gb")], [f"s5_g1{r}"], out=g1[r][:], in_=psb[pi][:], func=AF.Sigmoid,
                    bias=gb[:, oc:oc + 1])
                vop("vector", "tensor_tensor", [f"s5_g1{r}", f"s5_yg{oc}"], [f"s5_yo{s_}"], out=yo_s[s_][:, oc, :], in0=g1[r][:],
                    in1=yg[oc][:, cs], op=ALU.mult)
            P.dma("sync", ymix1.rearrange("(c p) t -> p c t", p=128)[:, 0:4, cs], yo_s[s_][:], reads=[f"s5_yo{s_}"],
                  writes=["ymix1"])
        P.end_phase()

    if upto <= 7:
        P.finish()
        return nc

    out_ffn(1)
    P.finish()
    return nc


INPUT_ORDER = ["x", "ab_norm", "ab_w_in", "ab_conv_w", "ab_conv_b", "ab_gate_a_w", "ab_gate_a_b",
               "ab_gate_x_w", "ab_gate_x_b", "ab_lambda", "ab_w_out", "cd_norm", "cd_w_in",
               "cd_lam_re", "cd_lam_im", "cd_log_dt", "cd_b_re", "cd_b_im", "cd_c_re", "cd_c_im",
               "cd_d", "cd_glu_w", "cd_glu_b", "cd_w_out", "ffn_norm", "ffn_w_gate", "ffn_w_up",
               "ffn_w_down", "final_norm"]


def make_in_maps(inputs, cores):
    f = lambda a: np.ascontiguousarray(np.asarray(a, dtype=np.float32))
    shared = {
        "ab_norm": f(inputs["ab_norm"][0]), "ab_w_in": f(inputs["ab_w_in"][0]),
        "ab_conv_w": f(inputs["ab_conv_w"][0, :, 0, :]), "ab_conv_b": f(inputs["ab_conv_b"][0]),
        "ab_gate_a_w": f(inputs["ab_gate_a_w"][0]), "ab_gate_a_b": f(inputs["ab_gate_a_b"][0].reshape(512)),
        "ab_gate_x_w": f(inputs["ab_gate_x_w"][0]), "ab_gate_x_b": f(inputs["ab_gate_x_b"][0].reshape(512)),
        "ab_lambda": f(inputs["ab_lambda"][0]), "ab_w_out": f(inputs["ab_w_out"][0]),
        "cd_norm": f(inputs["cd_norm"][0]), "cd_w_in": f(inputs["cd_w_in"][0]),
        "cd_lam_re": f(inputs["cd_lam_re"][0]), "cd_lam_im": f(inputs["cd_lam_im"][0]),
        "cd_log_dt": f(inputs["cd_log_dt"][0]), "cd_b_re": f(inputs["cd_b_re"][0]),
        "cd_b_im": f(inputs["cd_b_im"][0]), "cd_c_re": f(inputs["cd_c_re"][0]),
        "cd_c_im": f(inputs["cd_c_im"][0]), "cd_d": f(inputs["cd_d"][0]),
        "cd_glu_w": f(inputs["cd_glu_w"][0]), "cd_glu_b": f(inputs["cd_glu_b"][0]),
        "cd_w_out": f(inputs["cd_w_out"][0]), "ffn_norm": f(inputs["ffn_norm"]),
        "ffn_w_gate": f(inputs["ffn_w_gate"]), "ffn_w_up": f(inputs["ffn_w_up"]),
        "ffn_w_down": f(inputs["ffn_w_down"]), "final_norm": f(inputs["final_norm"]),
    }
    maps = []
    for b in cores:
        m = dict(shared)
        m["x"] = f(inputs["x"][b])
        maps.append(m)
    return maps


def kernel(**inputs):
    nc = build()
    in_maps = make_in_maps(inputs, list(range(8)))
    res = run_bass_kernel_spmd(nc, in_maps, core_ids=list(range(8)))
    return np.stack([np.asarray(r["y"]) for r in res.results], axis=0).astype(np.float32)
```

```python
import contextlib
import math
import numpy as np
import concourse.bass as bass
import concourse.mybir as mybir
from concourse.bass_utils import run_bass_kernel_spmd

F32 = mybir.dt.float32
BF16 = mybir.dt.bfloat16
AF = mybir.ActivationFunctionType
ALU = mybir.AluOpType
AX = mybir.AxisListType

T = 4096
D = 1024
NT = T // 512
FH = 2816
NJ = FH // 128
EPS = 1e-6


class Prog:
    ENG = ("tensor", "vector", "scalar", "gpsimd", "sync")

    def __init__(self, nc):
        self.nc = nc
        self.es = contextlib.ExitStack()
        self.ops = {e: [] for e in self.ENG}
        self.sems = {}
        self.cnt = {}
        self.seen = {e: {} for e in self.ENG}
        self.lastw = {}
        self.readers = {}
        self.block = None
        for e in self.ENG:
            self._sem("e_" + e)

    def _sem(self, name):
        if name not in self.sems:
            self.sems[name] = self.es.enter_context(self.nc.semaphore(name))
            self.cnt[name] = 0
        return name

    def sb(self, name, shape, dt):
        return self.es.enter_context(self.nc.sbuf_tensor(name, list(shape), dt))

    def ps(self, name, shape, dt=F32):
        return self.es.enter_context(self.nc.psum_tensor(name, list(shape), dt))

    def _deps(self, eng, reads, writes):
        need = {}

        def add(tok):
            if tok is not None:
                need[tok[0]] = max(need.get(tok[0], 0), tok[1])

        for k in reads:
            add(self.lastw.get(k))
        for k in writes:
            add(self.lastw.get(k))
            for s, v in self.readers.get(k, {}).items():
                add((s, v))
        own = "e_" + eng
        for s, v in need.items():
            if eng == "tensor" and s == own:
                continue
            if self.seen[eng].get(s, 0) < v:
                self.ops[eng].append(("wait", s, v))
                self.seen[eng][s] = v

    def _commit(self, tok, reads, writes):
        for k in writes:
            self.lastw[k] = tok
            self.readers[k] = {}
        for k in reads:
            r = self.readers.setdefault(k, {})
            r[tok[0]] = max(r.get(tok[0], 0), tok[1])

    def op(self, eng, meth, reads=(), writes=(), **kw):
        self._deps(eng, reads, writes)
        s = "e_" + eng
        self.cnt[s] += 1
        tok = (s, self.cnt[s])
        self.ops[eng].append(("op", meth, kw, s, 1))
        self._commit(tok, reads, writes)

    def dma(self, q, out, in_, reads=(), writes=(), semkey=None, **kw):
        self._deps(q, reads, writes)
        s = self._sem("d_" + (semkey or writes[0]))
        self.cnt[s] += 16
        tok = (s, self.cnt[s])
        kw = dict(kw)
        kw["out"] = out
        kw["in_"] = in_
        self.ops[q].append(("op", "dma_start", kw, s, 16))
        self._commit(tok, reads, writes)

    def pe_drain(self):
        c = self.cnt["e_tensor"]
        if c > 0:
            self.ops["tensor"].append(("wait", "e_tensor", c))

    def barrier(self):
        for eng in self.ENG:
            for s, c in self.cnt.items():
                if c > 0 and self.seen[eng].get(s, 0) < c and not (eng == "tensor" and s == "e_tensor"):
                    self.ops[eng].append(("wait", s, c))
                    self.seen[eng][s] = c

    def flush(self):
        if self.block is None:
            self.block = self.nc.Block()
            self.blk = self.block.__enter__()
        for eng in self.ENG:
            items = self.ops[eng]
            self.ops[eng] = []
            if not items:
                continue

            def body(e, items=items):
                for it in items:
                    if it[0] == "wait":
                        e.wait_ge(self.sems[it[1]], it[2])
                    else:
                        getattr(e, it[1])(**it[2]).then_inc(self.sems[it[3]], it[4])
            getattr(self.blk, eng)(body)

    def end_phase(self):
        self.barrier()
        self.flush()

    def finish(self):
        for s, c in self.cnt.items():
            if s.startswith("d_") and c > 0 and self.seen["sync"].get(s, 0) < c:
                self.ops["sync"].append(("wait", s, c))
                self.seen["sync"][s] = c
        self.flush()
        self.block.__exit__(None, None, None)
        self.es.close()


def build(dbg=(), upto=99):
    nc = bass.Bass("TRN2", target_bir_lowering=False)
    P = Prog(nc)

    def dram(name, shape, dt, kind=None):
        if kind is None:
            kind = "ExternalOutput" if name in dbg else "Internal"
        return nc.dram_tensor(name, list(shape), dt, kind=kind).ap()

    def ext(name, shape):
        return dram(name, shape, F32, kind="ExternalInput")

    x_in = ext("x", [T, D])
    ab_norm = ext("ab_norm", [D])
    ab_w_in = ext("ab_w_in", [D, 2560])
    ab_conv_w = ext("ab_conv_w", [4, 512])
    ab_conv_b = ext("ab_conv_b", [512])
    ab_gate_a_w = ext("ab_gate_a_w", [8, 64, 64])
    ab_gate_a_b = ext("ab_gate_a_b", [512])
    ab_gate_x_w = ext("ab_gate_x_w", [8, 64, 64])
    ab_gate_x_b = ext("ab_gate_x_b", [512])
    ab_lambda = ext("ab_lambda", [512])
    ab_w_out = ext("ab_w_out", [D, D])
    cd_norm = ext("cd_norm", [D])
    cd_w_in = ext("cd_w_in", [D, 2048])
    cd_lam_re = ext("cd_lam_re", [32, 64])
    cd_lam_im = ext("cd_lam_im", [32, 64])
    cd_log_dt = ext("cd_log_dt", [32])
    cd_b_re = ext("cd_b_re", [32, 64, 16])
    cd_b_im = ext("cd_b_im", [32, 64, 16])
    cd_c_re = ext("cd_c_re", [32, 16, 64])
    cd_c_im = ext("cd_c_im", [32, 16, 64])
    cd_d = ext("cd_d", [512])
    cd_glu_w = ext("cd_glu_w", [512, 512])
    cd_glu_b = ext("cd_glu_b", [512])
    cd_w_out = ext("cd_w_out", [D, D])
    ffn_norm = ext("ffn_norm", [2, D])
    ffn_w_gate = ext("ffn_w_gate", [2, D, FH])
    ffn_w_up = ext("ffn_w_up", [2, D, FH])
    ffn_w_down = ext("ffn_w_down", [2, FH, D])
    final_norm = ext("final_norm", [D])
    y_out = dram("y", [T, D], F32, kind="ExternalOutput")

    w_in0_bf = dram("w_in0_bf", [D, 2560], BF16)
    w_out0_bf = dram("w_out0_bf", [D, D], BF16)
    w_in1_bf = dram("w_in1_bf", [D, 2048], BF16)
    w_out1_bf = dram("w_out1_bf", [D, D], BF16)
    wg_bf = [dram(f"wg_bf{l}", [NJ, 128, 8, 128], BF16) for l in range(2)]
    wu_bf = [dram(f"wu_bf{l}", [NJ, 128, 8, 128], BF16) for l in range(2)]
    wd_bf = [dram(f"wd_bf{l}", [FH, D], BF16) for l in range(2)]
    xT = [dram(f"xT{i}", [D, T], F32) for i in range(5)]
    rgin = dram("rgin", [1024, T], F32)
    qk0 = dram("qk0", [1024, T], BF16)
    v0 = dram("v0", [T, 512], BF16)
    ymix0 = dram("ymix0", [1024, T], BF16)
    s5u = dram("s5u", [512, T], F32)
    qk1 = dram("qk1", [1024, T], BF16)
    v1 = dram("v1", [T, 512], BF16)
    ymix1 = dram("ymix1", [1024, T], BF16)
    s5wd = dram("s5wd", [16, 8, 2, 16, 128], BF16)

    ones_bf = P.sb("ones_bf", [128, 128], BF16)
    ones_f = P.sb("ones_f", [128, 128], F32)
    ident_f = P.sb("ident_f", [128, 128], F32)
    P.op("gpsimd", "memset", writes=["ones_bf"], ap=ones_bf[:], constant=1.0)
    P.op("gpsimd", "memset", writes=["ones_f"], ap=ones_f[:], constant=1.0)
    P.op("gpsimd", "affine_select", reads=["ones_f"], writes=["ident_f"],
         out=ident_f[:], in_=ones_f[:], pattern=[[-1, 128]], compare_op=ALU.is_equal, fill=0.0,
         base=0, channel_multiplier=1)
    eps_c = P.sb("eps_c", [128, 1], F32)
    P.op("gpsimd", "memset", writes=["eps_c"], ap=eps_c[:], constant=EPS)
    gains = P.sb("gains", [128, 5, 8], F32)
    for i, g in enumerate([ab_norm, ffn_norm[0], cd_norm, ffn_norm[1], final_norm]):
        P.dma("sync", gains[:, i, :], g.rearrange("(c p) -> p c", p=128), writes=["gains"],
              allow_slow_non_contiguous=True)

    psb = [P.ps(f"psb{i}", [128, 512]) for i in range(8)]

    def cast_copy(dst, src, key, nsplit, axis_rows):
        n = axis_rows // nsplit
        for i in range(nsplit):
            P.dma("gpsimd", dst[i * n:(i + 1) * n], src[i * n:(i + 1) * n], writes=[key])

    cast_copy(w_in0_bf, ab_w_in, "w_in0_bf", 8, D)
    cast_copy(w_out0_bf, ab_w_out, "w_out0_bf", 4, D)

    def cast_ffn(l):
        for j in range(NJ):
            P.dma("gpsimd", wg_bf[l][j], ffn_w_gate[l].rearrange("(c p) n -> p c n", p=128)[:, :, j * 128:(j + 1) * 128],
                  writes=[f"wg_bf{l}"])
            P.dma("gpsimd", wu_bf[l][j], ffn_w_up[l].rearrange("(c p) n -> p c n", p=128)[:, :, j * 128:(j + 1) * 128],
                  writes=[f"wu_bf{l}"])
        cast_copy(wd_bf[l], ffn_w_down[l], f"wd_bf{l}", 11, FH)

    cast_ffn(0)
    cast_copy(w_in1_bf, cd_w_in, "w_in1_bf", 8, D)
    cast_copy(w_out1_bf, cd_w_out, "w_out1_bf", 4, D)
    cast_ffn(1)

    evac_rr = [0]

    def evac(out_ap, in_ap, reads, writes):
        evac_rr[0] ^= 1
        if evac_rr[0]:
            P.op("scalar", "copy", reads=reads, writes=writes, out=out_ap, in_=in_ap)
        else:
            P.op("vector", "tensor_copy", reads=reads, writes=writes, out=out_ap, in_=in_ap)

    def mm(pi, lhsT, rhs, start, stop, reads, out=None):
        P.op("tensor", "matmul", reads=reads, writes=[f"psb{pi}"],
             out=(psb[pi][:] if out is None else out), lhsT=lhsT, rhs=rhs, start=start, stop=stop,
             skip_group_check=True)

    sq_t = P.sb("sq_t", [128, 8, 512], BF16)
    rstd_t = P.sb("rstd_t", [128, 512], F32)

    def rmsnorm_T(xt, xkey, gi, out_t, okey):
        P.op("scalar", "activation", reads=[xkey], writes=["sq_t"], out=sq_t[:], in_=xt[:], func=AF.Square)
        for c in range(8):
            mm(0, ones_bf[:], sq_t[:, c, :], c == 0, c == 7, ["sq_t", "ones_bf"])
        P.op("scalar", "activation", reads=["psb0", "eps_c"], writes=["rstd_t"],
             out=rstd_t[:], in_=psb[0][:], func=AF.Ln, scale=1.0 / D, bias=eps_c[:])
        P.op("scalar", "activation", reads=["rstd_t"], writes=["rstd_t"],
             out=rstd_t[:], in_=rstd_t[:], func=AF.Exp, scale=-0.5)
        for c in range(8):
            P.op("vector", "scalar_tensor_tensor", reads=[xkey, "rstd_t", "gains"], writes=[okey],
                 out=out_t[:, c, :], in0=xt[:, c, :], scalar=gains[:, gi, c:c + 1], in1=rstd_t[:],
                 op0=ALU.mult, op1=ALU.mult)

    def inproj(layer):
        ncol = 2560 if layer == 0 else 2048
        nf = 8 if layer == 0 else 4
        wsrc = w_in0_bf if layer == 0 else w_in1_bf
        wkey = "w_in0_bf" if layer == 0 else "w_in1_bf"
        f_dst, f_key = (rgin, "rgin") if layer == 0 else (s5u, "s5u")
        qk_dst, qk_key = (qk0, "qk0") if layer == 0 else (qk1, "qk1")
        v_dst, v_key = (v0, "v0") if layer == 0 else (v1, "v1")
        gi = 0 if layer == 0 else 2
        with contextlib.ExitStack() as ph:
            w_in = ph.enter_context(nc.sbuf_tensor(f"w_in_a{layer}", [128, 8, ncol], BF16))
            hw = ncol // 2
            for hh in range(2):
                P.dma("sync", w_in[:, :, hh * hw:(hh + 1) * hw],
                      wsrc.rearrange("(c p) n -> p c n", p=128)[:, :, hh * hw:(hh + 1) * hw],
                      reads=[wkey], writes=["w_in_a"])
            if layer == 0:
                xtok = [ph.enter_context(nc.sbuf_tensor(f"xtok{i}", [128, 4, D], F32)) for i in range(2)]
            xt_a = [ph.enter_context(nc.sbuf_tensor(f"xt_a{layer}{i}", [128, 8, 512], F32)) for i in range(2)]
            h_a = [ph.enter_context(nc.sbuf_tensor(f"h_a{layer}{i}", [128, 8, 512], BF16)) for i in range(2)]
            st_rg = [ph.enter_context(nc.sbuf_tensor(f"st_rg{layer}{i}", [128, nf, 512], F32)) for i in range(2)]
            st_qk = [ph.enter_context(nc.sbuf_tensor(f"st_qk{layer}{i}", [128, 8, 512], BF16)) for i in range(2)]
            st_v = [ph.enter_context(nc.sbuf_tensor(f"st_v{layer}{i}", [128, 4, 512], BF16)) for i in range(2)]

            def load_norm(it):
                s = it % 2
                t0 = it * 512
                if layer == 0:
                    P.dma("sync", xtok[s][:], x_in[t0:t0 + 512, :].rearrange("(s p) d -> p s d", p=128),
                          writes=[f"xtok{s}"])
                    for c in range(8):
                        pi = 1 + (c % 2)
                        for sub in range(4):
                            P.op("tensor", "transpose", reads=[f"xtok{s}", "ident_f"], writes=[f"psb{pi}"],
                                 out=psb[pi][:, sub * 128:(sub + 1) * 128],
                                 in_=xtok[s][:, sub, c * 128:(c + 1) * 128], identity=ident_f[:])
                        evac(xt_a[s][:, c, :], psb[pi][:], [f"psb{pi}"], [f"xt_a{s}"])
                    P.dma("sync", xT[0].rearrange("(c p) t -> p c t", p=128)[:, :, t0:t0 + 512], xt_a[s][:],
                          reads=[f"xt_a{s}"], writes=["xT0"])
                else:
                    P.dma("sync", xt_a[s][:], xT[2].rearrange("(c p) t -> p c t", p=128)[:, :, t0:t0 + 512],
                          reads=["xT2"], writes=[f"xt_a{s}"])
                rmsnorm_T(xt_a[s], f"xt_a{s}", gi, h_a[s], f"h_a{s}")

            def project(it):
                s = it % 2
                t0 = it * 512
                hh, hk = h_a[s], f"h_a{s}"
                for m in range(nf + 8):
                    pi = 3 + (m % 5)
                    for k in range(8):
                        mm(pi, w_in[:, k, m * 128:(m + 1) * 128], hh[:, k, :], k == 0, k == 7, ["w_in_a", hk])
                    if m < nf:
                        evac(st_rg[s][:, m, :], psb[pi][:], [f"psb{pi}"], [f"st_rg{s}"])
                    elif layer == 1 and m < nf + 4:
                        P.op("vector", "tensor_scalar", reads=[f"psb{pi}"], writes=[f"st_qk{s}"],
                             out=st_qk[s][:, m - nf, :], in0=psb[pi][:], scalar1=128.0 ** -0.5, scalar2=None,
                             op0=ALU.mult)
                    else:
                        evac(st_qk[s][:, m - nf, :], psb[pi][:], [f"psb{pi}"], [f"st_qk{s}"])
                for sub in range(4):
                    pi = 3 + (sub % 5)
                    for k in range(8):
                        mm(pi, hh[:, k, sub * 128:(sub + 1) * 128], w_in[:, k, ncol - 512:ncol], k == 0, k == 7,
                           ["w_in_a", hk])
                    evac(st_v[s][:, sub, :], psb[pi][:], [f"psb{pi}"], [f"st_v{s}"])
                P.dma("sync", f_dst.rearrange("(c p) t -> p c t", p=128)[:, :, t0:t0 + 512], st_rg[s][:],
                      reads=[f"st_rg{s}"], writes=[f_key])
                P.dma("sync", qk_dst.rearrange("(c p) t -> p c t", p=128)[:, :, t0:t0 + 512], st_qk[s][:],
                      reads=[f"st_qk{s}"], writes=[qk_key])
                P.dma("sync", v_dst[t0:t0 + 512, :].rearrange("(s p) d -> p s d", p=128), st_v[s][:],
                      reads=[f"st_v{s}"], writes=[v_key])

            load_norm(0)
            for it in range(NT):
                if it + 1 < NT:
                    load_norm(it + 1)
                project(it)
            P.end_phase()

    def out_ffn(layer):
        wo_src, wo_key = (w_out0_bf, "w_out0_bf") if layer == 0 else (w_out1_bf, "w_out1_bf")
        ym_src, ym_key = (ymix0, "ymix0") if layer == 0 else (ymix1, "ymix1")
        x_src, x_key = xT[2 * layer], f"xT{2 * layer}"
        x_dst, x_dkey = xT[2 * layer + 2], f"xT{2 * layer + 2}"
        gi = 1 if layer == 0 else 3
        with contextlib.ExitStack() as ph:
            def sbt(name, shape, dt):
                return ph.enter_context(nc.sbuf_tensor(f"{name}_{layer}", list(shape), dt))
            wo = sbt("of_wo", [128, 8, D], BF16)
            wd = sbt("of_wd", [128, NJ, D], BF16)
            P.dma("sync", wo[:], wo_src.rearrange("(c p) n -> p c n", p=128), reads=[wo_key], writes=["of_wo"])
            for i in range(2):
                P.dma("sync", wd[:, i * 11:(i + 1) * 11, :],
                      wd_bf[layer].rearrange("(j p) n -> p j n", p=128)[:, i * 11:(i + 1) * 11, :],
                      reads=[f"wd_bf{layer}"], writes=["of_wd"])
            ym = [sbt(f"of_ym{i}", [128, 8, 512], BF16) for i in range(2)]
            xt = [sbt(f"of_x{i}", [128, 8, 512], F32) for i in range(2)]
            hT = [sbt(f"of_h{i}", [128, 8, 512], BF16) for i in range(2)]
            act = sbt("of_act", [128, NJ, 512], BF16)
            sg = [sbt(f"of_sg{i}", [128, 512], F32) for i in range(2)]
            wgu = [sbt(f"of_wgu{i}", [128, 2, 8, 128], BF16) for i in range(3)]
            if layer == 1:
                yo = sbt("of_yo", [128, 8, 512], F32)
                ytok = [sbt("of_ytok0", [128, 4, D], F32)] * 2
            nw = [0]

            def pre(it):
                s = it % 2
                t0 = it * 512
                P.dma("sync", ym[s][:], ym_src.rearrange("(c p) t -> p c t", p=128)[:, :, t0:t0 + 512],
                      reads=[ym_key], writes=[f"of_ym{s}"])
                P.dma("sync", xt[s][:], x_src.rearrange("(c p) t -> p c t", p=128)[:, :, t0:t0 + 512],
                      reads=[x_key], writes=[f"of_x{s}"])
                for m in range(8):
                    pi = 1 + (m % 3)
                    for k in range(8):
                        mm(pi, wo[:, k, m * 128:(m + 1) * 128], ym[s][:, k, :], k == 0, k == 7,
                           ["of_wo", f"of_ym{s}"])
                    P.op("vector", "tensor_tensor", reads=[f"psb{pi}", f"of_x{s}"], writes=[f"of_x{s}"],
                         out=xt[s][:, m, :], in0=psb[pi][:], in1=xt[s][:, m, :], op=ALU.add)
                rmsnorm_T(xt[s], f"of_x{s}", gi, hT[s], f"of_h{s}")

            def gate_up(it):
                s = it % 2
                for j in range(NJ):
                    ws = nw[0] % 3
                    nw[0] += 1
                    P.dma("sync", wgu[ws][:, 0], wg_bf[layer][j], reads=[f"wg_bf{layer}"], writes=[f"of_wgu{ws}"])
                    P.dma("sync", wgu[ws][:, 1], wu_bf[layer][j], reads=[f"wu_bf{layer}"], writes=[f"of_wgu{ws}"])
                    pg, pu = 4 + 2 * (j % 2), 5 + 2 * (j % 2)
                    for k in range(8):
                        mm(pg, wgu[ws][:, 0, k, :], hT[s][:, k, :], k == 0, k == 7, [f"of_wgu{ws}", f"of_h{s}"])
                    for k in range(8):
                        mm(pu, wgu[ws][:, 1, k, :], hT[s][:, k, :], k == 0, k == 7, [f"of_wgu{ws}", f"of_h{s}"])
                    P.op("scalar", "activation", reads=[f"psb{pg}"], writes=[f"of_sg{j % 2}"], out=sg[j % 2][:],
                         in_=psb[pg][:], func=AF.Silu)
                    P.op("vector", "tensor_tensor", reads=[f"of_sg{j % 2}", f"psb{pu}"], writes=["of_act"],
                         out=act[:, j, :], in0=sg[j % 2][:], in1=psb[pu][:], op=ALU.mult)

            def down(it):
                s = it % 2
                t0 = it * 512
                for m in range(8):
                    pi = 1 + (m % 3)
                    for j in range(NJ):
                        mm(pi, wd[:, j, m * 128:(m + 1) * 128], act[:, j, :], j == 0, j == NJ - 1,
                           ["of_wd", "of_act"])
                    P.op("vector", "tensor_tensor", reads=[f"psb{pi}", f"of_x{s}"], writes=[f"of_x{s}"],
                         out=xt[s][:, m, :], in0=psb[pi][:], in1=xt[s][:, m, :], op=ALU.add)
                if layer == 0 or "xT4" in dbg:
                    P.dma("sync", x_dst.rearrange("(c p) t -> p c t", p=128)[:, :, t0:t0 + 512], xt[s][:],
                          reads=[f"of_x{s}"], writes=[x_dkey])
                if layer == 1:
                    rmsnorm_T(xt[s], f"of_x{s}", 4, yo, "of_yo")
                    for sub in range(4):
                        for c in range(8):
                            pi = 1 + (sub % 3)
                            P.op("tensor", "transpose", reads=["of_yo", "ident_f"], writes=[f"psb{pi}"],
                                 out=psb[pi][:, (c % 4) * 128:(c % 4 + 1) * 128],
                                 in_=yo[:, c, sub * 128:(sub + 1) * 128], identity=ident_f[:])
                            if c % 4 == 3:
                                evac(ytok[s][:, sub, (c // 4) * 512:(c // 4 + 1) * 512], psb[pi][:], [f"psb{pi}"],
                                     ["of_ytok0"])
                    P.dma("sync", y_out[t0:t0 + 512, :].rearrange("(s p) d -> p s d", p=128), ytok[s][:],
                          reads=["of_ytok0"], writes=["y"])

            pre(0)
            for it in range(NT):
                gate_up(it)
                if it + 1 < NT:
                    pre(it + 1)
                down(it)
            P.end_phase()

    inproj(0)

    if upto <= 1:
        P.finish()
        return nc

    with contextlib.ExitStack() as ph:
        def sbt(name, shape, dt):
            return ph.enter_context(nc.sbuf_tensor(name, list(shape), dt))
        convw = sbt("rg_convw", [128, 4, 4], F32)
        convb = sbt("rg_convb", [128, 4], F32)
        ba = sbt("rg_ba", [128, 4], F32)
        bx = sbt("rg_bx", [128, 4], F32)
        lam = sbt("rg_lam", [128, 4], F32)
        cvec = sbt("rg_cvec", [128, 4], F32)
        cvec2 = sbt("rg_cvec2", [128, 4], F32)
        wst = sbt("rg_wst", [128, 2, 4, 128], F32)
        wbd = sbt("rg_wbd", [128, 2, 4, 128], BF16)
        for j in range(4):
            P.dma("sync", convw[:, j, :], ab_conv_w[j].rearrange("(c p) -> p c", p=128), writes=["rg_convw"],
                  allow_slow_non_contiguous=True)
        for tl, src, key in ((convb, ab_conv_b, "rg_convb"), (ba, ab_gate_a_b, "rg_ba"),
                             (bx, ab_gate_x_b, "rg_bx"), (lam, ab_lambda, "rg_lam")):
            P.dma("sync", tl[:], src.rearrange("(c p) -> p c", p=128), writes=[key],
                  allow_slow_non_contiguous=True)
        P.op("gpsimd", "memset", writes=["rg_wst"], ap=wst[:], constant=0.0)
        for gi_, wsrc in enumerate((ab_gate_a_w, ab_gate_x_w)):
            for hd in range(8):
                cc, hl = hd // 2, hd % 2
                P.dma("sync", wst[hl * 64:(hl + 1) * 64, gi_, cc, hl * 64:(hl + 1) * 64], wsrc[hd],
                      writes=["rg_wst"])
        P.op("vector", "tensor_copy", reads=["rg_wst"], writes=["rg_wbd"], out=wbd[:], in_=wst[:])
        P.op("scalar", "activation", reads=["rg_lam"], writes=["rg_cvec"], out=cvec[:], in_=lam[:],
             func=AF.Exp, scale=-1.0)
        P.op("scalar", "activation", reads=["rg_cvec", "ones_f"], writes=["rg_cvec"], out=cvec[:], in_=cvec[:],
             func=AF.Ln, bias=ones_f[:, 0:1])
        P.op("vector", "tensor_scalar", reads=["rg_cvec"], writes=["rg_cvec2"], out=cvec2[:], in0=cvec[:],
             scalar1=-16.0, scalar2=None, op0=ALU.mult)
        P.op("vector", "tensor_scalar", reads=["rg_cvec"], writes=["rg_cvec"], out=cvec[:], in0=cvec[:],
             scalar1=-8.0, scalar2=None, op0=ALU.mult)
        B = [sbt(f"rgB{i}", [128, T], F32) for i in range(7)]
        xc_bf = sbt("rg_xcbf", [128, T], BF16)
        y_bf = sbt("rg_ybf", [128, T], BF16)
        rg_rows = rgin.rearrange("(c p) t -> c p t", p=128)
        ym_rows = ymix0.rearrange("(c p) t -> c p t", p=128)
        for cc in range(4):
            xr, gt, xc, rr, ii, a2, hh = B
            P.dma("sync", xr[:], rg_rows[cc], reads=["rgin"], writes=["rgB0"])
            P.dma("sync", gt[:], rg_rows[4 + cc], reads=["rgin"], writes=["rgB1"])
            P.op("vector", "tensor_scalar", reads=["rgB0", "rg_convw", "rg_convb"], writes=["rgB2"],
                 out=xc[:], in0=xr[:], scalar1=convw[:, 3, cc:cc + 1], scalar2=convb[:, cc:cc + 1],
                 op0=ALU.mult, op1=ALU.add)
            for j in (2, 1, 0):
                dl = 3 - j
                P.op("vector", "scalar_tensor_tensor", reads=["rgB0", "rgB2", "rg_convw"], writes=["rgB2"],
                     out=xc[:, dl:], in0=xr[:, 0:T - dl], scalar=convw[:, j, cc:cc + 1], in1=xc[:, dl:],
                     op0=ALU.mult, op1=ALU.add)
            P.op("gpsimd", "tensor_copy", reads=["rgB2"], writes=["rg_xcbf"], out=xc_bf[:], in_=xc[:])
            for tt in range(8):
                for gi_, (dst, dkey, bias) in enumerate(((rr, "rgB3", ba), (ii, "rgB4", bx))):
                    pi = (tt * 2 + gi_) % 4
                    mm(pi, wbd[:, gi_, cc, :], xc_bf[:, tt * 512:(tt + 1) * 512], True, True,
                       ["rg_wbd", "rg_xcbf"])
                    P.op("scalar", "activation", reads=[f"psb{pi}", "rg_ba", "rg_bx"], writes=[dkey],
                         out=dst[:, tt * 512:(tt + 1) * 512], in_=psb[pi][:], func=AF.Sigmoid,
                         bias=bias[:, cc:cc + 1])
            P.op("scalar", "activation", reads=["rgB3", "rg_cvec2"], writes=["rgB5"], out=a2[:], in_=rr[:],
                 func=AF.Exp, scale=cvec2[:, cc:cc + 1])
            P.op("scalar", "activation", reads=["rgB3", "rg_cvec"], writes=["rgB3"], out=rr[:], in_=rr[:],
                 func=AF.Exp, scale=cvec[:, cc:cc + 1])
            P.op("vector", "tensor_scalar", reads=["rgB5"], writes=["rgB5"], out=a2[:], in0=a2[:],
                 scalar1=-1.0, scalar2=1.0, op0=ALU.mult, op1=ALU.add)
            P.op("scalar", "activation", reads=["rgB5"], writes=["rgB5"], out=a2[:], in_=a2[:], func=AF.Sqrt)
            P.op("gpsimd", "tensor_tensor", reads=["rgB4", "rgB2"], writes=["rgB4"], out=ii[:], in0=ii[:],
                 in1=xc[:], op=ALU.mult)
            P.op("vector", "tensor_tensor", reads=["rgB4", "rgB5"], writes=["rgB4"], out=ii[:], in0=ii[:],
                 in1=a2[:], op=ALU.mult)
            P.op("vector", "tensor_tensor_scan", reads=["rgB3", "rgB4"], writes=["rgB6"], out=hh[:],
                 data0=rr[:], data1=ii[:], initial=0.0, op0=ALU.mult, op1=ALU.add)
            P.op("gpsimd", "tensor_tensor", reads=["rgB1"], writes=["rgB0"], out=xr[:], in0=gt[:], in1=gt[:],
                 op=ALU.mult)
            P.op("gpsimd", "tensor_scalar", reads=["rgB0"], writes=["rgB0"], out=xr[:], in0=xr[:],
                 scalar1=0.044715, scalar2=1.0, op0=ALU.mult, op1=ALU.add)
            P.op("gpsimd", "tensor_tensor", reads=["rgB0", "rgB1"], writes=["rgB0"], out=xr[:], in0=xr[:],
                 in1=gt[:], op=ALU.mult)
            P.op("scalar", "activation", reads=["rgB0"], writes=["rgB0"], out=xr[:], in_=xr[:],
                 func=AF.Sigmoid, scale=1.5957691216057308)
            P.op("vector", "tensor_tensor", reads=["rgB6", "rgB1"], writes=["rgB6"], out=hh[:], in0=hh[:],
                 in1=gt[:], op=ALU.mult)
            P.op("vector", "tensor_tensor", reads=["rgB6", "rgB0"], writes=["rg_ybf"], out=y_bf[:], in0=hh[:],
                 in1=xr[:], op=ALU.mult)
            P.dma("sync", ym_rows[cc], y_bf[:], reads=["rg_ybf"], writes=["ymix0"])
        P.end_phase()

    if upto <= 2:
        P.finish()
        return nc

    with contextlib.ExitStack() as ph:
        def sbt(name, shape, dt):
            return ph.enter_context(nc.sbuf_tensor(name, list(shape), dt))
        tri = sbt("sb_tri", [128, 128], BF16)
        mstr = sbt("sb_mstr", [128, 128], F32)
        P.op("gpsimd", "affine_select", reads=["ones_bf"], writes=["sb_tri"], out=tri[:], in_=ones_bf[:],
             pattern=[[-1, 128]], compare_op=ALU.is_ge, fill=0.0, base=0, channel_multiplier=1)
        P.op("gpsimd", "affine_select", reads=["ones_f"], writes=["sb_mstr"], out=mstr[:], in_=ones_f[:],
             pattern=[[1, 128]], compare_op=ALU.is_gt, fill=0.0, base=0, channel_multiplier=-1)
        v_all = sbt("sb_v", [128, 32, 512], BF16)
        v_src = v0.rearrange("(n p) d -> p n d", p=128)
        for i in range(4):
            P.dma("sync", v_all[:, i * 8:(i + 1) * 8, :], v_src[:, i * 8:(i + 1) * 8, :], reads=["v0"],
                  writes=["sb_v"])
        qT = [sbt(f"sb_q{i}", [128, T], BF16) for i in range(2)]
        kT = [sbt(f"sb_k{i}", [128, T], BF16) for i in range(2)]
        yst = [sbt(f"sb_y{i}", [128, T], BF16) for i in range(2)]
        for i in range(2):
            P.op("gpsimd", "memset", writes=[f"sb_q{i}"], ap=qT[i][:], constant=0.0)
            P.op("gpsimd", "memset", writes=[f"sb_k{i}"], ap=kT[i][:], constant=0.0)
        e_t = [sbt(f"sb_e{i}", [128, 512], F32) for i in range(3)]
        sp_t = [sbt(f"sb_sp{i}", [128, 512], BF16) for i in range(3)]
        en_t = [sbt(f"sb_en{i}", [128, 512], F32) for i in range(2)]
        w_t = [sbt(f"sb_w{i}", [128, 512], BF16) for i in range(2)]
        lacc_b = [sbt(f"sb_laccb{i}", [128, 512], BF16) for i in range(2)]
        qk_rows = qk0.rearrange("(h p) t -> h p t", p=64)
        ym128 = ymix0.rearrange("(h p) t -> h p t", p=128)
        items = []
        for hd in range(8):
            for qi in range(8):
                q0 = qi * 512
                kbs = list(range(q0 // 128 + 3, -1, -1))
                for bi, kb in enumerate(kbs):
                    items.append(dict(hd=hd, qi=qi, q0=q0, kb=kb, bi=bi, last=(kb == 0), idx=len(items)))

        def geom(it):
            c0 = max(0, it["kb"] * 128 - it["q0"])
            return c0, slice(c0, 512), slice(c0, c0 + 128), it["kb"] * 128 >= it["q0"]

        def st0(it):
            hd, hs, r = it["hd"], it["hd"] % 2, it["idx"] % 2
            if it["qi"] == 0 and it["bi"] == 0:
                P.dma("sync", qT[hs][0:64, :], qk_rows[hd], reads=["qk0"], writes=[f"sb_q{hs}"])
                P.dma("sync", kT[hs][0:64, :], qk_rows[8 + hd], reads=["qk0"], writes=[f"sb_k{hs}"])
            c0, cs, dg, diag = geom(it)
            mm(r, kT[hs][:, it["kb"] * 128:(it["kb"] + 1) * 128], qT[hs][:, it["q0"] + c0:it["q0"] + 512], True, True,
               [f"sb_k{hs}", f"sb_q{hs}"], out=psb[r][:, cs])

        def st1(it):
            r, r3 = it["idx"] % 2, it["idx"] % 3
            c0, cs, dg, diag = geom(it)
            P.op("scalar", "activation", reads=[f"psb{r}"], writes=[f"sb_e{r3}"], out=e_t[r3][:, cs],
                 in_=psb[r][:, cs], func=AF.Exp, scale=0.125)
            P.op("scalar", "activation", reads=[f"sb_e{r3}", "ones_f"], writes=[f"sb_sp{r3}"],
                 out=sp_t[r3][:, cs], in_=e_t[r3][:, cs], func=AF.Ln, bias=ones_f[:, 0:1])
            if diag:
                P.op("vector", "tensor_tensor", reads=[f"sb_sp{r3}", "sb_mstr"], writes=[f"sb_sp{r3}"],
                     out=sp_t[r3][:, dg], in0=sp_t[r3][:, dg], in1=mstr[:], op=ALU.mult)

        def st2(it):
            r, r3 = it["idx"] % 2, it["idx"] % 3
            lq = (it["hd"] * 8 + it["qi"]) % 2
            c0, cs, dg, diag = geom(it)
            pc = 2 + r
            if it["bi"] == 0:
                P.op("vector", "memset", writes=[f"sb_laccb{lq}"], ap=lacc_b[lq][:], constant=0.0)
            mm(pc, tri[:], sp_t[r3][:, cs], True, it["bi"] == 0, ["sb_tri", f"sb_sp{r3}"], out=psb[pc][:, cs])
            if it["bi"] > 0:
                mm(pc, ones_bf[:], lacc_b[lq][:, cs], False, True, ["ones_bf", f"sb_laccb{lq}"], out=psb[pc][:, cs])
            if not it["last"]:
                P.op("vector", "tensor_tensor", reads=[f"sb_laccb{lq}", f"sb_sp{r3}"], writes=[f"sb_laccb{lq}"],
                     out=lacc_b[lq][:, cs], in0=lacc_b[lq][:, cs], in1=sp_t[r3][:, cs], op=ALU.add)

        def st3(it):
            r, r3 = it["idx"] % 2, it["idx"] % 3
            c0, cs, dg, diag = geom(it)
            pc = 2 + r
            P.op("scalar", "activation", reads=[f"psb{pc}"], writes=[f"sb_en{r}"], out=en_t[r][:, cs],
                 in_=psb[pc][:, cs], func=AF.Exp, scale=-1.0)
            P.op("vector", "tensor_tensor", reads=[f"sb_e{r3}", f"sb_en{r}"], writes=[f"sb_w{r}"],
                 out=w_t[r][:, cs], in0=e_t[r3][:, cs], in1=en_t[r][:, cs], op=ALU.mult)
            if diag:
                P.op("vector", "tensor_tensor", reads=[f"sb_w{r}", "sb_mstr"], writes=[f"sb_w{r}"],
                     out=w_t[r][:, dg], in0=w_t[r][:, dg], in1=mstr[:], op=ALU.mult)

        def st4(it):
            hd, r = it["hd"], it["idx"] % 2
            c0, cs, dg, diag = geom(it)
            po = 4 + (it["qi"] % 2)
            ys = (hd // 2) % 2
            hr = slice((hd % 2) * 64, (hd % 2) * 64 + 64)
            mm(po, v_all[:, it["kb"], (hd // 2) * 128:(hd // 2) * 128 + 128], w_t[r][:, cs], it["bi"] == 0, it["last"],
               ["sb_v", f"sb_w{r}"], out=psb[po][:, cs])
            if it["last"]:
                evac(yst[ys][hr, it["q0"]:it["q0"] + 512], psb[po][hr, :], [f"psb{po}"], [f"sb_y{ys}"])
                if it["qi"] == 7 and hd % 2 == 1:
                    P.dma("sync", ym128[4 + hd // 2], yst[ys][:], reads=[f"sb_y{ys}"], writes=["ymix0"])

        stages = [st0, st1, st2, st3, st4]
        for step in range(len(items) + len(stages) - 1):
            for j in range(len(stages) - 1, -1, -1):
                t_ = step - j
                if 0 <= t_ < len(items):
                    stages[j](items[t_])
        P.end_phase()

    if upto <= 3:
        P.finish()
        return nc

    out_ffn(0)
    if upto <= 4:
        P.finish()
        return nc

    inproj(1)
    if upto <= 5:
        P.finish()
        return nc

    NEG = -30000.0
    with contextlib.ExitStack() as ph:
        def sbt(name, shape, dt):
            return ph.enter_context(nc.sbuf_tensor(name, list(shape), dt))
        slopes = [2.0 ** (-2.0 * (h + 1)) for h in range(4)]
        io33 = sbt("mb_io33", [128, 33], F32)
        pidx = sbt("mb_pidx", [128, 2], F32)
        biasT = sbt("mb_biasT", [128, 4, 33], F32)
        nsl = sbt("mb_nsl", [128, 4, 2], F32)
        P.op("gpsimd", "iota", writes=["mb_io33"], out=io33[:], pattern=[[-128, 33]], base=128,
             channel_multiplier=1, allow_small_or_imprecise_dtypes=True)
        P.op("gpsimd", "iota", writes=["mb_pidx"], out=pidx[:], pattern=[[128, 2]], base=0,
             channel_multiplier=1, allow_small_or_imprecise_dtypes=True)
        for h in range(4):
            P.op("vector", "tensor_scalar", reads=["mb_io33"], writes=["mb_biasT"], out=biasT[:, h, :],
                 in0=io33[:], scalar1=slopes[h], scalar2=None, op0=ALU.mult)
            P.op("vector", "tensor_scalar", reads=["mb_pidx"], writes=["mb_nsl"], out=nsl[:, h, :],
                 in0=pidx[:], scalar1=-slopes[h], scalar2=None, op0=ALU.mult)
        pastm = sbt("mb_pastm", [128, 32, 32], F32)
        P.op("gpsimd", "memset", writes=["mb_pastm"], ap=pastm[:], constant=0.0)
        pm4 = pastm[:].rearrange("p (b e) n -> p b e n", e=2)[:, :, :, 0:16]
        P.op("gpsimd", "affine_select", reads=["mb_pastm"], writes=["mb_pastm"], out=pm4, in_=pm4,
             pattern=[[1, 16], [0, 2], [-1, 16]], compare_op=ALU.is_gt, fill=NEG, base=0, channel_multiplier=0)
        efull = sbt("mb_efull", [128, 128, 128], BF16)
        P.op("gpsimd", "memset", writes=["mb_efull"], ap=efull[:], constant=1.0)
        P.op("gpsimd", "affine_select", reads=["mb_efull"], writes=["mb_efull"], out=efull[:], in_=efull[:],
             pattern=[[-1, 128], [0, 128]], compare_op=ALU.is_equal, fill=0.0, base=0, channel_multiplier=1)
        mc = sbt("mb_mc", [128, 128], F32)
        P.op("gpsimd", "affine_select", reads=["ones_f"], writes=["mb_mc"], out=mc[:], in_=ones_f[:],
             pattern=[[1, 128]], compare_op=ALU.is_ge, fill=0.0, base=0, channel_multiplier=-1)
        v_all = sbt("mb_v", [128, 32, 512], BF16)
        v_src = v1.rearrange("(n p) d -> p n d", p=128)
        for i in range(4):
            P.dma("sync", v_all[:, i * 8:(i + 1) * 8, :], v_src[:, i * 8:(i + 1) * 8, :], reads=["v1"],
                  writes=["mb_v"])
        qT = [sbt(f"mb_q{i}", [128, T], BF16) for i in range(2)]
        kT = [sbt(f"mb_k{i}", [128, T], BF16) for i in range(2)]
        yst = [sbt(f"mb_y{i}", [128, T], BF16) for i in range(2)]
        km_f = sbt("mb_kmf", [128, 16], F32)
        km_b = sbt("mb_kmb", [128, 16], BF16)
        gm = sbt("mb_gm", [128, 32, 32], F32)
        ng = sbt("mb_ng", [128, 32, 32], F32)
        m8 = sbt("mb_m8", [128, 32, 8], F32)
        rt4 = [sbt(f"mb_rt4{i}", [128, 8, 128], BF16) for i in range(2)]
        w_t = [sbt(f"mb_w{i}", [128, 256], BF16) for i in range(3)]
        rz = [sbt(f"mb_rz{i}", [128, 256], F32) for i in range(2)]
        P.op("gpsimd", "memset", writes=["mb_gm"], ap=gm[:], constant=0.0)
        P.op("gpsimd", "memset", writes=["mb_ng"], ap=ng[:], constant=0.0)
        qk_rows = qk1.rearrange("(h p) t -> h p t", p=128)
        ym128 = ymix1.rearrange("(h p) t -> h p t", p=128)
        def gating(hd):
            hs = hd % 2
            P.dma("sync", qT[hs][:], qk_rows[hd], reads=["qk1"], writes=[f"mb_q{hs}"])
            P.dma("sync", kT[hs][:], qk_rows[4 + hd], reads=["qk1"], writes=[f"mb_k{hs}"])
            P.op("vector", "tensor_reduce", reads=[f"mb_k{hs}"], writes=["mb_kmf"], out=km_f[:],
                 in_=kT[hs][:].rearrange("p (n k) -> p n k", k=256), axis=AX.X, op=ALU.add)
            P.op("vector", "tensor_scalar", reads=["mb_kmf"], writes=["mb_kmb"], out=km_b[:], in0=km_f[:],
                 scalar1=1.0 / 256.0, scalar2=None, op0=ALU.mult)
            for i in range(32):
                mm(6, qT[hs][:, i * 128:(i + 1) * 128], km_b[:], True, True, [f"mb_q{hs}", "mb_kmb"],
                   out=psb[6][:, i * 16:(i + 1) * 16])
            P.op("vector", "tensor_tensor", reads=["psb6", "mb_pastm"], writes=["mb_gm"], out=gm[:, :, 0:16],
                 in0=psb[6][:].rearrange("p (i n) -> p i n", n=16), in1=pastm[:, :, 0:16], op=ALU.add)
            for i in range(32):
                P.op("vector", "max", reads=["mb_gm"], writes=["mb_m8"], out=m8[:, i, :], in_=gm[:, i, 0:16])
            P.op("vector", "tensor_tensor", reads=["mb_gm", "mb_m8"], writes=["mb_ng"], out=ng[:, :, 0:16],
                 in0=gm[:, :, 0:16], in1=m8[:, :, 2:3].to_broadcast([128, 32, 16]), op=ALU.is_ge)
            P.op("vector", "tensor_scalar", reads=["mb_ng"], writes=["mb_ng"], out=ng[:, :, 0:16],
                 in0=ng[:, :, 0:16], scalar1=-1.0, scalar2=-NEG, op0=ALU.add, op1=ALU.mult)
            P.op("vector", "tensor_tensor", reads=["mb_ng", "mb_pastm"], writes=["mb_ng"], out=ng[:, :, 0:16],
                 in0=ng[:, :, 0:16], in1=pastm[:, :, 0:16], op=ALU.add)
            P.op("vector", "memset", writes=["mb_ng"], ap=ng[:, :, 16:17], constant=0.0)
            ng4 = ng[:].rearrange("p (b e) n -> p b e n", e=2)
            for e_ in range(2):
                P.op("vector", "tensor_scalar", reads=["mb_ng", "mb_nsl"], writes=["mb_ng"],
                     out=ng4[:, :, e_, 0:17], in0=ng4[:, :, e_, 0:17], scalar1=nsl[:, hd, e_:e_ + 1],
                     scalar2=None, op0=ALU.add)
            for g in range(8):
                pi = 6 + (g // 4)
                P.op("tensor", "transpose", reads=["mb_ng", "ident_f"], writes=[f"psb{pi}"],
                     out=psb[pi][:, (g % 4) * 128:(g % 4 + 1) * 128],
                     in_=ng[:, 4 * g:4 * g + 4, :].rearrange("p a n -> p (a n)"), identity=ident_f[:])
                if g % 4 == 3:
                    evac(rt4[hs][:, g - 3:g + 1, :].rearrange("p a t -> p (a t)"), psb[pi][:], [f"psb{pi}"],
                         [f"mb_rt4{hs}"])

        items = []
        for hd in range(4):
            for b in range(16):
                for kt in range(2 * b + 2):
                    items.append(dict(hd=hd, b=b, kt=kt, nkt=2 * b + 2, idx=len(items)))

        def mgeom(it):
            c0 = 128 if it["kt"] == 2 * it["b"] + 1 else 0
            return c0, slice(c0, 256), it["kt"] >= 2 * it["b"]

        def m0(it):
            hd, hs, b, kt, r = it["hd"], it["hd"] % 2, it["b"], it["kt"], it["idx"] % 2
            if b == 8 and kt == 0 and hd < 3:
                gating(hd + 1)
            c0, cs, own = mgeom(it)
            n_row = 16 if own else kt // 2
            mm(r, kT[hs][:, kt * 128:(kt + 1) * 128], qT[hs][:, b * 256 + c0:(b + 1) * 256], True, False,
               [f"mb_k{hs}", f"mb_q{hs}"], out=psb[r][:, cs])
            for e_ in range(c0 // 128, 2):
                il = 2 * (b % 2) + e_
                mm(r, efull[:, il * 32 + n_row, :], rt4[hs][:, b // 2, :], False, e_ == 1,
                   ["mb_efull", f"mb_rt4{hs}"], out=psb[r][:, e_ * 128:(e_ + 1) * 128])

        def m1(it):
            hd, b, kt, r, ws = it["hd"], it["b"], it["kt"], it["idx"] % 2, it["idx"] % 3
            c0, cs, own = mgeom(it)
            dd = 2 * b - kt + 1
            P.op("scalar", "activation", reads=[f"psb{r}", "mb_biasT"], writes=[f"mb_w{ws}"],
                 out=w_t[ws][:, cs], in_=psb[r][:, cs], func=AF.Exp, bias=biasT[:, hd, dd:dd + 1])
            if own:
                P.op("vector", "tensor_tensor", reads=[f"mb_w{ws}", "mb_mc"], writes=[f"mb_w{ws}"],
                     out=w_t[ws][:, c0:c0 + 128], in0=w_t[ws][:, c0:c0 + 128], in1=mc[:], op=ALU.mult)

        def m2(it):
            hd, hs, b, kt, ws = it["hd"], it["hd"] % 2, it["b"], it["kt"], it["idx"] % 3
            c0, cs, own = mgeom(it)
            po, pz = 2 + (b % 2), 4 + (b % 2)
            last = kt == it["nkt"] - 1
            mm(po, v_all[:, kt, hd * 128:(hd + 1) * 128], w_t[ws][:, cs], kt == 0, last,
               ["mb_v", f"mb_w{ws}"], out=psb[po][:, cs])
            mm(pz, ones_bf[:], w_t[ws][:, cs], kt == 0, last, ["ones_bf", f"mb_w{ws}"], out=psb[pz][:, cs])
            if last:
                P.op("vector", "reciprocal", reads=[f"psb{pz}"], writes=[f"mb_rz{b % 2}"], out=rz[b % 2][:],
                     in_=psb[pz][:, 0:256])
                P.op("vector", "tensor_tensor", reads=[f"psb{po}", f"mb_rz{b % 2}"], writes=[f"mb_y{hs}"],
                     out=yst[hs][:, b * 256:(b + 1) * 256], in0=psb[po][:, 0:256], in1=rz[b % 2][:], op=ALU.mult)
                if b == 15:
                    P.dma("sync", ym128[4 + hd], yst[hs][:], reads=[f"mb_y{hs}"], writes=["ymix1"])

        gating(0)
        mst = [m0, m1, m2]
        for step in range(len(items) + len(mst) - 1):
            for j in range(len(mst) - 1, -1, -1):
                t_ = step - j
                if 0 <= t_ < len(items):
                    mst[j](items[t_])
        P.end_phase()

    if upto <= 6:
        P.finish()
        return nc

    INV_2PI = 1.0 / (2.0 * math.pi)
    MAGIC = 12582912.0
    SIN_SCALE = 6.283185
    with contextlib.ExitStack() as ph:
        def sbt(name, shape, dt):
            return ph.enter_context(nc.sbuf_tensor(name, list(shape), dt))

        def vop(eng, meth, reads, writes, **kw):
            P.op(eng, meth, reads=reads, writes=writes, **kw)

        Ts = sbt("s5_Ts", [128, 4, 8, 2, 128], BF16)
        To = sbt("s5_To", [128, 16, 8, 2, 32], BF16)
        Kt = sbt("s5_Kt", [128, 4, 8, 128], BF16)
        phiT = sbt("s5_phiT", [128, 16], F32)
        rhoT = sbt("s5_rhoT", [128, 16], F32)
        negpi = sbt("s5_negpi", [128, 1], F32)
        pst = contextlib.ExitStack()
        sbt_outer = sbt

        def sbt(name, shape, dt):
            return pst.enter_context(nc.sbuf_tensor(name, list(shape), dt))
        lr = sbt("s5_lr", [128, 2, 64], F32)
        li = sbt("s5_li", [128, 2, 64], F32)
        ldt = sbt("s5_ldt", [128, 2], F32)
        bre = sbt("s5_bre", [128, 2, 64, 16], F32)
        bim = sbt("s5_bim", [128, 2, 64, 16], F32)
        cst = sbt("s5_cst", [128, 2, 16, 128], F32)
        for tl, key in ((lr, "s5_lr"), (li, "s5_li"), (ldt, "s5_ldt"), (bre, "s5_bre"), (bim, "s5_bim"),
                        (cst, "s5_cst")):
            P.op("gpsimd", "memset", writes=[key], ap=tl[:], constant=0.0)
        P.dma("sync", lr[0:16], cd_lam_re.rearrange("(a g) n -> a g n", g=2), writes=["s5_lr"])
        P.dma("sync", li[0:16], cd_lam_im.rearrange("(a g) n -> a g n", g=2), writes=["s5_li"])
        P.dma("sync", ldt[0:16], cd_log_dt.rearrange("(a g) -> a g", g=2), writes=["s5_ldt"])
        P.dma("sync", bre[0:16], cd_b_re.rearrange("(a g) n p -> a g n p", g=2), writes=["s5_bre"])
        P.dma("sync", bim[0:16], cd_b_im.rearrange("(a g) n p -> a g n p", g=2), writes=["s5_bim"])
        for ri_, csrc in ((0, cd_c_re), (1, cd_c_im)):
            for g_ in range(2):
                P.dma("sync", cst[0:16, ri_, :, g_ * 64:(g_ + 1) * 64],
                      csrc.rearrange("(a g) p n -> a g p n", g=2)[:, g_], writes=["s5_cst"])
        P.op("gpsimd", "memset", writes=["s5_negpi"], ap=negpi[:], constant=-0.5 * math.pi)
        dtt = sbt("s5_dt", [128, 2], F32)
        vop("scalar", "activation", ["s5_ldt"], ["s5_dt"], out=dtt[:], in_=ldt[:], func=AF.Exp)
        sm = {}
        for nm in ("lrdt", "lidt", "mag", "a1", "sinv", "cosv", "abre", "abim", "den", "t1", "t2", "fre", "fim"):
            sm[nm] = sbt("s5_" + nm, [128, 2, 64], F32)

        def k(nm):
            return "s5_" + nm
        dt_bc = dtt[:].unsqueeze(2).to_broadcast([128, 2, 64])
        vop("vector", "tensor_tensor", [k("lr"), k("dt")], [k("lrdt")], out=sm["lrdt"][:], in0=lr[:], in1=dt_bc, op=ALU.mult)
        vop("vector", "tensor_tensor", [k("li"), k("dt")], [k("lidt")], out=sm["lidt"][:], in0=li[:], in1=dt_bc, op=ALU.mult)
        vop("scalar", "activation", [k("lrdt")], [k("mag")], out=sm["mag"][:], in_=sm["lrdt"][:], func=AF.Exp)
        vop("vector", "tensor_scalar", [k("lidt")], [k("a1")], out=sm["a1"][:], in0=sm["lidt"][:], scalar1=INV_2PI,
            scalar2=MAGIC, op0=ALU.mult, op1=ALU.add)
        vop("vector", "tensor_scalar", [k("a1")], [k("a1")], out=sm["a1"][:], in0=sm["a1"][:], scalar1=-MAGIC,
            scalar2=None, op0=ALU.add)
        vop("vector", "scalar_tensor_tensor", [k("lidt"), k("a1")], [k("a1")], out=sm["a1"][:], in0=sm["lidt"][:],
            scalar=INV_2PI, in1=sm["a1"][:], op0=ALU.mult, op1=ALU.subtract)
        vop("scalar", "activation", [k("a1")], [k("sinv")], out=sm["sinv"][:], in_=sm["a1"][:], func=AF.Sin,
            scale=SIN_SCALE)
        vop("vector", "scalar_tensor_tensor", [k("a1")], [k("a1")], out=sm["a1"][:], in0=sm["a1"][:], scalar=-1.0,
            in1=sm["a1"][:], op0=ALU.mult, op1=ALU.max)
        vop("scalar", "activation", [k("a1"), k("negpi")], [k("cosv")], out=sm["cosv"][:], in_=sm["a1"][:],
            func=AF.Sin, scale=SIN_SCALE, bias=negpi[:])
        vop("vector", "scalar_tensor_tensor", [k("cosv"), k("mag")], [k("abre")], out=sm["abre"][:], in0=sm["cosv"][:],
            scalar=-1.0, in1=sm["mag"][:], op0=ALU.mult, op1=ALU.mult)
        vop("vector", "tensor_tensor", [k("sinv"), k("mag")], [k("abim")], out=sm["abim"][:], in0=sm["sinv"][:],
            in1=sm["mag"][:], op=ALU.mult)
        vop("vector", "tensor_tensor", [k("lr")], [k("den")], out=sm["den"][:], in0=lr[:], in1=lr[:], op=ALU.mult)
        vop("vector", "tensor_tensor", [k("li")], [k("t1")], out=sm["t1"][:], in0=li[:], in1=li[:], op=ALU.mult)
        vop("vector", "tensor_tensor", [k("den"), k("t1")], [k("den")], out=sm["den"][:], in0=sm["den"][:], in1=sm["t1"][:], op=ALU.add)
        vop("vector", "tensor_scalar", [k("den")], [k("den")], out=sm["den"][:], in0=sm["den"][:], scalar1=1e-30,
            scalar2=None, op0=ALU.max)
        vop("vector", "reciprocal", [k("den")], [k("den")], out=sm["den"][:], in_=sm["den"][:])
        vop("vector", "tensor_scalar", [k("abre")], [k("t2")], out=sm["t2"][:], in0=sm["abre"][:], scalar1=-1.0,
            scalar2=None, op0=ALU.add)
        vop("vector", "tensor_tensor", [k("t2"), k("lr")], [k("fre")], out=sm["fre"][:], in0=sm["t2"][:], in1=lr[:], op=ALU.mult)
        vop("vector", "tensor_tensor", [k("abim"), k("li")], [k("t1")], out=sm["t1"][:], in0=sm["abim"][:], in1=li[:], op=ALU.mult)
        vop("vector", "tensor_tensor", [k("fre"), k("t1")], [k("fre")], out=sm["fre"][:], in0=sm["fre"][:], in1=sm["t1"][:], op=ALU.add)
        vop("vector", "tensor_tensor", [k("fre"), k("den")], [k("fre")], out=sm["fre"][:], in0=sm["fre"][:], in1=sm["den"][:], op=ALU.mult)
        vop("vector", "tensor_tensor", [k("abim"), k("lr")], [k("fim")], out=sm["fim"][:], in0=sm["abim"][:], in1=lr[:], op=ALU.mult)
        vop("vector", "tensor_tensor", [k("t2"), k("li")], [k("t1")], out=sm["t1"][:], in0=sm["t2"][:], in1=li[:], op=ALU.mult)
        vop("vector", "tensor_tensor", [k("fim"), k("t1")], [k("fim")], out=sm["fim"][:], in0=sm["fim"][:], in1=sm["t1"][:], op=ALU.subtract)
        vop("vector", "tensor_tensor", [k("fim"), k("den")], [k("fim")], out=sm["fim"][:], in0=sm["fim"][:], in1=sm["den"][:], op=ALU.mult)
        bbre = sbt("s5_bbre", [128, 2, 64, 16], F32)
        bbim = sbt("s5_bbim", [128, 2, 64, 16], F32)
        tb1 = sbt("s5_tb1", [128, 2, 64, 16], F32)
        tb2 = sbt("s5_tb2", [128, 2, 64, 16], F32)
        fre_bc = sm["fre"][:].unsqueeze(3).to_broadcast([128, 2, 64, 16])
        fim_bc = sm["fim"][:].unsqueeze(3).to_broadcast([128, 2, 64, 16])
        vop("vector", "tensor_tensor", [k("bre"), k("fre")], [k("tb1")], out=tb1[:], in0=bre[:], in1=fre_bc, op=ALU.mult)
        vop("vector", "tensor_tensor", [k("bim"), k("fim")], [k("tb2")], out=tb2[:], in0=bim[:], in1=fim_bc, op=ALU.mult)
        vop("vector", "tensor_tensor", [k("tb1"), k("tb2")], [k("bbre")], out=bbre[:], in0=tb1[:], in1=tb2[:], op=ALU.subtract)
        vop("vector", "tensor_tensor", [k("bim"), k("fre")], [k("tb1")], out=tb1[:], in0=bim[:], in1=fre_bc, op=ALU.mult)
        vop("vector", "tensor_tensor", [k("bre"), k("fim")], [k("tb2")], out=tb2[:], in0=bre[:], in1=fim_bc, op=ALU.mult)
        vop("vector", "tensor_tensor", [k("tb1"), k("tb2")], [k("bbim")], out=bbim[:], in0=tb1[:], in1=tb2[:], op=ALU.add)
        pw = sbt("s5_pw", [128, 9, 2, 128], F32)
        pt1 = sbt("s5_pt1", [128, 128], F32)
        pt2 = sbt("s5_pt2", [128, 128], F32)
        P.op("gpsimd", "memset", writes=[k("pw")], ap=pw[:, 0, 0, :], constant=1.0)
        P.op("gpsimd", "memset", writes=[k("pw")], ap=pw[:, 0, 1, :], constant=0.0)
        abre_f = sm["abre"][:].rearrange("p a b -> p (a b)")
        abim_f = sm["abim"][:].rearrange("p a b -> p (a b)")
        for kk in range(1, 9):
            vop("vector", "tensor_tensor", [k("pw"), k("abre")], [k("pt1")], out=pt1[:], in0=pw[:, kk - 1, 0, :], in1=abre_f, op=ALU.mult)
            vop("vector", "tensor_tensor", [k("pw"), k("abim")], [k("pt2")], out=pt2[:], in0=pw[:, kk - 1, 1, :], in1=abim_f, op=ALU.mult)
            vop("vector", "tensor_tensor", [k("pt1"), k("pt2")], [k("pw")], out=pw[:, kk, 0, :], in0=pt1[:], in1=pt2[:], op=ALU.subtract)
            vop("vector", "tensor_tensor", [k("pw"), k("abim")], [k("pt1")], out=pt1[:], in0=pw[:, kk - 1, 0, :], in1=abim_f, op=ALU.mult)
            vop("vector", "tensor_tensor", [k("pw"), k("abre")], [k("pt2")], out=pt2[:], in0=pw[:, kk - 1, 1, :], in1=abre_f, op=ALU.mult)
            vop("vector", "tensor_tensor", [k("pt1"), k("pt2")], [k("pw")], out=pw[:, kk, 1, :], in0=pt1[:], in1=pt2[:], op=ALU.add)
        hm = sbt("s5_hm", [128, 2], F32)
        P.op("gpsimd", "memset", writes=[k("hm")], ap=hm[:], constant=0.0)
        P.op("gpsimd", "memset", writes=[k("hm")], ap=hm[0:64, 0:1], constant=1.0)
        P.op("gpsimd", "memset", writes=[k("hm")], ap=hm[64:128, 1:2], constant=1.0)
        Wall = [sbt(f"s5_Wall{i}", [128, 2, 16, 128], BF16) for i in range(2)]
        Wf = [sbt(f"s5_Wf{i}", [128, 2, 16, 128], F32) for i in range(2)]
        raw = [sbt(f"s5_raw{i}", [128, 2, 16, 16], F32) for i in range(2)]
        WTb = sbt("s5_WTb", [128, 16, 8, 2, 32], BF16)
        ToC = sbt("s5_ToC", [128, 16, 2, 32], BF16)
        ntr = [0]

        def transpose_mask(src, skey, dst_fn, dkey):
            rw = ntr[0] % 2
            ntr[0] += 1
            for ri in range(2):
                for p4 in range(4):
                    pi = 1 + (p4 % 2)
                    for pp in range(4):
                        P.op("tensor", "transpose", reads=[skey, "ident_f"], writes=[f"psb{pi}"],
                             out=psb[pi][:, pp * 128:(pp + 1) * 128], in_=src[:, ri, p4 * 4 + pp, :], identity=ident_f[:])
                    evac(raw[rw][:, ri, p4 * 4:(p4 + 1) * 4, :],
                         psb[pi][:].rearrange("p (a b) -> p a b", b=128)[:, :, 0:16], [f"psb{pi}"], [f"s5_raw{rw}"])
            for ri in range(2):
                for glp in range(2):
                    vop("vector", "tensor_scalar", [f"s5_raw{rw}", k("hm")], [dkey], out=dst_fn(ri, glp),
                        in0=raw[rw][:, ri, :, :].rearrange("p a b -> p b a"), scalar1=hm[:, glp:glp + 1], scalar2=None,
                        op0=ALU.mult)

        for kk in range(8):
            wf = Wf[kk % 2]
            wkey = f"s5_Wf{kk % 2}"
            pre_bc = pw[:, kk, 0, :].rearrange("p (g n) -> p g n", g=2).unsqueeze(3).to_broadcast([128, 2, 64, 16])
            pim_bc = pw[:, kk, 1, :].rearrange("p (g n) -> p g n", g=2).unsqueeze(3).to_broadcast([128, 2, 64, 16])
            vop("vector", "tensor_tensor", [k("bbre"), k("pw")], [k("tb1")], out=tb1[:], in0=bbre[:], in1=pre_bc, op=ALU.mult)
            vop("vector", "tensor_tensor", [k("bbim"), k("pw")], [k("tb2")], out=tb2[:], in0=bbim[:], in1=pim_bc, op=ALU.mult)
            vop("vector", "tensor_tensor", [k("tb1"), k("tb2")], [wkey], out=wf[:, 0].rearrange("a p (g n) -> a g n p", g=2),
                in0=tb1[:], in1=tb2[:], op=ALU.subtract)
            vop("vector", "tensor_tensor", [k("bbim"), k("pw")], [k("tb1")], out=tb1[:], in0=bbim[:], in1=pre_bc, op=ALU.mult)
            vop("vector", "tensor_tensor", [k("bbre"), k("pw")], [k("tb2")], out=tb2[:], in0=bbre[:], in1=pim_bc, op=ALU.mult)
            vop("vector", "tensor_tensor", [k("tb1"), k("tb2")], [wkey], out=wf[:, 1].rearrange("a p (g n) -> a g n p", g=2),
                in0=tb1[:], in1=tb2[:], op=ALU.add)
            vop("gpsimd", "tensor_copy", [wkey], [f"s5_Wall{kk % 2}"], out=Wall[kk % 2][:], in_=wf[:])
            P.dma("sync", s5wd[:, 7 - kk], Wall[kk % 2][0:16], reads=[f"s5_Wall{kk % 2}"], writes=["s5wd"])
            transpose_mask(wf, wkey, lambda ri, glp, kk=kk: WTb[:, :, kk, ri, glp * 16:(glp + 1) * 16], k("WTb"))
        P.op("gpsimd", "memset", writes=[k("Ts")], ap=Ts[:], constant=0.0)
        for pair in range(16):
            ct, q = pair // 4, pair % 4
            for gl in range(2):
                P.dma("sync" if (pair + gl) % 2 == 0 else "gpsimd",
                      Ts[32 * q + 16 * gl:32 * q + 16 * gl + 16, ct, :, :, gl * 64:(gl + 1) * 64].rearrange("p j r n -> p (j r) n"),
                      s5wd[pair, :, :, :, gl * 64:(gl + 1) * 64].rearrange("j r p n -> p (j r) n"),
                      reads=["s5wd"], writes=[k("Ts")])
        tcl = [t_[:].rearrange("a g n p -> a (g n p)").rearrange("a (x y) -> a x y", y=128) for t_ in (tb1, tb2)]
        for i in range(-1, 8):
            wf = Wf[i % 2]
            wkey = f"s5_Wf{i % 2}"
            pre_bc = pw[:, i + 1, 0, :].unsqueeze(1).to_broadcast([128, 16, 128])
            pim_bc = pw[:, i + 1, 1, :].unsqueeze(1).to_broadcast([128, 16, 128])
            vop("vector", "tensor_tensor", [k("cst"), k("pw")], [k("tb1")], out=tcl[0], in0=cst[:, 0], in1=pre_bc, op=ALU.mult)
            vop("vector", "tensor_tensor", [k("cst"), k("pw")], [k("tb2")], out=tcl[1], in0=cst[:, 1], in1=pim_bc, op=ALU.mult)
            vop("vector", "tensor_tensor", [k("tb1"), k("tb2")], [wkey], out=wf[:, 0], in0=tcl[0], in1=tcl[1], op=ALU.subtract)
            vop("vector", "tensor_tensor", [k("cst"), k("pw")], [k("tb1")], out=tcl[0], in0=cst[:, 0], in1=pim_bc, op=ALU.mult)
            vop("vector", "tensor_tensor", [k("cst"), k("pw")], [k("tb2")], out=tcl[1], in0=cst[:, 1], in1=pre_bc, op=ALU.mult)
            vop("vector", "scalar_tensor_tensor", [k("tb1"), k("tb2")], [wkey], out=wf[:, 1], in0=tcl[0], scalar=-1.0,
                in1=tcl[1], op0=ALU.mult, op1=ALU.subtract)
            if i < 0:
                transpose_mask(wf, wkey, lambda ri, glp: ToC[:, :, ri, glp * 16:(glp + 1) * 16], k("ToC"))
            else:
                transpose_mask(wf, wkey, lambda ri, glp, i=i: To[:, :, i, ri, glp * 16:(glp + 1) * 16], k("To"))
        P.op("gpsimd", "memset", writes=[k("Kt")], ap=Kt[:], constant=0.0)
        for ct in range(4):
            pk = 3 + (ct % 2)
            P.pe_drain()
            for q in range(4):
                pair = ct * 4 + q
                for tau in range(8):
                    for ri in range(2):
                        P.op("tensor", "matmul", reads=[k("WTb"), k("ToC")], writes=[f"psb{pk}"],
                             out=psb[pk][32 * q:32 * q + 32, tau * 32:(tau + 1) * 32], lhsT=WTb[:, pair, tau, ri, :],
                             rhs=ToC[:, pair, ri, :], start=(ri == 0), stop=(ri == 1), tile_position=(0, 32 * q), skip_group_check=True)
            P.pe_drain()
            for q in range(4):
                evac(Kt[32 * q:32 * q + 32, ct, :, 32 * q:32 * q + 32],
                     psb[pk][32 * q:32 * q + 32, 0:256].rearrange("p (a b) -> p a b", b=32), [f"psb{pk}"], [k("Kt")])
        P.op("tensor", "transpose", reads=[k("lidt"), "ident_f"], writes=["psb1"], out=psb[1][:, 0:128],
             in_=sm["lidt"][:].rearrange("p a b -> p (a b)"), identity=ident_f[:])
        P.op("tensor", "transpose", reads=[k("lrdt"), "ident_f"], writes=["psb1"], out=psb[1][:, 128:256],
             in_=sm["lrdt"][:].rearrange("p a b -> p (a b)"), identity=ident_f[:])
        vop("vector", "tensor_scalar", ["psb1"], [k("phiT")], out=phiT[:], in0=psb[1][:, 0:16], scalar1=8.0 * INV_2PI,
            scalar2=None, op0=ALU.mult)
        vop("scalar", "activation", ["psb1"], [k("rhoT")], out=rhoT[:], in_=psb[1][:, 128:144], func=AF.Exp, scale=8.0)
        P.end_phase()
        pst.close()
        sbt = sbt_outer
        dvec = sbt("s5_d", [128, 4], F32)
        gb = sbt("s5_gb", [128, 4], F32)
        P.dma("sync", dvec[:], cd_d.rearrange("(c p) -> p c", p=128), writes=[k("d")], allow_slow_non_contiguous=True)
        P.dma("sync", gb[:], cd_glu_b.rearrange("(c p) -> p c", p=128), writes=[k("gb")], allow_slow_non_contiguous=True)
        gw = sbt("s5_gw", [128, 4, 512], BF16)
        P.dma("gpsimd", gw[:], cd_glu_w.rearrange("(c p) n -> p c n", p=128), writes=[k("gw")])
        NCH = T // 8
        iot = sbt("s5_iota", [128, NCH], F32)
        P.op("gpsimd", "iota", writes=[k("iota")], out=iot[:], pattern=[[1, NCH]], base=0,
             channel_multiplier=0, allow_small_or_imprecise_dtypes=True)
        yg = [sbt(f"s5_yg{c}", [128, T], BF16) for c in range(4)]
        g1 = [sbt(f"s5_g1{i}", [128, 512], F32) for i in range(2)]
        g2 = [sbt(f"s5_g2{i}", [128, 512], F32) for i in range(2)]
        mst = contextlib.ExitStack()

        def sbt(name, shape, dt):
            return mst.enter_context(nc.sbuf_tensor(name, list(shape), dt))
        u_b = [sbt(f"s5_ub{i}", [128, T], BF16) for i in range(2)]
        um = [sbt(f"s5_um{q}", [128, T], BF16) for q in range(4)]
        for q in range(4):
            P.op("gpsimd", "memset", writes=[f"s5_um{q}"], ap=um[q][:], constant=0.0)
        ang = [sbt(f"s5_ang{i}", [128, NCH], F32) for i in range(2)]
        nsn = [sbt(f"s5_ns{i}", [128, NCH], F32) for i in range(3)]
        ncs = [sbt(f"s5_nc{i}", [128, NCH], F32) for i in range(3)]
        rho_t = [sbt(f"s5_rho{i}", [128, NCH], F32) for i in range(3)]
        wre = [sbt(f"s5_wre{i}", [128, NCH], F32) for i in range(2)]
        wim = [sbt(f"s5_wim{i}", [128, NCH], F32) for i in range(2)]
        xre = sbt("s5_xre", [128, NCH], F32)
        xim = sbt("s5_xim", [128, NCH], F32)
        ta = sbt("s5_ta", [128, NCH], F32)
        tbb = sbt("s5_tbb", [128, NCH], F32)
        tc_ = sbt("s5_tc", [128, NCH], F32)
        Xa = [[[sbt(f"s5_X{c}{q}{ri}", [128, NCH], BF16) for ri in range(2)] for q in range(4)] for c in range(2)]
        for c in range(2):
            for q in range(4):
                for ri in range(2):
                    P.op("gpsimd", "memset", writes=[f"s5_X{c}{q}{ri}"], ap=Xa[c][q][ri][:], constant=0.0)
        u_rows = s5u.rearrange("(c p) t -> c p t", p=128)
        items = []
        for ct in range(4):
            for q in range(4):
                items.append(dict(ct=ct, q=q, pair=ct * 4 + q, idx=len(items)))

        def sA(it):
            ct, q, pair, p = it["ct"], it["q"], it["pair"], it["idx"]
            if q == 0:
                P.dma("gpsimd", u_b[ct % 2][:], u_rows[ct], reads=["s5u"], writes=[f"s5_ub{ct % 2}"])
                for qq in range(4):
                    P.dma("gpsimd", um[qq][32 * qq:32 * qq + 32, :], u_rows[ct][32 * qq:32 * qq + 32, :], reads=["s5u"],
                          writes=[f"s5_um{qq}"])
            sa, s3 = p % 2, p % 3
            vop("vector", "tensor_scalar", [k("iota"), k("phiT")], [f"s5_ang{sa}"], out=ang[sa][:], in0=iot[:],
                scalar1=phiT[:, pair:pair + 1], scalar2=MAGIC, op0=ALU.mult, op1=ALU.add)
            vop("vector", "tensor_scalar", [f"s5_ang{sa}"], [f"s5_ang{sa}"], out=ang[sa][:], in0=ang[sa][:], scalar1=-MAGIC,
                scalar2=None, op0=ALU.add)
            vop("vector", "scalar_tensor_tensor", [k("iota"), k("phiT"), f"s5_ang{sa}"], [f"s5_ang{sa}"], out=ang[sa][:],
                in0=iot[:], scalar=phiT[:, pair:pair + 1], in1=ang[sa][:], op0=ALU.mult, op1=ALU.subtract)
            vop("scalar", "activation", [f"s5_ang{sa}"], [f"s5_ns{s3}"], out=nsn[s3][:], in_=ang[sa][:], func=AF.Sin,
                scale=-SIN_SCALE)
            vop("vector", "scalar_tensor_tensor", [f"s5_ang{sa}"], [f"s5_ang{sa}"], out=ang[sa][:], in0=ang[sa][:], scalar=-1.0,
                in1=ang[sa][:], op0=ALU.mult, op1=ALU.max)
            vop("scalar", "activation", [f"s5_ang{sa}", k("negpi")], [f"s5_nc{s3}"], out=ncs[s3][:], in_=ang[sa][:],
                func=AF.Sin, scale=SIN_SCALE, bias=negpi[:])
            vop("scalar", "activation", [k("iota"), k("rhoT")], [f"s5_rho{s3}"], out=rho_t[s3][:], in_=iot[:],
                func=AF.Identity, scale=0.0, bias=rhoT[:, pair:pair + 1])

        def sB(it):
            ct, q, pair, p = it["ct"], it["q"], it["pair"], it["idx"]
            r, s3 = p % 2, p % 3
            pr, pm = 2 * r, 2 * r + 1
            for ri, pb in ((0, pr), (1, pm)):
                for j in range(8):
                    mm(pb, Ts[:, ct, j, ri, :], um[q][:, j:T:8], j == 0, j == 7, [k("Ts"), f"s5_um{q}"])
            vop("vector", "tensor_tensor", [f"psb{pr}", f"s5_nc{s3}"], [k("ta")], out=ta[:], in0=psb[pr][:], in1=ncs[s3][:], op=ALU.mult)
            vop("vector", "tensor_tensor", [f"psb{pm}", f"s5_ns{s3}"], [k("tbb")], out=tbb[:], in0=psb[pm][:], in1=nsn[s3][:], op=ALU.mult)
            vop("gpsimd", "tensor_tensor", [k("ta"), k("tbb")], [f"s5_wre{r}"], out=wre[r][:], in0=ta[:], in1=tbb[:], op=ALU.add)
            vop("vector", "tensor_tensor", [f"psb{pm}", f"s5_nc{s3}"], [k("tc")], out=tc_[:], in0=psb[pm][:], in1=ncs[s3][:], op=ALU.mult)
            vop("vector", "tensor_tensor", [f"psb{pr}", f"s5_ns{s3}"], [k("tbb")], out=tbb[:], in0=psb[pr][:], in1=nsn[s3][:], op=ALU.mult)
            vop("gpsimd", "tensor_tensor", [k("tc"), k("tbb")], [f"s5_wim{r}"], out=wim[r][:], in0=tc_[:], in1=tbb[:], op=ALU.subtract)

        def sC(it):
            ct, q, pair, p = it["ct"], it["q"], it["pair"], it["idx"]
            r, s3, xc = p % 2, p % 3, ct % 2
            vop("vector", "tensor_tensor_scan", [f"s5_rho{s3}", f"s5_wre{r}"], [k("xre")], out=xre[:],
                data0=rho_t[s3][:], data1=wre[r][:], initial=0.0, op0=ALU.mult, op1=ALU.add)
            vop("vector", "tensor_tensor_scan", [f"s5_rho{s3}", f"s5_wim{r}"], [k("xim")], out=xim[:],
                data0=rho_t[s3][:], data1=wim[r][:], initial=0.0, op0=ALU.mult, op1=ALU.add)
            n1 = NCH - 1
            vop("gpsimd", "tensor_tensor", [k("xre"), f"s5_nc{s3}"], [k("ta")], out=ta[:], in0=xre[:], in1=ncs[s3][:], op=ALU.mult)
            vop("vector", "tensor_tensor", [k("xim"), f"s5_ns{s3}"], [k("tc")], out=tc_[:], in0=xim[:], in1=nsn[s3][:], op=ALU.mult)
            vop("gpsimd", "tensor_tensor", [k("ta"), k("tc")], [f"s5_X{xc}{q}0"], out=Xa[xc][q][0][:, 1:NCH], in0=ta[:, 0:n1],
                in1=tc_[:, 0:n1], op=ALU.subtract)
            vop("vector", "tensor_tensor", [k("xre"), f"s5_ns{s3}"], [k("ta")], out=ta[:], in0=xre[:], in1=nsn[s3][:], op=ALU.mult)
            vop("gpsimd", "tensor_tensor", [k("xim"), f"s5_nc{s3}"], [k("tc")], out=tc_[:], in0=xim[:], in1=ncs[s3][:], op=ALU.mult)
            vop("vector", "tensor_tensor", [k("ta"), k("tc")], [f"s5_X{xc}{q}1"], out=Xa[xc][q][1][:, 1:NCH], in0=ta[:, 0:n1],
                in1=tc_[:, 0:n1], op=ALU.add)
            if q == 3:
                cb = ct % 2
                for half in range(2):
                    ilist = list(range(half * 4, half * 4 + 4))
                    for i in ilist:
                        py = 4 + (i % 4)
                        for tau in range(i + 1):
                            mm(py, Kt[:, ct, tau, :], u_b[cb][:, i - tau:T:8], tau == 0, False, [k("Kt"), f"s5_ub{cb}"])
                    P.pe_drain()
                    for i in ilist:
                        py = 4 + (i % 4)
                        for qq in range(4):
                            for ri in range(2):
                                P.op("tensor", "matmul", reads=[k("To"), f"s5_X{xc}{qq}{ri}"], writes=[f"psb{py}"],
                                     out=psb[py][32 * qq:32 * qq + 32, :], lhsT=To[:, ct * 4 + qq, i, ri, :],
                                     rhs=Xa[xc][qq][ri][:], start=False, stop=(qq == 3 and ri == 1),
                                     tile_position=(0, 32 * qq), skip_group_check=True)
                    P.pe_drain()
                    for i in ilist:
                        py = 4 + (i % 4)
                        rr = i % 2
                        vop("vector", "scalar_tensor_tensor", [f"s5_ub{cb}", k("d"), f"psb{py}"], [f"s5_g1{rr}"], out=g1[rr][:],
                            in0=u_b[cb][:, i:T:8], scalar=dvec[:, ct:ct + 1], in1=psb[py][:], op0=ALU.mult, op1=ALU.add)
                        vop("scalar", "activation", [f"s5_g1{rr}"], [f"s5_g2{rr}"], out=g2[rr][:], in_=g1[rr][:], func=AF.Square,
                            scale=math.sqrt(0.044715))
                        vop("vector", "scalar_tensor_tensor", [f"s5_g2{rr}", f"s5_g1{rr}"], [f"s5_g2{rr}"], out=g2[rr][:],
                            in0=g2[rr][:], scalar=1.0, in1=g1[rr][:], op0=ALU.add, op1=ALU.mult)
                        vop("scalar", "activation", [f"s5_g2{rr}"], [f"s5_g2{rr}"], out=g2[rr][:], in_=g2[rr][:], func=AF.Sigmoid,
                            scale=1.5957691216057308)
                        vop("vector", "tensor_tensor", [f"s5_g1{rr}", f"s5_g2{rr}"], [f"s5_yg{ct}"], out=yg[ct][:, i:T:8],
                            in0=g1[rr][:], in1=g2[rr][:], op=ALU.mult)

        sst = [sA, sB, sC]
        for step in range(len(items) + len(sst) - 1):
            for j in range(len(sst) - 1, -1, -1):
                t_ = step - j
                if 0 <= t_ < len(items):
                    sst[j](items[t_])
        P.end_phase()
        mst.close()
        sbt = sbt_outer
        yo_s = [sbt(f"s5_yo{i}", [128, 4, 512], BF16) for i in range(2)]
        for tt in range(8):
            cs = slice(tt * 512, (tt + 1) * 512)
            s_ = tt % 2
            for oc in range(4):
                pi = oc
                for ct in range(4):
                    mm(pi, gw[:, ct, oc * 128:(oc + 1) * 128], yg[ct][:, cs], ct == 0, ct == 3, [k("gw"), f"s5_yg{ct}"])
                r = oc % 2
                vop("scalar", "activation", [f"psb{pi}", k("gb")], [f"s5_g1{r}"], out=g1[r][:], in_=psb[pi][:], func=AF.Sigmoid,
                    bias=gb[:, oc:oc + 1])
                vop("vector", "tensor_tensor", [f"s5_g1{r}", f"s5_yg{oc}"], [f"s5_yo{s_}"], out=yo_s[s_][:, oc, :], in0=g1[r][:],
                    in1=yg[oc][:, cs], op=ALU.mult)
            P.dma("sync", ymix1.rearrange("(c p) t -> p c t", p=128)[:, 0:4, cs], yo_s[s_][:], reads=[f"s5_yo{s_}"],
                  writes=["ymix1"])
        P.end_phase()

    if upto <= 7:
        P.finish()
        return nc

    out_ffn(1)
    P.finish()
    return nc


INPUT_ORDER = ["x", "ab_norm", "ab_w_in", "ab_conv_w", "ab_conv_b", "ab_gate_a_w", "ab_gate_a_b",
               "ab_gate_x_w", "ab_gate_x_b", "ab_lambda", "ab_w_out", "cd_norm", "cd_w_in",
               "cd_lam_re", "cd_lam_im", "cd_log_dt", "cd_b_re", "cd_b_im", "cd_c_re", "cd_c_im",
               "cd_d", "cd_glu_w", "cd_glu_b", "cd_w_out", "ffn_norm", "ffn_w_gate", "ffn_w_up",
               "ffn_w_down", "final_norm"]


def make_in_maps(inputs, cores):
    f = lambda a: np.ascontiguousarray(np.asarray(a, dtype=np.float32))
    shared = {
        "ab_norm": f(inputs["ab_norm"][0]), "ab_w_in": f(inputs["ab_w_in"][0]),
        "ab_conv_w": f(inputs["ab_conv_w"][0, :, 0, :]), "ab_conv_b": f(inputs["ab_conv_b"][0]),
        "ab_gate_a_w": f(inputs["ab_gate_a_w"][0]), "ab_gate_a_b": f(inputs["ab_gate_a_b"][0].reshape(512)),
        "ab_gate_x_w": f(inputs["ab_gate_x_w"][0]), "ab_gate_x_b": f(inputs["ab_gate_x_b"][0].reshape(512)),
        "ab_lambda": f(inputs["ab_lambda"][0]), "ab_w_out": f(inputs["ab_w_out"][0]),
        "cd_norm": f(inputs["cd_norm"][0]), "cd_w_in": f(inputs["cd_w_in"][0]),
        "cd_lam_re": f(inputs["cd_lam_re"][0]), "cd_lam_im": f(inputs["cd_lam_im"][0]),
        "cd_log_dt": f(inputs["cd_log_dt"][0]), "cd_b_re": f(inputs["cd_b_re"][0]),
        "cd_b_im": f(inputs["cd_b_im"][0]), "cd_c_re": f(inputs["cd_c_re"][0]),
        "cd_c_im": f(inputs["cd_c_im"][0]), "cd_d": f(inputs["cd_d"][0]),
        "cd_glu_w": f(inputs["cd_glu_w"][0]), "cd_glu_b": f(inputs["cd_glu_b"][0]),
        "cd_w_out": f(inputs["cd_w_out"][0]), "ffn_norm": f(inputs["ffn_norm"]),
        "ffn_w_gate": f(inputs["ffn_w_gate"]), "ffn_w_up": f(inputs["ffn_w_up"]),
        "ffn_w_down": f(inputs["ffn_w_down"]), "final_norm": f(inputs["final_norm"]),
    }
    maps = []
    for b in cores:
        m = dict(shared)
        m["x"] = f(inputs["x"][b])
        maps.append(m)
    return maps


def kernel(**inputs):
    nc = build()
    in_maps = make_in_maps(inputs, list(range(8)))
    res = run_bass_kernel_spmd(nc, in_maps, core_ids=list(range(8)))
    return np.stack([np.asarray(r["y"]) for r in res.results], axis=0).astype(np.float32)
```
